# Optimizing a Trainium2 kernel written in Bass

```python
import math
import jax
import jax.numpy as jnp
from jax import lax
import numpy as np

D_MODEL = 1024
BATCH = 8
SEQ = 4096
DEPTH = 4

GRID_W = 64
N_MEM = 256
HEAD_DIM = 64
MIX_WIDTH = D_MODEL
POOL_WIDTH = MIX_WIDTH // 4
POOL_WINDOWS = (2, 4, 8, 16)
POOL_GROUP = POOL_WIDTH // len(POOL_WINDOWS)
MLSTM_WIDTH = MIX_WIDTH // 4
MLSTM_HEADS = MLSTM_WIDTH // HEAD_DIM
MLSTM_CHUNK = 128
MLSTM_CONV = 5
ATTN_Q_WIDTH = MIX_WIDTH // 2
ATTN_Q_HEADS = ATTN_Q_WIDTH // HEAD_DIM
ATTN_KV_HEADS = ATTN_Q_HEADS // 4
ATTN_KV_WIDTH = ATTN_KV_HEADS * HEAD_DIM
Q_BLOCK = 128
ROPE_THETA = 10000.0
IN_SPLITS = (POOL_WIDTH, MLSTM_WIDTH, MLSTM_WIDTH, MLSTM_WIDTH, MLSTM_WIDTH,
             2 * MLSTM_HEADS, 2 * MLSTM_HEADS, ATTN_Q_WIDTH, ATTN_KV_WIDTH, ATTN_KV_WIDTH)
IN_COLS = sum(IN_SPLITS)
CA_HEADS = 4
CA_WIDTH = D_MODEL // 2
CA_HEAD_DIM = CA_WIDTH // CA_HEADS
N_EXPERTS = 16
EC_CAPACITY_FACTOR = 2
D_FF_EXPERT = 2 * D_MODEL
EPS = 1e-6

kernel_name = 'hybrid_pool_mlstm_gqa_ec_encoder'

F32 = jnp.float32


def rmsnorm(x, g):
    xf = x.astype(F32)
    y = xf * lax.rsqrt(jnp.mean(xf * xf, axis=-1, keepdims=True) + EPS)
    return (y * g.astype(F32)).astype(x.dtype)


def head_rmsnorm(a, g):
    af = a.astype(F32)
    return af * lax.rsqrt(jnp.mean(af * af, axis=-1, keepdims=True) + EPS) * g.astype(F32)


def axial_rope_tables(s):
    rows = s // GRID_W
    row_ids = jnp.repeat(jnp.arange(rows), GRID_W).astype(F32)
    col_ids = jnp.tile(jnp.arange(GRID_W), rows).astype(F32)
    n_freq = HEAD_DIM // 4
    inv_freq = ROPE_THETA ** (-jnp.arange(n_freq, dtype=F32) / n_freq)
    ang = jnp.concatenate([row_ids[:, None] * inv_freq, col_ids[:, None] * inv_freq], axis=-1)
    return jnp.cos(ang), jnp.sin(ang)


def apply_rope(a, cos, sin):
    a2 = a.reshape(*a.shape[:-1], HEAD_DIM // 2, 2)
    x0, x1 = a2[..., 0], a2[..., 1]
    c = cos[None, :, None, :]
    sn = sin[None, :, None, :]
    return jnp.stack([x0 * c - x1 * sn, x0 * sn + x1 * c], axis=-1).reshape(a.shape)


def pool_mixer(u, w, scale):
    b, s, _ = u.shape
    uf = u.astype(F32)
    cs = jnp.concatenate([jnp.zeros((b, 1, POOL_WIDTH), F32), jnp.cumsum(uf, axis=1)], axis=1)
    t = jnp.arange(s)
    diffs = []
    for g, win in enumerate(POOL_WINDOWS):
        lo = jnp.clip(t - win // 2, 0, s)
        hi = jnp.clip(t + win // 2, 0, s)
        c0 = g * POOL_GROUP
        csg = cs[:, :, c0:c0 + POOL_GROUP]
        mean = (csg[:, hi] - csg[:, lo]) / (hi - lo).astype(F32)[None, :, None]
        diffs.append(mean - uf[:, :, c0:c0 + POOL_GROUP])
    d = jnp.stack(diffs, axis=2)
    y = jnp.einsum('bsgc,gce->bsge', d, w.astype(F32)).reshape(b, s, POOL_WIDTH)
    return (y * scale.astype(F32)).astype(u.dtype)


def mlstm_scan(q, k, v, log_i, log_f):
    b, h, s, dh = q.shape
    nc = s // MLSTM_CHUNK

    def chunks(a):
        a = a.reshape(b, h, nc, MLSTM_CHUNK, *a.shape[3:])
        return jnp.moveaxis(a, 2, 0)

    lower = jnp.tril(jnp.ones((MLSTM_CHUNK, MLSTM_CHUNK), dtype=bool))

    def step(carry, inp):
        c_st, n_st, m_st = carry
        qc, kc, vc, li, lf = inp
        bcum = jnp.cumsum(lf, axis=-1)
        dmat = jnp.where(lower, bcum[..., :, None] - bcum[..., None, :] + li[..., None, :], -jnp.inf)
        inter = bcum + m_st[..., None]
        m_j = jnp.maximum(inter, jnp.max(dmat, axis=-1))
        a_mat = jnp.exp(dmat - m_j[..., None]) * jnp.einsum('bhjd,bhsd->bhjs', qc, kc)
        a_int = jnp.exp(inter - m_j)
        num = (jnp.einsum('bhjs,bhse->bhje', a_mat, vc)
               + a_int[..., None] * jnp.einsum('bhed,bhjd->bhje', c_st, qc))
        den = jnp.sum(a_mat, axis=-1) + a_int * jnp.einsum('bhd,bhjd->bhj', n_st, qc)
        h_out = num / jnp.maximum(jnp.abs(den), jnp.exp(-m_j))[..., None]
        b_last = bcum[..., -1]
        g = b_last[..., None] - bcum + li
        m_new = jnp.maximum(b_last + m_st, jnp.max(g, axis=-1))
        w_s = jnp.exp(g - m_new[..., None])
        decay = jnp.exp(b_last + m_st - m_new)
        c_new = decay[..., None, None] * c_st + jnp.einsum('bhs,bhse,bhsd->bhed', w_s, vc, kc)
        n_new = decay[..., None] * n_st + jnp.einsum('bhs,bhsd->bhd', w_s, kc)
        return (c_new, n_new, m_new), h_out

    init = (jnp.zeros((b, h, dh, dh), F32), jnp.zeros((b, h, dh), F32), jnp.zeros((b, h), F32))
    _, hs = lax.scan(step, init, (chunks(q), chunks(k), chunks(v), chunks(log_i), chunks(log_f)))
    return jnp.moveaxis(hs, 0, 2).reshape(b, h, s, dh)


def mlstm_mixer(q, k, v, o, i_pre, f_pre, conv_w, norm_g):
    b, s, _ = q.shape
    qk = jnp.concatenate([q, k], axis=-1)
    qk = lax.conv_general_dilated(qk, conv_w[:, None, :].astype(qk.dtype), window_strides=(1,),
                                  padding=[(MLSTM_CONV // 2, MLSTM_CONV // 2)],
                                  dimension_numbers=('NWC', 'WIO', 'NWC'),
                                  feature_group_count=2 * MLSTM_WIDTH)
    qk = jax.nn.silu(qk)
    q, k = jnp.split(qk, 2, axis=-1)

    def heads(a):
        return a.astype(F32).reshape(b, s, MLSTM_HEADS, HEAD_DIM).transpose(0, 2, 1, 3)

    qh, kh, vh = heads(q), heads(k) / math.sqrt(HEAD_DIM), heads(v)
    log_i = i_pre.astype(F32).reshape(b, s, 2, MLSTM_HEADS).transpose(2, 0, 3, 1)
    log_f = jax.nn.log_sigmoid(f_pre.astype(F32)).reshape(b, s, 2, MLSTM_HEADS).transpose(2, 0, 3, 1)
    flip = lambda a: jnp.flip(a, axis=2)
    h_fwd = mlstm_scan(qh, kh, vh, log_i[0], log_f[0])
    h_bwd = flip(mlstm_scan(flip(qh), flip(kh), flip(vh), flip(log_i[1]), flip(log_f[1])))
    hsum = h_fwd + h_bwd
    hsum = hsum * lax.rsqrt(jnp.mean(hsum * hsum, axis=-1, keepdims=True) + EPS)
    hsum = hsum.transpose(0, 2, 1, 3).reshape(b, s, MLSTM_WIDTH) * norm_g.astype(F32)
    return (hsum * jax.nn.sigmoid(o.astype(F32))).astype(q.dtype)


def attn_mixer(q, k, v, qn_g, kn_g, cos, sin):
    b, s, _ = q.shape
    group = ATTN_Q_HEADS // ATTN_KV_HEADS
    qh = apply_rope(head_rmsnorm(q.reshape(b, s, ATTN_Q_HEADS, HEAD_DIM), qn_g), cos, sin)
    kh = apply_rope(head_rmsnorm(k.reshape(b, s, ATTN_KV_HEADS, HEAD_DIM), kn_g), cos, sin)
    qh = (qh * HEAD_DIM ** -0.5).astype(q.dtype)
    kh = kh.astype(k.dtype).transpose(0, 2, 1, 3)
    vh = v.reshape(b, s, ATTN_KV_HEADS, HEAD_DIM).transpose(0, 2, 1, 3)
    nb = s // Q_BLOCK
    qb = qh.reshape(b, nb, Q_BLOCK, ATTN_KV_HEADS, group, HEAD_DIM).transpose(1, 0, 3, 4, 2, 5)

    def block(qi):
        sc = jnp.einsum('bhgqd,bhkd->bhgqk', qi, kh, preferred_element_type=F32)
        p = jax.nn.softmax(sc, axis=-1).astype(vh.dtype)
        return jnp.einsum('bhgqk,bhkd->bhgqd', p, vh)

    ob = lax.map(block, qb)
    return ob.transpose(1, 0, 4, 2, 3, 5).reshape(b, s, ATTN_Q_WIDTH)


def mem_cross_attn(hn, mem, wq, wkv, wo):
    b, s, _ = hn.shape
    q = (hn @ wq).reshape(b, s, CA_HEADS, CA_HEAD_DIM)
    kv = (mem @ wkv).reshape(b, mem.shape[1], 2, CA_HEADS, CA_HEAD_DIM)
    sc = jnp.einsum('bshd,bmhd->bhsm', q, kv[:, :, 0], preferred_element_type=F32) * CA_HEAD_DIM ** -0.5
    p = jax.nn.softmax(sc, axis=-1).astype(hn.dtype)
    o = jnp.einsum('bhsm,bmhd->bshd', p, kv[:, :, 1]).reshape(b, s, CA_WIDTH)
    return o @ wo


def ec_moe(h, router_w, w_gu, w_down):
    b, n, d = h.shape
    cap = EC_CAPACITY_FACTOR * n // N_EXPERTS
    logits = jnp.einsum('bnd,de->bne', h, router_w, preferred_element_type=F32)
    aff = jax.nn.softmax(logits, axis=-1)
    gates, idx = lax.top_k(jnp.swapaxes(aff, 1, 2), cap)
    xs = jax.vmap(lambda hb, ib: hb[ib])(h, idx)

    def expert(args):
        xe, wgu, wd = args
        gu = jnp.einsum('bcd,df->bcf', xe, wgu)
        ga, up = jnp.split(gu, 2, axis=-1)
        return jnp.einsum('bcf,fd->bcd', jax.nn.silu(ga) * up, wd)

    ys = lax.map(expert, (jnp.swapaxes(xs, 0, 1), w_gu, w_down))
    ys = jnp.swapaxes(ys, 0, 1) * gates[..., None].astype(ys.dtype)
    return jax.vmap(lambda ib, yb: jnp.zeros((n, d), yb.dtype).at[ib.reshape(-1)].add(yb.reshape(-1, d)))(idx, ys)


def setup_inputs(seed: int = 0) -> dict:
    key = jax.random.key(seed)
    ks = jax.random.split(key, 24)
    nrm = lambda k, shape, scale: jax.random.normal(k, shape, F32) * scale
    gain = lambda k, shape: 1.0 + 0.05 * jax.random.normal(k, shape, F32)
    gate_b = jnp.concatenate([nrm(ks[7], (DEPTH, 2 * MLSTM_HEADS), 0.1),
                              3.0 + nrm(ks[8], (DEPTH, 2 * MLSTM_HEADS), 0.5)], axis=-1)
    return {
        'x': nrm(ks[0], (BATCH, SEQ, D_MODEL), 1.0),
        'mem': nrm(ks[1], (BATCH, N_MEM, D_MODEL), 1.0),
        'norm_mix_g': gain(ks[2], (DEPTH, D_MODEL)),
        'w_in': nrm(ks[3], (DEPTH, D_MODEL, IN_COLS), D_MODEL ** -0.5),
        'pool_w': nrm(ks[4], (DEPTH, len(POOL_WINDOWS), POOL_GROUP, POOL_GROUP), POOL_GROUP ** -0.5),
        'pool_scale': gain(ks[5], (DEPTH, POOL_WIDTH)),
        'mlstm_conv_w': nrm(ks[6], (DEPTH, MLSTM_CONV, 2 * MLSTM_WIDTH), MLSTM_CONV ** -0.5),
        'mlstm_gate_b': gate_b,
        'mlstm_norm_g': gain(ks[9], (DEPTH, MLSTM_WIDTH)),
        'q_norm_g': gain(ks[10], (DEPTH, HEAD_DIM)),
        'k_norm_g': gain(ks[11], (DEPTH, HEAD_DIM)),
        'w_out': nrm(ks[12], (DEPTH, MIX_WIDTH, D_MODEL), MIX_WIDTH ** -0.5),
        'norm_mem_g': gain(ks[13], (DEPTH, D_MODEL)),
        'ca_wq': nrm(ks[14], (DEPTH, D_MODEL, CA_WIDTH), D_MODEL ** -0.5),
        'ca_wkv': nrm(ks[15], (DEPTH, D_MODEL, 2 * CA_WIDTH), D_MODEL ** -0.5),
        'ca_wo': nrm(ks[16], (DEPTH, CA_WIDTH, D_MODEL), CA_WIDTH ** -0.5),
        'norm_ffn_g': gain(ks[17], (DEPTH, D_MODEL)),
        'router_w': nrm(ks[18], (DEPTH, D_MODEL, N_EXPERTS), D_MODEL ** -0.5),
        'expert_w_gu': nrm(ks[19], (DEPTH, N_EXPERTS, D_MODEL, 2 * D_FF_EXPERT), D_MODEL ** -0.5),
        'expert_w_down': nrm(ks[20], (DEPTH, N_EXPERTS, D_FF_EXPERT, D_MODEL), D_FF_EXPERT ** -0.5),
        'final_norm_g': gain(ks[21], (D_MODEL,)),
    }


def reference(x, mem, norm_mix_g, w_in, pool_w, pool_scale, mlstm_conv_w, mlstm_gate_b,
              mlstm_norm_g, q_norm_g, k_norm_g, w_out, norm_mem_g, ca_wq, ca_wkv, ca_wo,
              norm_ffn_g, router_w, expert_w_gu, expert_w_down, final_norm_g):
    s = x.shape[1]
    cos, sin = axial_rope_tables(s)
    offsets = np.cumsum(IN_SPLITS)[:-1].tolist()
    n_gate = 2 * MLSTM_HEADS
    for layer in range(DEPTH):
        hn = rmsnorm(x, norm_mix_g[layer])
        proj = hn @ w_in[layer]
        u_pool, m_q, m_k, m_v, m_o, m_i, m_f, a_q, a_k, a_v = jnp.split(proj, offsets, axis=-1)
        gb = mlstm_gate_b[layer]
        y_pool = pool_mixer(u_pool, pool_w[layer], pool_scale[layer])
        y_mlstm = mlstm_mixer(m_q, m_k, m_v, m_o, m_i + gb[:n_gate], m_f + gb[n_gate:],
                              mlstm_conv_w[layer], mlstm_norm_g[layer])
        y_attn = attn_mixer(a_q, a_k, a_v, q_norm_g[layer], k_norm_g[layer], cos, sin)
        x = x + jnp.concatenate([y_pool, y_mlstm, y_attn], axis=-1) @ w_out[layer]
        x = x + mem_cross_attn(rmsnorm(x, norm_mem_g[layer]), mem, ca_wq[layer], ca_wkv[layer], ca_wo[layer])
        x = x + ec_moe(rmsnorm(x, norm_ffn_g[layer]), router_w[layer], expert_w_gu[layer], expert_w_down[layer])
    return rmsnorm(x, final_norm_g)
```

```python
import numpy as np
from contextlib import ExitStack
import concourse.bass as bass
import concourse.mybir as mybir
from concourse.bass_utils import run_bass_kernel_spmd

F32 = mybir.dt.float32
BF16 = mybir.dt.bfloat16
U32 = mybir.dt.uint32
I32 = mybir.dt.int32
AF = mybir.ActivationFunctionType
ALU = mybir.AluOpType
AX = mybir.AxisListType

S = 4096
D = 1024
NT = S // 128
DEPTH = 4
INC = 2064
EPS = 1e-6
NEXP = 16
CAP = 512
DFF = 2048


class Tile:
    def __init__(self, kb, t, name):
        self.kb = kb
        self.t = t
        self.name = name
        self.last_write = None
        self.readers = {}
        self.dsem = None

    def __getitem__(self, key):
        return TAP(self.t[key], self)

    def ap(self):
        return TAP(self.t[:], self)


class TAP:
    def __init__(self, ap, tile):
        self.ap = ap
        self.tile = tile

    def __getitem__(self, key):
        return TAP(self.ap[key], self.tile)

    def __getattr__(self, name):
        attr = getattr(self.ap, name)
        if callable(attr):
            def f(*a, **kw):
                r = attr(*a, **kw)
                if isinstance(r, bass.AP):
                    return TAP(r, self.tile)
                return r
            return f
        return attr


class Eng:
    def __init__(self, kb, name, eng, sem):
        self.kb = kb
        self.name = name
        self.eng = eng
        self.sem = sem
        self.count = 0
        self.known = {}

    def __getattr__(self, fn):
        def f(*a, **kw):
            return self.kb.emit(self, fn, a, kw)
        return f


class KB:
    def __init__(self, nc, es, n_dma_sems=92):
        self.nc = nc
        self.es = es
        self.sems = {}
        self.engs = {}
        for name, eng in (("pe", nc.tensor), ("dve", nc.vector), ("act", nc.scalar),
                          ("pool", nc.gpsimd), ("sp", nc.sync)):
            sem = es.enter_context(nc.semaphore("S_" + name))
            self.sems[name] = sem
            self.engs[name] = Eng(self, name, eng, name)
        self.pe, self.dve, self.act, self.pool, self.sp = (self.engs[n] for n in ("pe", "dve", "act", "pool", "sp"))
        self.dma_free = []
        self.dma_count = {}
        for i in range(n_dma_sems):
            key = "D%d" % i
            self.sems[key] = es.enter_context(nc.semaphore(key))
            self.dma_count[key] = 0
            self.dma_free.append(key)
        self.phase_tiles = []
        self.phase_stack = None
        self.all_tiles = []
        self.n_inst = 0
        self.n_wait = 0

    def begin_phase(self):
        self.phase_stack = ExitStack()
        self.phase_tiles = []

    def end_phase(self):
        self.barrier()
        for t in self.phase_tiles:
            if t.dsem is not None:
                self.dma_free.append(t.dsem)
                t.dsem = None
        self.phase_stack.close()
        self.phase_stack = None
        self.phase_tiles = []

    def begin_hold(self):
        self.hold_stack = ExitStack()
        self.hold_tiles = []

    def end_hold(self):
        for t in self.hold_tiles:
            if t.dsem is not None:
                self.dma_free.append(t.dsem)
                t.dsem = None
        self.hold_stack.close()
        self.hold_stack = None
        self.hold_tiles = []

    def sb(self, name, shape, dtype, glob=False, hold=False):
        st = self.es if glob else (self.hold_stack if hold else self.phase_stack)
        self.n_alloc = getattr(self, "n_alloc", 0) + 1
        name = "%s_%d" % (name, self.n_alloc)
        t = st.enter_context(self.nc.sbuf_tensor(name, list(shape), dtype))
        tl = Tile(self, t, name)
        if hold:
            self.hold_tiles.append(tl)
        elif not glob:
            self.phase_tiles.append(tl)
        self.all_tiles.append(tl)
        return tl

    def ps(self, name, shape, dtype):
        t = self.es.enter_context(self.nc.psum_tensor(name, list(shape), dtype))
        tl = Tile(self, t, name)
        self.all_tiles.append(tl)
        return tl

    def token(self, name):
        tl = Tile(self, None, name)
        self.all_tiles.append(tl)
        return tl

    def tile_dsem(self, tile):
        if tile.dsem is None:
            tile.dsem = self.dma_free.pop()
        return tile.dsem

    def emit(self, E, fn, args, kw):
        reads, writes = [], []
        kw = dict(kw)
        xr = kw.pop("_reads", [])
        xw = kw.pop("_writes", [])

        def unwrap(v, is_out):
            if isinstance(v, TAP):
                (writes if is_out else reads).append(v.tile)
                return v.ap
            return v
        a2 = [unwrap(a, (i == 0 and fn in ("matmul", "transpose"))) for i, a in enumerate(args)]
        kw2 = {k_: unwrap(v, k_ in ("out", "accum_out", "ap", "out_ap")) for k_, v in kw.items()}
        reads.extend(xr)
        writes.extend(xw)
        is_dma = fn in ("dma_start", "indirect_dma_start")
        deps = {}

        def add(ev):
            if ev is None:
                return
            sk, val, src = ev
            if src is E and E.name == "pe":
                return
            if deps.get(sk, 0) < val:
                deps[sk] = val
        for t in reads:
            add(t.last_write)
        for t in writes:
            add(t.last_write)
            for sk, (val, src) in t.readers.items():
                add((sk, val, src))
        for sk, val in deps.items():
            if E.known.get(sk, 0) >= val:
                continue
            E.eng.wait_ge(self.sems[sk], val)
            E.known[sk] = val
            self.n_wait += 1
        inst = getattr(E.eng, fn)(*a2, **kw2)
        self.n_inst += 1
        if is_dma:
            sbt = None
            for t in writes + reads:
                if t.t is not None:
                    sbt = t
                    break
            sk = self.tile_dsem(sbt)
            self.dma_count[sk] += 16
            inst.then_inc(self.sems[sk], 16)
            ev = (sk, self.dma_count[sk], None)
        else:
            E.count += 1
            inst.then_inc(self.sems[E.sem], 1)
            ev = (E.sem, E.count, E)
        for t in writes:
            t.last_write = ev
            t.readers = {}
        for t in reads:
            if t in writes:
                continue
            sk, val, src = ev
            if t.readers.get(sk, (0, None))[0] < val:
                t.readers[sk] = (val, src)
        return inst

    def barrier(self):
        sp = self.sp
        for name in ("pe", "dve", "act", "pool"):
            e = self.engs[name]
            if sp.known.get(name, 0) < e.count:
                sp.eng.wait_ge(self.sems[name], e.count)
                sp.known[name] = e.count
        for sk, c in self.dma_count.items():
            if c > 0 and sp.known.get(sk, 0) < c:
                sp.eng.wait_ge(self.sems[sk], c)
                sp.known[sk] = c
        sp.count += 1
        sp.eng.nop().then_inc(self.sems["sp"], 1)
        for name in ("pe", "dve", "act", "pool"):
            e = self.engs[name]
            e.eng.wait_ge(self.sems["sp"], sp.count)
        for e in self.engs.values():
            for n2, e2 in self.engs.items():
                e.known[n2] = e2.count
            for sk, c in self.dma_count.items():
                e.known[sk] = c
        for t in self.all_tiles:
            t.last_write = None
            t.readers = {}


def bfview(pt):
    return pt[:].bitcast(BF16)


def load_cast(kb, dst, src, eng=None):
    n = src.shape[-1]
    step = 2048
    for c0 in range(0, n, step):
        c1 = min(n, c0 + step)
        kb.pool.dma_start(out=dst[..., c0:c1], in_=src[..., c0:c1])


DBG = {}


def build(depth=DEPTH, debug=(), phases=None, moe_depth=DEPTH, final_norm=True):
    nc = bass.Bass("TRN2", target_bir_lowering=False)
    dbg = set(debug)

    def din(name, shape, dt=F32):
        return nc.dram_tensor(name, list(shape), dt, kind="ExternalInput").ap()

    def dscr(name, shape, dt=F32):
        kind = "ExternalOutput" if name in dbg else "Internal"
        return nc.dram_tensor(name, list(shape), dt, kind=kind).ap()

    x_in = din("x", [S, D])
    mem_in = din("mem", [256, D])
    W = {}
    W["norm_mix_g"] = din("norm_mix_g", [DEPTH, D])
    W["w_in"] = din("w_in", [DEPTH, D, INC])
    W["pool_w"] = din("pool_w", [DEPTH, 4, 64, 64])
    W["pool_scale"] = din("pool_scale", [DEPTH, 256])
    W["mlstm_conv_wT"] = din("mlstm_conv_wT", [DEPTH, 512, 5])
    W["mlstm_gate_b"] = din("mlstm_gate_b", [DEPTH, 16])
    W["mlstm_norm_g"] = din("mlstm_norm_g", [DEPTH, 256])
    W["q_norm_g"] = din("q_norm_g", [DEPTH, 64])
    W["k_norm_g"] = din("k_norm_g", [DEPTH, 64])
    W["w_out"] = din("w_out", [DEPTH, D, D])
    W["norm_mem_g"] = din("norm_mem_g", [DEPTH, D])
    W["ca_wq"] = din("ca_wq", [DEPTH, D, 512])
    W["ca_wkv"] = din("ca_wkv", [DEPTH, D, D])
    W["ca_wo"] = din("ca_wo", [DEPTH, 512, D])
    W["norm_ffn_g"] = din("norm_ffn_g", [DEPTH, D])
    W["router_w"] = din("router_w", [DEPTH, D, NEXP])
    W["expert_w_gu"] = din("expert_w_gu", [moe_depth, NEXP, D, 2 * DFF])
    W["expert_w_down"] = din("expert_w_down", [moe_depth, NEXP, DFF, D])
    W["final_norm_g"] = din("final_norm_g", [D])
    C_cos = din("c_cos", [S, 32])
    C_sin = din("c_sin", [S, 32])
    C_identf = din("c_identf", [128, 128])
    C_invc = din("c_invc", [256, S])
    C_tri = din("c_tri", [128, 256])
    out = nc.dram_tensor("out", [S, D], F32, kind="ExternalOutput").ap()

    xs = dscr("xs", [S, D])
    featT = dscr("featT", [768, S])
    mv_d = dscr("mv_d", [S, 256], BF16)
    mo_d = dscr("mo_d", [S, 256], BF16)
    gates_d = dscr("gates_d", [S, 16])
    qT_d = dscr("qT_d", [4, 128, S], BF16)
    kT_d = dscr("kT_d", [2, 128, S], BF16)
    av_d = dscr("av_d", [S, 128], BF16)
    yT_d = dscr("yT_d", [D, S], BF16)
    hn_d = dscr("hn_d", [S, D], BF16)
    aff_d = dscr("aff_d", [NEXP, S])
    qk_d = dscr("qk_d", [4, 128, S], BF16)

    DBG.clear()
    if "dbg_X" in dbg:
        DBG["X"] = dscr("dbg_X", [128, 4 * D], BF16)
        DBG["hT"] = dscr("dbg_hT", [128, 16 * CAP], BF16)
        DBG["yg"] = dscr("dbg_yg", [128, D], F32)
    if "dbg_idx" in dbg:
        DBG["idx"] = dscr("dbg_idx", [128, 64], U32)
        DBG["g"] = dscr("dbg_g", [128, 64], F32)
    es = ExitStack()
    with es:
        kb = KB(nc, es)
        pe, dve, act, pool, sp = kb.pe, kb.dve, kb.act, kb.pool, kb.sp
        PS = [kb.ps("psum%d" % i, [128, 512], F32) for i in range(8)]
        identf = kb.sb("identf", [128, 128], F32, glob=True)
        identb = kb.sb("identb", [128, 128], BF16, glob=True)
        idxT = kb.sb("G_idxT", [128, 4, NEXP], U32, glob=True)
        gT = kb.sb("G_gT", [128, 4, NEXP], F32, glob=True)
        sp.dma_start(out=identf[:], in_=C_identf[:, :])
        pool.dma_start(out=identb[:], in_=C_identf[:, :])

        for layer in range(depth):
            if phases is None or "A" in phases:
                phase_A(kb, nc, layer, W, PS, identb, (x_in if layer == 0 else xs), featT, mv_d, mo_d, gates_d, qT_d, kT_d, av_d, C_cos, C_sin)
            if phases is None or "B" in phases:
                phase_B(kb, nc, layer, W, PS, featT, yT_d, C_invc)
            if phases is None or "C" in phases:
                phase_C(kb, nc, layer, W, PS, identb, featT, mv_d, mo_d, gates_d, qk_d, yT_d, C_tri)
            efw = None
            if phases is None or "E" in phases:
                kb.begin_hold()
                efw = {"wo": kb.sb("E_wo", [128, 8, D], BF16, hold=True), "wq": kb.sb("E_wq", [128, 8, 512], BF16, hold=True),
                       "wkv": kb.sb("E_wkv", [128, 8, D], BF16, hold=True), "cwo": kb.sb("E_cwo", [128, 4, D], BF16, hold=True)}
                for c in range(8):
                    kb.pool.dma_start(out=efw["wo"][:, c, :], in_=W["w_out"][layer][c * 128:(c + 1) * 128, :])
                    kb.pool.dma_start(out=efw["wq"][:, c, :], in_=W["ca_wq"][layer][c * 128:(c + 1) * 128, :])
                    kb.pool.dma_start(out=efw["wkv"][:, c, :], in_=W["ca_wkv"][layer][c * 128:(c + 1) * 128, :])
                for c in range(4):
                    kb.pool.dma_start(out=efw["cwo"][:, c, :], in_=W["ca_wo"][layer][c * 128:(c + 1) * 128, :])
            if phases is None or "D" in phases:
                phase_D(kb, nc, layer, PS, qT_d, kT_d, av_d, yT_d)
            if phases is None or "E" in phases:
                phase_EF(kb, nc, layer, W, PS, identb, (x_in if layer == 0 else xs), xs, yT_d, mem_in, efw,
                         g1args={"identf": identf, "hn_d": hn_d, "aff_d": aff_d})
                kb.end_hold()
            if phases is None or "G" in phases:
                phase_G(kb, nc, layer, W, PS, identb, identf, xs, hn_d, idxT, gT, aff_d)

        kb.begin_phase()
        cp = [kb.sb("cpo%d" % i, [128, 4, D], F32) for i in range(2)]
        co = [kb.sb("cpq%d" % i, [128, 4, D], F32) for i in range(2)]
        fst = [kb.sb("fst%d" % i, [128, 4], F32) for i in range(2)]
        fjunk = kb.sb("fjunk", [128, D], BF16)
        fg = kb.sb("fg", [128, D], F32)
        sp.dma_start(out=fg[:], in_=W["final_norm_g"].partition_broadcast(128))
        for blk in range(8):
            t = cp[blk % 2]
            o = co[blk % 2]
            stt = fst[blk % 2]
            rows = slice(blk * 512, (blk + 1) * 512)
            sp.dma_start(out=t[:], in_=xs[rows, :].rearrange("(j p) d -> p j d", p=128))
            if final_norm:
                for j in range(4):
                    act.activation(out=fjunk[:], in_=t[:, j, :], func=AF.Square, accum_out=stt[:, j:j + 1])
                act.activation(out=stt[:], in_=stt[:], func=AF.Sqrt, scale=1.0 / D, bias=EPS)
                dve.reciprocal(out=stt[:], in_=stt[:])
                for j in range(4):
                    dve.scalar_tensor_tensor(out=o[:, j, :], in0=t[:, j, :], scalar=stt[:, j:j + 1], in1=fg[:], op0=ALU.mult, op1=ALU.mult)
                sp.dma_start(out=out[rows, :].rearrange("(j p) d -> p j d", p=128), in_=o[:])
            else:
                sp.dma_start(out=out[rows, :].rearrange("(j p) d -> p j d", p=128), in_=t[:])
        kb.end_phase()
        print("instructions", kb.n_inst, "waits", kb.n_wait)
    return nc


def phase_A(kb, nc, layer, W, PS, identb, xs, featT, mv_d, mo_d, gates_d, qT_d, kT_d, av_d, C_cos, C_sin):
    pe, dve, act, pool, sp = kb.pe, kb.dve, kb.act, kb.pool, kb.sp
    kb.begin_phase()
    w = kb.sb("A_w", [128, 8, INC], BF16)
    wsrc = W["w_in"][layer].rearrange("(c p) n -> p c n", p=128)
    for c in range(8):
        pool.dma_start(out=w[:, c, 0:1280], in_=wsrc[:, c, 0:1280])
        pool.dma_start(out=w[:, c, 1280:1792], in_=wsrc[:, c, 1296:1808])
        pool.dma_start(out=w[:, c, 1792:1808], in_=wsrc[:, c, 1280:1296])
        pool.dma_start(out=w[:, c, 1808:2064], in_=wsrc[:, c, 1808:2064])
    g_bc = kb.sb("A_g", [128, D], F32)
    sp.dma_start(out=g_bc[:], in_=W["norm_mix_g"][layer].partition_broadcast(128))
    gq = kb.sb("A_gq", [128, 64], F32)
    gk = kb.sb("A_gk", [128, 64], F32)
    gb = kb.sb("A_gb", [128, 16], F32)
    sp.dma_start(out=gq[:], in_=W["q_norm_g"][layer].partition_broadcast(128))
    sp.dma_start(out=gk[:], in_=W["k_norm_g"][layer].partition_broadcast(128))
    sp.dma_start(out=gb[:], in_=W["mlstm_gate_b"][layer].partition_broadcast(128))
    dve.tensor_scalar(out=gq[:], in0=gq[:], scalar1=0.125, scalar2=None, op0=ALU.mult)
    cos_t = kb.sb("A_cos", [128, NT, 32], F32)
    sin_t = kb.sb("A_sin", [128, NT, 32], F32)
    sp.dma_start(out=cos_t[:], in_=C_cos.rearrange("(j p) f -> p j f", p=128))
    sp.dma_start(out=sin_t[:], in_=C_sin.rearrange("(j p) f -> p j f", p=128))

    xb = [kb.sb("A_x%d" % i, [128, 4, D], F32) for i in range(2)]
    hn = [kb.sb("A_hn%d" % i, [128, D], BF16) for i in range(2)]
    junk = kb.sb("A_junk", [128, D], BF16)
    st = [kb.sb("A_st%d" % i, [128, 4], F32) for i in range(2)]
    hnT = [kb.sb("A_hnT%d" % i, [128, 8, 512], BF16) for i in range(2)]
    fst = [kb.sb("A_fst%d" % i, [128, 512], F32) for i in range(2)]
    vo = [kb.sb("A_vo%d" % i, [128, 512], BF16) for i in range(2)]
    g2 = [kb.sb("A_g2%d" % i, [128, 272], F32) for i in range(2)]
    gt = [kb.sb("A_gt%d" % i, [128, 16], F32) for i in range(2)]
    avs = [kb.sb("A_av%d" % i, [128, 128], BF16) for i in range(2)]
    qs = [kb.sb("A_qs%d" % i, [128, 512], F32) for i in range(2)]
    sq = kb.sb("A_sq", [128, 512], F32)
    ss8 = kb.sb("A_ss8", [128, 8], F32)
    tmp = [kb.sb("A_tmp%d" % i, [128, 256], F32) for i in range(4)]
    qr = [kb.sb("A_qr%d" % i, [128, 512], BF16) for i in range(2)]
    kr = [kb.sb("A_kr%d" % i, [128, 2, 2, 64], BF16) for i in range(2)]
    qTb = [kb.sb("A_qTb%d" % i, [128, 4, 512], BF16) for i in range(2)]
    kTb = [kb.sb("A_kTb%d" % i, [128, 2, 512], BF16) for i in range(2)]

    sqk = kb.sb("A_sqk", [128, 128], F32)
    ss8k = kb.sb("A_ss8k", [128, 2], F32)
    tmpk = [kb.sb("A_tmpk%d" % i, [128, 64], F32) for i in range(4)]

    def hnr_a(src, nh, sqb, ssb):
        n = nh * 64
        dve.tensor_tensor(out=sqb[:, 0:n], in0=src, in1=src, op=ALU.mult)
        dve.tensor_reduce(out=ssb[:, 0:nh], in_=sqb[:, 0:n].rearrange("p (h d) -> p h d", d=64), axis=AX.X, op=ALU.add)

    def hnr_s(nh, ssb):
        act.activation(out=ssb[:, 0:nh], in_=ssb[:, 0:nh], func=AF.Sqrt, scale=1.0 / 64, bias=EPS)

    def hnr_b(src, nh, g, j_tile, dst_views, sqb, ssb, tmps):
        n = nh * 64
        dve.reciprocal(out=ssb[:, 0:nh], in_=ssb[:, 0:nh])
        s3 = src.rearrange("p (h d) -> p h d", d=64)
        q3 = sqb[:, 0:n].rearrange("p (h d) -> p h d", d=64)
        dve.tensor_tensor(out=q3, in0=s3, in1=ssb[:, 0:nh].unsqueeze(2).to_broadcast([128, nh, 64]), op=ALU.mult)
        dve.tensor_tensor(out=q3, in0=q3, in1=g[:].unsqueeze(1).to_broadcast([128, nh, 64]), op=ALU.mult)
        q4 = sqb[:, 0:n].rearrange("p (h i two) -> p h i two", h=nh, two=2)
        x0 = q4[:, :, :, 0]
        x1 = q4[:, :, :, 1]
        cb = cos_t[:, j_tile, :].unsqueeze(1).to_broadcast([128, nh, 32])
        sb_ = sin_t[:, j_tile, :].unsqueeze(1).to_broadcast([128, nh, 32])
        m = nh * 32
        tv = [t[:, 0:m].rearrange("p (h i) -> p h i", h=nh) for t in tmps]
        dve.tensor_tensor(out=tv[0], in0=x0, in1=cb, op=ALU.mult)
        dve.tensor_tensor(out=tv[1], in0=x1, in1=sb_, op=ALU.mult)
        dve.tensor_tensor(out=tv[2], in0=x0, in1=sb_, op=ALU.mult)
        dve.tensor_tensor(out=tv[3], in0=x1, in1=cb, op=ALU.mult)
        for dv in dst_views:
            dve.tensor_tensor(out=dv[:, :, :, 0], in0=tv[0], in1=tv[1], op=ALU.subtract)
            dve.tensor_tensor(out=dv[:, :, :, 1], in0=tv[2], in1=tv[3], op=ALU.add)

    def prologue(blk):
        rows = slice(blk * 512, (blk + 1) * 512)
        X = xb[blk % 2]
        HT = hnT[blk % 2]
        sp.dma_start(out=X[:], in_=xs[rows, :].rearrange("(j p) d -> p j d", p=128))
        stt = st[blk % 2]
        for j in range(4):
            act.activation(out=junk[:], in_=X[:, j, :], func=AF.Square, accum_out=stt[:, j:j + 1])
        act.activation(out=stt[:], in_=stt[:], func=AF.Sqrt, scale=1.0 / D, bias=EPS)
        dve.reciprocal(out=stt[:], in_=stt[:])
        for j in range(4):
            H = hn[j % 2]
            dve.scalar_tensor_tensor(out=H[:], in0=X[:, j, :], scalar=stt[:, j:j + 1], in1=g_bc[:], op0=ALU.mult, op1=ALU.mult)
            pt = PS[j % 2]
            pv = bfview(pt)
            for c in range(8):
                pe.transpose(pv[:, c * 128:(c + 1) * 128], H[:, c * 128:(c + 1) * 128], identb[:])
            act.activation(out=HT[:, :, j * 128:(j + 1) * 128], in_=pv.rearrange("p (c t) -> p c t", c=8), func=AF.Copy)

    def mainA(blk):
        rows = slice(blk * 512, (blk + 1) * 512)
        HT = hnT[blk % 2]
        for ch in range(6):
            pt = PS[2 + ch % 2]
            for c in range(8):
                pe.matmul(pt[:], lhsT=w[:, c, ch * 128:(ch + 1) * 128], rhs=HT[:, c, :], start=(c == 0), stop=(c == 7))
            f = fst[ch % 2]
            act.activation(out=f[:], in_=pt[:], func=AF.Copy)
            sp.dma_start(out=featT[ch * 128:(ch + 1) * 128, rows], in_=f[:])
        QT = qTb[blk % 2]
        KT = kTb[blk % 2]

        def part1(j):
            jt = blk * 4 + j
            trow = slice(jt * 128, (jt + 1) * 128)
            p1, p2, p3 = PS[4], PS[5], PS[7]
            for c in range(8):
                pe.matmul(p1[:], lhsT=HT[:, c, j * 128:(j + 1) * 128], rhs=w[:, c, 768:1280], start=(c == 0), stop=(c == 7))
            for c in range(8):
                pe.matmul(p2[:, 0:272], lhsT=HT[:, c, j * 128:(j + 1) * 128], rhs=w[:, c, 1792:2064], start=(c == 0), stop=(c == 7))
            for c in range(8):
                pe.matmul(p3[:], lhsT=HT[:, c, j * 128:(j + 1) * 128], rhs=w[:, c, 1280:1792], start=(c == 0), stop=(c == 7))
            V = vo[j % 2]
            G2 = g2[j % 2]
            Q = qs[j % 2]
            GT = gt[j % 2]
            AV = avs[j % 2]
            KR = kr[j % 2]
            QR = qr[j % 2]
            act.activation(out=G2[:], in_=p2[:, 0:272], func=AF.Copy)
            act.activation(out=Q[:], in_=p3[:], func=AF.Copy)
            act.activation(out=V[:, 0:256], in_=p1[:, 0:256], func=AF.Copy)
            act.activation(out=V[:, 256:512], in_=p1[:, 256:512], func=AF.Sigmoid)
            sp.dma_start(out=mv_d[trow, :], in_=V[:, 0:256])
            sp.dma_start(out=mo_d[trow, :], in_=V[:, 256:512])
            dve.tensor_tensor(out=GT[:], in0=G2[:, 0:16], in1=gb[:], op=ALU.add)
            dve.tensor_copy(out=AV[:], in_=G2[:, 144:272])
            sp.dma_start(out=av_d[trow, :], in_=AV[:])
            hnr_a(G2[:, 16:144], 2, sqk, ss8k)
            hnr_a(Q[:], 8, sq, ss8)
            act.activation(out=GT[:, 8:16], in_=GT[:, 8:16], func=AF.Exp, scale=-1.0)
            act.activation(out=GT[:, 8:16], in_=GT[:, 8:16], func=AF.Ln, bias=1.0)
            hnr_s(2, ss8k)
            hnr_s(8, ss8)
            dve.tensor_scalar(out=GT[:, 8:16], in0=GT[:, 8:16], scalar1=-1.0, scalar2=None, op0=ALU.mult)
            sp.dma_start(out=gates_d[trow, :], in_=GT[:])
            hnr_b(G2[:, 16:144], 2, gk, jt,
                  [KR[:, :, 0, :].rearrange("p h (i two) -> p h i two", two=2),
                   KR[:, :, 1, :].rearrange("p h (i two) -> p h i two", two=2)], sqk, ss8k, tmpk)
            hnr_b(Q[:], 8, gq, jt, [QR[:].rearrange("p (h i two) -> p h i two", h=8, two=2)], sq, ss8, tmp)

        def part2(j):
            KR = kr[j % 2]
            QR = qr[j % 2]
            pv = bfview(PS[6])
            for kv in range(2):
                pe.transpose(pv[:, kv * 128:(kv + 1) * 128], KR[:, kv, :, :].rearrange("p a d -> p (a d)"), identb[:])
            for c in range(4):
                pe.transpose(pv[:, 256 + c * 128:256 + (c + 1) * 128], QR[:, c * 128:(c + 1) * 128], identb[:])
            act.activation(out=KT[:, :, j * 128:(j + 1) * 128], in_=pv[:, 0:256].rearrange("p (c t) -> p c t", c=2), func=AF.Copy)
            act.activation(out=QT[:, :, j * 128:(j + 1) * 128], in_=pv[:, 256:768].rearrange("p (c t) -> p c t", c=4), func=AF.Copy)
        for j in range(4):
            part1(j)
            if j >= 1:
                part2(j - 1)
        part2(3)
        sp.dma_start(out=qT_d[:, :, rows].rearrange("c p t -> p c t"), in_=QT[:])
        sp.dma_start(out=kT_d[:, :, rows].rearrange("c p t -> p c t"), in_=KT[:])

    prologue(0)
    for blk in range(8):
        if blk + 1 < 8:
            prologue(blk + 1)
        mainA(blk)
    kb.end_phase()


def phase_B(kb, nc, layer, W, PS, featT, yT_d, C_invc):
    pe, dve, act, pool, sp = kb.pe, kb.dve, kb.act, kb.pool, kb.sp
    kb.begin_phase()
    PADL = 16
    WID = S + 32
    U = kb.sb("B_U", [128, 2, WID], F32)
    dve.memset(U[:, :, 0:PADL], 0.0)
    dve.memset(U[:, :, PADL + S:WID], 0.0)
    for c in range(2):
        sp.dma_start(out=U[:, c, PADL:PADL + S], in_=featT[c * 128:(c + 1) * 128, :])
    invc = kb.sb("B_invc", [128, 2, S], F32)
    sp.dma_start(out=invc[:], in_=C_invc.rearrange("(c p) t -> p c t", p=128))
    BD = kb.sb("B_BD", [128, 2, 128], BF16)
    dve.memset(BD[:], 0.0)
    for c in range(2):
        pool.dma_start(out=BD[0:64, c, 0:64], in_=W["pool_w"][layer][2 * c])
        pool.dma_start(out=BD[64:128, c, 64:128], in_=W["pool_w"][layer][2 * c + 1])
    psc = kb.sb("B_psc", [128, 2], F32)
    for c in range(2):
        sp.dma_start(out=psc[:, c:c + 1], in_=W["pool_scale"][layer][c * 128:(c + 1) * 128].rearrange("(p o) -> p o", o=1))
    P2a = kb.sb("B_P2a", [128, WID], F32)
    P2b = kb.sb("B_P2b", [128, WID], F32)
    P4b = kb.sb("B_P4b", [128, WID], F32)
    P8b = kb.sb("B_P8b", [128, WID], F32)
    Ss = kb.sb("B_S", [128, 2, S], F32)
    dT = kb.sb("B_dT", [128, 2, S], BF16)

    def rng(t, lo, hi, sh=0):
        return slice(PADL + lo + sh, PADL + hi + sh)
    lo = -8
    u0, u1 = U[:, 0, :], U[:, 1, :]
    pool.tensor_tensor(out=Ss[0:64, 0, :], in0=U[0:64, 0, rng(0, 0, S, -1)], in1=U[0:64, 0, rng(0, 0, S)], op=ALU.add)
    dve.tensor_tensor(out=P2a[64:128, rng(0, lo, S + 12)], in0=U[64:128, 0, rng(0, lo, S + 12)], in1=U[64:128, 0, rng(0, lo, S + 12, 1)], op=ALU.add)
    dve.tensor_tensor(out=Ss[64:128, 0, :], in0=P2a[64:128, rng(0, 0, S, -2)], in1=P2a[64:128, rng(0, 0, S)], op=ALU.add)
    pool.tensor_tensor(out=P2b[:, rng(0, lo, S + 12)], in0=U[:, 1, rng(0, lo, S + 12)], in1=U[:, 1, rng(0, lo, S + 12, 1)], op=ALU.add)
    pool.tensor_tensor(out=P4b[:, rng(0, lo, S + 8)], in0=P2b[:, rng(0, lo, S + 8)], in1=P2b[:, rng(0, lo, S + 8, 2)], op=ALU.add)
    dve.tensor_tensor(out=Ss[0:64, 1, :], in0=P4b[0:64, rng(0, 0, S, -4)], in1=P4b[0:64, rng(0, 0, S)], op=ALU.add)
    pool.tensor_tensor(out=P8b[64:128, rng(0, lo, S + 4)], in0=P4b[64:128, rng(0, lo, S + 4)], in1=P4b[64:128, rng(0, lo, S + 4, 4)], op=ALU.add)
    dve.tensor_tensor(out=Ss[64:128, 1, :], in0=P8b[64:128, rng(0, 0, S, -8)], in1=P8b[64:128, rng(0, 0, S)], op=ALU.add)
    for c in range(2):
        dve.tensor_tensor(out=Ss[:, c, :], in0=Ss[:, c, :], in1=invc[:, c, :], op=ALU.mult)
        dve.tensor_tensor(out=dT[:, c, :], in0=Ss[:, c, :], in1=U[:, c, PADL:PADL + S], op=ALU.subtract)
    yst = [kb.sb("B_y%d" % i, [128, 512], BF16) for i in range(2)]
    k = 0
    for blk in range(8):
        for c in range(2):
            pt = PS[k % 2]
            pe.matmul(pt[:], lhsT=BD[:, c, :], rhs=dT[:, c, blk * 512:(blk + 1) * 512], start=True, stop=True)
            Y = yst[k % 2]
            k += 1
            act.activation(out=Y[:], in_=pt[:], func=AF.Copy, scale=psc[:, c:c + 1])
            sp.dma_start(out=yT_d[c * 128:(c + 1) * 128, blk * 512:(blk + 1) * 512], in_=Y[:])
    kb.end_phase()


def phase_C(kb, nc, layer, W, PS, identb, featT, mv_d, mo_d, gates_d, qk_d, yT_d, C_tri):
    pe, dve, act, pool, sp = kb.pe, kb.dve, kb.act, kb.pool, kb.sp
    kb.begin_phase()
    cw = kb.sb("C_cw", [128, 4, 5], F32)
    sp.dma_start(out=cw[:], in_=W["mlstm_conv_wT"][layer].rearrange("(c p) j -> p c j", p=128))
    Xp = [kb.sb("C_Xp%d" % i, [128, S + 4], F32) for i in range(2)]
    acc = [kb.sb("C_acc%d" % i, [128, S], F32) for i in range(2)]
    sgm = [kb.sb("C_sgm%d" % i, [128, S], F32) for i in range(2)]
    qko = [kb.sb("C_qko%d" % i, [128, S], BF16) for i in range(2)]
    for i in range(2):
        dve.memset(Xp[i][:, 0:2], 0.0)
        dve.memset(Xp[i][:, S + 2:S + 4], 0.0)
    def loadC0(ci):
        sp.dma_start(out=Xp[ci % 2][:, 2:S + 2], in_=featT[256 + ci * 128:256 + (ci + 1) * 128, :])
    loadC0(0)
    for ci in range(4):
        X = Xp[ci % 2]
        A_ = acc[ci % 2]
        G_ = sgm[ci % 2]
        O_ = qko[ci % 2]
        if ci + 1 < 4:
            loadC0(ci + 1)
        dve.tensor_scalar(out=A_[:], in0=X[:, 0:S], scalar1=cw[:, ci, 0:1], scalar2=None, op0=ALU.mult)
        for j in range(1, 5):
            dve.scalar_tensor_tensor(out=A_[:], in0=X[:, j:j + S], scalar=cw[:, ci, j:j + 1], in1=A_[:], op0=ALU.mult, op1=ALU.add)
        act.activation(out=G_[:], in_=A_[:], func=AF.Sigmoid)
        dve.scalar_tensor_tensor(out=O_[:], in0=A_[:], scalar=(1.0 if ci < 2 else 0.125), in1=G_[:], op0=ALU.mult, op1=ALU.mult)
        sp.dma_start(out=qk_d[ci], in_=O_[:])
    kb.end_phase()
    import os
    CSTOP = int(os.environ.get("CSTOP", "9"))
    if CSTOP <= 0:
        return
    kb.begin_phase()
    QK = kb.sb("C_QK", [128, 4, S], BF16)
    sp.dma_start(out=QK[:], in_=qk_d.rearrange("c p t -> p c t"))
    TRI = kb.sb("C_TRI", [128, 2, 128], F32)
    sp.dma_start(out=TRI[:], in_=C_tri.rearrange("p (a j) -> p a j", a=2))
    ones = kb.sb("C_ones", [128, 128], F32)
    dve.memset(ones[:], 1.0)
    G = kb.sb("C_G", [128, NT, 16], F32)
    sp.dma_start(out=G[:], in_=gates_d.rearrange("(c p) n -> p c n", p=128))
    ybf = kb.sb("C_ybf", [128, NT, 256], BF16)
    sp.dma_start(out=ybf[:], in_=mv_d.rearrange("(c p) n -> p c n", p=128))
    Va = kb.sb("C_Va", [128, NT, 4, 65], BF16)
    dve.memset(Va[:, :, :, 64:65], 1.0)
    dve.tensor_copy(out=Va[:, :, :, 0:64], in_=ybf[:].rearrange("p c (h d) -> p c h d", h=4))
    Kt = kb.sb("C_Kt", [128, NT, 256], BF16)
    for cg in range(8):
        pv = bfview(PS[cg % 2])
        for cl in range(4):
            c = cg * 4 + cl
            for hp in range(2):
                pe.transpose(pv[:, (cl * 2 + hp) * 128:(cl * 2 + hp + 1) * 128], QK[:, 2 + hp, c * 128:(c + 1) * 128], identb[:])
        act.activation(out=Kt[:, cg * 4:(cg + 1) * 4, :], in_=pv.rearrange("p (c n) -> p c n", c=4), func=AF.Copy)
    A = [kb.sb("C_A%d" % i, [128, NT, 4], F32) for i in range(2)]
    A2 = [kb.sb("C_A2%d" % i, [128, NT, 4], F32) for i in range(2)]
    Bc = [kb.sb("C_B%d" % i, [128, NT, 4], F32) for i in range(2)]
    Fc = [kb.sb("C_F%d" % i, [128, NT, 4], F32) for i in range(2)]
    at = kb.sb("C_at", [128, NT, 4], F32)
    lfc = kb.sb("C_lfc", [128, 2, NT * 4], F32)
    for d_ in range(2):
        dve.tensor_copy(out=lfc[:, d_, :].rearrange("p (c h) -> p c h", h=4), in_=G[:, :, 8 + 4 * d_:12 + 4 * d_])
    for d_ in range(2):
        lfv = lfc[:, d_, :]
        liv = G[:, :, 4 * d_:4 * d_ + 4]
        pc, ptot = PS[6], PS[7]
        pe.matmul(pc[:, 0:128], lhsT=TRI[:, d_, :], rhs=lfv, start=True, stop=True)
        pe.matmul(ptot[:, 0:128], lhsT=ones[:], rhs=lfv, start=True, stop=True)
        pc3 = pc[:, 0:128].rearrange("p (c h) -> p c h", h=4)
        pt3 = ptot[:, 0:128].rearrange("p (c h) -> p c h", h=4)
        dve.tensor_tensor(out=at[:], in0=liv, in1=pc3, op=ALU.subtract)
        act.activation(out=A[d_][:], in_=at[:], func=AF.Exp)
        dve.tensor_tensor(out=at[:], in0=at[:], in1=pt3, op=ALU.add)
        act.activation(out=A2[d_][:], in_=at[:], func=AF.Exp)
        act.activation(out=Bc[d_][:], in_=pc3, func=AF.Exp)
        act.activation(out=Fc[d_][:], in_=pt3, func=AF.Exp)
    if CSTOP <= 1:
        kb.end_phase()
        return
    Hd = [kb.sb("C_H%d" % i, [128, NT, 256], F32) for i in range(2)]
    Zf = [kb.sb("C_Zf%d" % i, [128, 4, 65], F32) for i in range(2)]
    Zb = [[kb.sb("C_Zb%d_%d" % (i, j), [128, 4, 65], BF16) for j in range(2)] for i in range(2)]
    for d_ in range(2):
        dve.memset(Zf[d_][:], 0.0)
        dve.memset(Zb[d_][0][:], 0.0)
    VWr = [kb.sb("C_VW%d" % i, [128, 2, 4, 65], BF16) for i in range(4)]
    SMr = [kb.sb("C_SM%d" % i, [128, 2, 2, 128], BF16) for i in range(4)]
    t4r = [kb.sb("C_t4%d" % i, [128, 4], F32) for i in range(4)]
    for it in range(NT):
        cc = [it, NT - 1 - it]
        VWs = [VWr[(2 * it + d_) % 4] for d_ in range(2)]
        SMs = [SMr[(2 * it + d_) % 4] for d_ in range(2)]
        t4s = [t4r[(2 * it + d_) % 4] for d_ in range(2)]
        pss = [[PS[0], PS[1]], [PS[2], PS[3]]]
        psn = [PS[4], PS[5]]
        psu = [PS[6], PS[7]]
        for d_ in range(2):
            c = cc[d_]
            VW = VWs[d_]
            dve.tensor_tensor(out=VW[:, 0], in0=Va[:, c], in1=A[d_][:, c, :].unsqueeze(2).to_broadcast([128, 4, 65]), op=ALU.mult)
            dve.tensor_tensor(out=VW[:, 1], in0=Va[:, c], in1=A2[d_][:, c, :].unsqueeze(2).to_broadcast([128, 4, 65]), op=ALU.mult)
        for d_ in range(2):
            c = cc[d_]
            cs = slice(c * 128, (c + 1) * 128)
            for h in range(4):
                pb = (h % 2) * 64
                pe.matmul(pss[d_][h % 2][:, (h // 2) * 128:(h // 2 + 1) * 128], lhsT=QK[pb:pb + 64, 2 + h // 2, cs], rhs=QK[pb:pb + 64, h // 2, cs], start=True, stop=True)
        for d_ in range(2):
            for hh in range(2):
                dve.tensor_tensor(out=SMs[d_][:, hh], in0=pss[d_][hh][:, 0:256].rearrange("p (h j) -> p h j", h=2),
                                  in1=TRI[:, d_, :].unsqueeze(1).to_broadcast([128, 2, 128]), op=ALU.mult)
        for d_ in range(2):
            c = cc[d_]
            cs = slice(c * 128, (c + 1) * 128)
            Zc = Zb[d_][it % 2]
            for h in range(4):
                pe.matmul(psn[d_][:, h * 65:(h + 1) * 65], lhsT=SMs[d_][:, h % 2, h // 2, :], rhs=VWs[d_][:, 0, h, :], start=True, stop=False)
                pe.matmul(psn[d_][:, h * 65:(h + 1) * 65], lhsT=QK[:, h // 2, cs], rhs=Zc[:, h, :], start=False, stop=True)
            for hp in range(2):
                pe.matmul(psu[d_][:, hp * 130:(hp + 1) * 130], lhsT=Kt[:, c, hp * 128:(hp + 1) * 128],
                          rhs=VWs[d_][:, 1, 2 * hp:2 * hp + 2, :].rearrange("p a b -> p (a b)"), start=True, stop=True)
        for d_ in range(2):
            c = cc[d_]
            for hp in range(2):
                for hh in range(2):
                    rows = slice(hh * 64, (hh + 1) * 64)
                    h = 2 * hp + hh
                    dve.scalar_tensor_tensor(out=Zf[d_][rows, h, :], in0=Zf[d_][rows, h, :], scalar=Fc[d_][rows, c, h:h + 1],
                                             in1=psu[d_][rows, hp * 130 + hh * 65:hp * 130 + (hh + 1) * 65], op0=ALU.mult, op1=ALU.add)
            act.activation(out=Zb[d_][(it + 1) % 2][:], in_=Zf[d_][:], func=AF.Copy)
        n3s = [psn[d_][:, 0:260].rearrange("p (h e) -> p h e", h=4) for d_ in range(2)]
        for d_ in range(2):
            dve.tensor_tensor(out=t4s[d_][:], in0=n3s[d_][:, :, 64], in1=Bc[d_][:, cc[d_], :], op=ALU.mult)
        for d_ in range(2):
            act.activation(out=t4s[d_][:], in_=t4s[d_][:], func=AF.Abs)
        for d_ in range(2):
            dve.tensor_scalar(out=t4s[d_][:], in0=t4s[d_][:], scalar1=1.0, scalar2=None, op0=ALU.max)
            dve.reciprocal(out=t4s[d_][:], in_=t4s[d_][:])
            dve.tensor_tensor(out=t4s[d_][:], in0=t4s[d_][:], in1=Bc[d_][:, cc[d_], :], op=ALU.mult)
            dve.tensor_tensor(out=Hd[d_][:, cc[d_], :].rearrange("p (h e) -> p h e", h=4), in0=n3s[d_][:, :, 0:64],
                              in1=t4s[d_][:].unsqueeze(2).to_broadcast([128, 4, 64]), op=ALU.mult)
    if CSTOP <= 2:
        kb.end_phase()
        return
    H0, H1 = Hd
    ng = kb.sb("C_ng", [128, 256], F32)
    sp.dma_start(out=ng[:], in_=W["mlstm_norm_g"][layer].partition_broadcast(128))
    mo = kb.sb("C_mo", [128, NT, 256], BF16)
    sp.dma_start(out=mo[:], in_=mo_d.rearrange("(c p) n -> p c n", p=128))
    ss = kb.sb("C_ss", [128, NT, 4], F32)
    for half in range(2):
        cs = slice(half * 16, (half + 1) * 16)
        dve.tensor_tensor(out=H0[:, cs, :], in0=H0[:, cs, :], in1=H1[:, cs, :], op=ALU.add)
        pool.tensor_tensor(out=H1[:, cs, :], in0=H0[:, cs, :], in1=H0[:, cs, :], op=ALU.mult)
        dve.tensor_reduce(out=ss[:, cs, :], in_=H1[:, cs, :].rearrange("p c (h e) -> p c h e", h=4), axis=AX.X, op=ALU.add)
    act.activation(out=ss[:], in_=ss[:], func=AF.Sqrt, scale=1.0 / 64, bias=EPS)
    dve.reciprocal(out=ss[:], in_=ss[:])
    for half in range(2):
        cs = slice(half * 16, (half + 1) * 16)
        dve.tensor_tensor(out=H0[:, cs, :].rearrange("p c (h e) -> p c h e", h=4), in0=H0[:, cs, :].rearrange("p c (h e) -> p c h e", h=4),
                          in1=ss[:, cs, :].unsqueeze(3).to_broadcast([128, 16, 4, 64]), op=ALU.mult)
        pool.tensor_tensor(out=H0[:, cs, :], in0=H0[:, cs, :], in1=ng[:].unsqueeze(1).to_broadcast([128, 16, 256]), op=ALU.mult)
        dve.tensor_tensor(out=ybf[:, cs, :], in0=H0[:, cs, :], in1=mo[:, cs, :], op=ALU.mult)
    yst = [kb.sb("C_yst%d" % i, [128, 1024], BF16) for i in range(2)]
    k = 0
    for hp in range(2):
        for cg in range(4):
            pv = bfview(PS[k % 2])
            Y = yst[k % 2]
            k += 1
            for cl in range(8):
                c = cg * 8 + cl
                pe.transpose(pv[:, cl * 128:(cl + 1) * 128], ybf[:, c, hp * 128:(hp + 1) * 128], identb[:])
            act.activation(out=Y[:], in_=pv, func=AF.Copy)
            sp.dma_start(out=yT_d[256 + hp * 128:256 + (hp + 1) * 128, cg * 1024:(cg + 1) * 1024], in_=Y[:])
    kb.end_phase()


def phase_D(kb, nc, layer, PS, qT_d, kT_d, av_d, yT_d):
    pe, dve, act, pool, sp = kb.pe, kb.dve, kb.act, kb.pool, kb.sp
    kb.begin_phase()
    kT2 = kb.sb("D_kT", [128, 2, S], BF16)
    sp.dma_start(out=kT2[:], in_=kT_d.rearrange("c p t -> p c t"))
    Va = kb.sb("D_Va", [128, NT, 2, 128], BF16)
    dve.memset(Va[:, :, :, 64:128], 1.0)
    Vst = kb.sb("D_Vst", [128, NT, 128], BF16)
    sp.dma_start(out=Vst[:], in_=av_d.rearrange("(c p) n -> p c n", p=128))
    dve.tensor_copy(out=Va[:, :, :, 0:64], in_=Vst[:].rearrange("p c (h d) -> p c h d", h=2))
    Qe = [kb.sb("D_Qe%d" % i, [128, S], BF16) for i in range(2)]
    Qo = [kb.sb("D_Qo%d" % i, [128, S], BF16) for i in range(2)]
    for i in range(2):
        pool.memset(Qe[i][64:128, :], 0.0)
        pool.memset(Qo[i][0:64, :], 0.0)

    def load_q(pr):
        sp.dma_start(out=Qe[pr % 2][0:64, :], in_=qT_d[pr, 0:64, :])
        sp.dma_start(out=Qo[pr % 2][64:128, :], in_=qT_d[pr, 64:128, :])
    P = [kb.sb("D_P%d" % i, [128, 512], BF16) for i in range(4)]
    rd = [kb.sb("D_rd%d" % i, [64, 512], F32) for i in range(2)]
    yo = [kb.sb("D_yo%d" % i, [64, 512], BF16) for i in range(2)]
    SB = PS[0:4]
    OB = PS[4:6]
    steps = []
    for pr in range(4):
        for hh in range(2):
            for qb in range(8):
                for kc in range(NT):
                    steps.append((pr, hh, qb, kc))
    load_q(0)

    def issue_S(i):
        pr, hh, qb, kc = steps[i]
        kv = pr // 2
        if hh == 0 and qb == 0 and kc == 0 and pr + 1 < 4:
            load_q(pr + 1)
        Qt = (Qe if hh == 0 else Qo)[pr % 2]
        pe.matmul(SB[i % 4][:], lhsT=kT2[:, kv, kc * 128:(kc + 1) * 128],
                  rhs=Qt[:, qb * 512:(qb + 1) * 512], start=True, stop=True)
    LOOK = 2
    for i in range(min(LOOK, len(steps))):
        issue_S(i)
    ob_i = 0
    for i, (pr, hh, qb, kc) in enumerate(steps):
        if i + LOOK < len(steps):
            issue_S(i + LOOK)
        kv = pr // 2
        Pt = P[i % 4]
        act.activation(out=Pt[:], in_=SB[i % 4][:], func=AF.Exp)
        O = OB[ob_i % 2]
        pe.matmul(O[:], lhsT=Va[:, kc, kv, :], rhs=Pt[:], start=(kc == 0), stop=(kc == NT - 1))
        if kc == NT - 1:
            h = 2 * pr + hh
            R = rd[ob_i % 2]
            Y = yo[ob_i % 2]
            dve.reciprocal(out=R[:], in_=O[64:128, :])
            dve.tensor_tensor(out=Y[:], in0=O[0:64, :], in1=R[:], op=ALU.mult)
            sp.dma_start(out=yT_d[512 + h * 64:512 + (h + 1) * 64, qb * 512:(qb + 1) * 512], in_=Y[:])
            ob_i += 1
    kb.end_phase()


def phase_EF(kb, nc, layer, W, PS, identb, xsrc, xs, yT_d, mem_in, efw, g1args=None):
    pe, dve, act, pool, sp = kb.pe, kb.dve, kb.act, kb.pool, kb.sp
    kb.begin_phase()
    wo, wq, wkv, cwo = efw["wo"], efw["wq"], efw["wkv"], efw["cwo"]
    g1_block = g1_finish = None
    if g1args is not None:
        g1_block, g1_finish = make_g1(kb, nc, layer, W, PS, g1args["identf"], g1args["hn_d"], g1args["aff_d"])
    g_bc = kb.sb("E_g", [128, D], F32)
    sp.dma_start(out=g_bc[:], in_=W["norm_mem_g"][layer].partition_broadcast(128))
    ones = kb.sb("E_ones", [128, 128], BF16)
    dve.memset(ones[:], 1.0)
    memb = kb.sb("E_memb", [128, 2, D], BF16)
    for mc in range(2):
        pool.dma_start(out=memb[:, mc, :], in_=mem_in[mc * 128:(mc + 1) * 128, :])
    memT = kb.sb("E_memT", [128, 8, 256], BF16)
    for mc in range(2):
        pv = bfview(PS[mc])
        for c in range(8):
            pe.transpose(pv[:, c * 128:(c + 1) * 128], memb[:, mc, c * 128:(c + 1) * 128], identb[:])
        act.activation(out=memT[:, :, mc * 128:(mc + 1) * 128], in_=pv.rearrange("p (c t) -> p c t", c=8), func=AF.Copy)
    kcT = kb.sb("E_kcT", [128, 4, 256], BF16)
    for h in range(4):
        pt = PS[2 + h % 2]
        for c in range(8):
            pe.matmul(pt[:, 0:256], lhsT=wkv[:, c, h * 128:(h + 1) * 128], rhs=memT[:, c, :], start=(c == 0), stop=(c == 7))
        act.activation(out=kcT[:, h, :], in_=pt[:, 0:256], func=AF.Copy)
    Vc = kb.sb("E_Vc", [128, 2, 512], BF16)
    for mc in range(2):
        pt = PS[4 + mc]
        for c in range(8):
            pe.matmul(pt[:], lhsT=memT[:, c, mc * 128:(mc + 1) * 128], rhs=wkv[:, c, 512:1024], start=(c == 0), stop=(c == 7))
        act.activation(out=Vc[:, mc, :], in_=pt[:], func=AF.Copy)

    xb = [kb.sb("E_x%d" % i, [128, 4, D], F32) for i in range(2)]
    Yb = [kb.sb("E_Y%d" % i, [128, 8, 512], BF16) for i in range(2)]
    hn = [kb.sb("E_hn%d" % i, [128, D], BF16) for i in range(2)]
    junk = kb.sb("E_junk", [128, D], BF16)
    st = [kb.sb("E_st%d" % i, [128, 4], F32) for i in range(2)]
    hnT = [kb.sb("E_hnT%d" % i, [128, 8, 512], BF16) for i in range(2)]
    qcT = [kb.sb("E_qcT%d" % i, [128, 4, 512], BF16) for i in range(2)]
    PT = [kb.sb("E_PT%d" % i, [128, 2, 512], BF16) for i in range(2)]
    rden = [kb.sb("E_rden%d" % i, [128, 512], F32) for i in range(2)]
    oT = [kb.sb("E_oT%d" % i, [128, 4, 512], BF16) for i in range(2)]
    SC = float(128 ** -0.5)
    def loadE(blk):
        rows_ = slice(blk * 512, (blk + 1) * 512)
        sp.dma_start(out=xb[blk % 2][:], in_=xsrc[rows_, :].rearrange("(j p) d -> p j d", p=128))
        sp.dma_start(out=Yb[blk % 2][:], in_=yT_d[:, rows_].rearrange("(c p) t -> p c t", p=128))

    def stage1(blk):
        X = xb[blk % 2]
        Y = Yb[blk % 2]
        HT = hnT[blk % 2]
        stt = st[blk % 2]
        for j in range(4):
            for half in range(2):
                pt = PS[half]
                for c in range(8):
                    pe.matmul(pt[:], lhsT=Y[:, c, j * 128:(j + 1) * 128], rhs=wo[:, c, half * 512:(half + 1) * 512], start=(c == 0), stop=(c == 7))
                dve.tensor_tensor(out=X[:, j, half * 512:(half + 1) * 512], in0=X[:, j, half * 512:(half + 1) * 512], in1=pt[:], op=ALU.add)
            act.activation(out=junk[:], in_=X[:, j, :], func=AF.Square, accum_out=stt[:, j:j + 1])
        act.activation(out=stt[:], in_=stt[:], func=AF.Sqrt, scale=1.0 / D, bias=EPS)
        dve.reciprocal(out=stt[:], in_=stt[:])
        for j in range(4):
            H = hn[j % 2]
            dve.scalar_tensor_tensor(out=H[:], in0=X[:, j, :], scalar=stt[:, j:j + 1], in1=g_bc[:], op0=ALU.mult, op1=ALU.mult)
            pt = PS[2 + j % 2]
            pv = bfview(pt)
            for c in range(8):
                pe.transpose(pv[:, c * 128:(c + 1) * 128], H[:, c * 128:(c + 1) * 128], identb[:])
            act.activation(out=HT[:, :, j * 128:(j + 1) * 128], in_=pv.rearrange("p (c t) -> p c t", c=8), func=AF.Copy)

    def stage2(blk):
        rows = slice(blk * 512, (blk + 1) * 512)
        X = xb[blk % 2]
        HT = hnT[blk % 2]
        QC = qcT[blk % 2]
        for h in range(4):
            pt = PS[4 + h % 2]
            for c in range(8):
                pe.matmul(pt[:], lhsT=wq[:, c, h * 128:(h + 1) * 128], rhs=HT[:, c, :], start=(c == 0), stop=(c == 7))
            act.activation(out=QC[:, h, :], in_=pt[:], func=AF.Copy)
        OT = oT[blk % 2]
        for h in range(4):
            Pt = PT[h % 2]
            for mc in range(2):
                pt = PS[6 + mc]
                pe.matmul(pt[:], lhsT=kcT[:, h, mc * 128:(mc + 1) * 128], rhs=QC[:, h, :], start=True, stop=True)
                act.activation(out=Pt[:, mc, :], in_=pt[:], func=AF.Exp, scale=SC)
            po = PS[4]
            pd = PS[5]
            for mc in range(2):
                pe.matmul(po[:], lhsT=Vc[:, mc, h * 128:(h + 1) * 128], rhs=Pt[:, mc, :], start=(mc == 0), stop=(mc == 1))
            for mc in range(2):
                pe.matmul(pd[:], lhsT=ones[:], rhs=Pt[:, mc, :], start=(mc == 0), stop=(mc == 1))
            R = rden[h % 2]
            dve.reciprocal(out=R[:], in_=pd[:])
            dve.tensor_tensor(out=OT[:, h, :], in0=po[:], in1=R[:], op=ALU.mult)
        for j in range(4):
            for half in range(2):
                pt = PS[6 + half]
                for c in range(4):
                    pe.matmul(pt[:], lhsT=OT[:, c, j * 128:(j + 1) * 128], rhs=cwo[:, c, half * 512:(half + 1) * 512], start=(c == 0), stop=(c == 3))
                dve.tensor_tensor(out=X[:, j, half * 512:(half + 1) * 512], in0=X[:, j, half * 512:(half + 1) * 512], in1=pt[:], op=ALU.add)
        sp.dma_start(out=xs[rows, :].rearrange("(j p) d -> p j d", p=128), in_=X[:])
        if g1_block is not None:
            g1_block(X, blk)
    loadE(0)
    loadE(1)
    stage1(0)
    for blk in range(8):
        if blk + 1 < 8:
            stage1(blk + 1)
        stage2(blk)
        if blk + 2 < 8:
            loadE(blk + 2)
    if g1_finish is not None:
        g1_finish()
    kb.end_phase()


def make_g1(kb, nc, layer, W, PS, identf, hn_d, aff_d):
    pe, dve, act, pool, sp = kb.pe, kb.dve, kb.act, kb.pool, kb.sp
    g_bc = kb.sb("G_g", [128, D], F32)
    sp.dma_start(out=g_bc[:], in_=W["norm_ffn_g"][layer].partition_broadcast(128))
    rw = kb.sb("G_rw", [128, 8, NEXP], F32)
    sp.dma_start(out=rw[:], in_=W["router_w"][layer].rearrange("(c p) e -> p c e", p=128))
    Hf = [kb.sb("G_Hf%d" % i, [128, D], F32) for i in range(2)]
    Hb = [kb.sb("G_Hb%d" % i, [128, D], BF16) for i in range(2)]
    HT = [kb.sb("G_HT%d" % i, [128, 8, 128], F32) for i in range(2)]
    junk = kb.sb("G_junk", [128, D], BF16)
    st = [kb.sb("G_st%d" % i, [128, 4], F32) for i in range(2)]
    LG = kb.sb("G_LG", [128, NT, NEXP], F32)
    mx = kb.sb("G_mx", [128, NT], F32)
    wst = [kb.sb("G_wst%d" % i, [NEXP, 512], F32) for i in range(2)]

    def block_fn(X, blk):
        stt = st[blk % 2]
        for j in range(4):
            act.activation(out=junk[:], in_=X[:, j, :], func=AF.Square, accum_out=stt[:, j:j + 1])
        act.activation(out=stt[:], in_=stt[:], func=AF.Sqrt, scale=1.0 / D, bias=EPS)
        dve.reciprocal(out=stt[:], in_=stt[:])
        for j in range(4):
            jt = blk * 4 + j
            H = Hf[j % 2]
            dve.scalar_tensor_tensor(out=H[:], in0=X[:, j, :], scalar=stt[:, j:j + 1], in1=g_bc[:], op0=ALU.mult, op1=ALU.mult)
            B_ = Hb[j % 2]
            act.activation(out=B_[:], in_=H[:], func=AF.Copy)
            sp.dma_start(out=hn_d[jt * 128:(jt + 1) * 128, :], in_=B_[:])
            T = HT[j % 2]
            for half in range(2):
                pt = PS[half]
                for c in range(4):
                    cc = half * 4 + c
                    pe.transpose(pt[:, c * 128:(c + 1) * 128], H[:, cc * 128:(cc + 1) * 128], identf[:])
                act.activation(out=T[:, half * 4:(half + 1) * 4, :], in_=pt[:].rearrange("p (c t) -> p c t", c=4), func=AF.Copy)
            pl = PS[2 + j % 2]
            for c in range(8):
                pe.matmul(pl[:, 0:NEXP], lhsT=T[:, c, :], rhs=rw[:, c, :], start=(c == 0), stop=(c == 7))
            dve.tensor_copy(out=LG[:, jt, :], in_=pl[:, 0:NEXP])

    def finish_fn():
        dve.tensor_reduce(out=mx[:], in_=LG[:], axis=AX.X, op=ALU.max)
        dve.tensor_tensor(out=LG[:], in0=LG[:], in1=mx[:].unsqueeze(2).to_broadcast([128, NT, NEXP]), op=ALU.subtract)
        act.activation(out=LG[:], in_=LG[:], func=AF.Exp)
        dve.tensor_reduce(out=mx[:], in_=LG[:], axis=AX.X, op=ALU.add)
        dve.reciprocal(out=mx[:], in_=mx[:])
        dve.tensor_tensor(out=LG[:], in0=LG[:], in1=mx[:].unsqueeze(2).to_broadcast([128, NT, NEXP]), op=ALU.mult)
        for blk in range(8):
            pt = PS[4 + blk % 2]
            for j in range(4):
                jt = blk * 4 + j
                pe.transpose(pt[0:NEXP, j * 128:(j + 1) * 128], LG[:, jt, :], identf[:])
            wt = wst[blk % 2]
            act.activation(out=wt[:], in_=pt[0:NEXP, :], func=AF.Copy)
            sp.dma_start(out=aff_d[:, blk * 512:(blk + 1) * 512], in_=wt[:])
    return block_fn, finish_fn


def phase_G(kb, nc, layer, W, PS, identb, identf, xs, hn_d, idxT, gT, aff_d):
    pe, dve, act, pool, sp = kb.pe, kb.dve, kb.act, kb.pool, kb.sp
    kb.begin_phase()
    ws = [kb.sb("G_w%d" % i, [128, 8, 1024], BF16) for i in range(6)]
    wgu = W["expert_w_gu"][layer]
    wdn = W["expert_w_down"][layer]

    def load_gu(e, p):
        half, g = p // 2, p % 2
        pool.dma_start(out=ws[p][:], in_=wgu[e][:, g * DFF + half * 1024:g * DFF + (half + 1) * 1024].rearrange("(c p) f -> p c f", p=128))

    def load_wd(e, p):
        pool.dma_start(out=ws[4 + p][:], in_=wdn[e][p * 1024:(p + 1) * 1024, :].rearrange("(c p) n -> p c n", p=128))
    for p in range(4):
        load_gu(0, p)
    for p in range(2):
        load_wd(0, p)
    work = kb.sb("G_work", [NEXP, S], F32)
    sp.dma_start(out=work[:], in_=aff_d[:, :])
    top = kb.sb("G_top", [NEXP, CAP], F32)
    idx = kb.sb("G_idx", [NEXP, CAP], U32)
    for r in range(CAP // 8):
        sl = slice(r * 8, (r + 1) * 8)
        dve.max(out=top[:, sl], in_=work[:])
        dve.max_index(out=idx[:, sl], in_max=top[:, sl], in_values=work[:])
        dve.match_replace(out=work[:], in_to_replace=top[:, sl], in_values=work[:], imm_value=-1.0)
    idxf = kb.sb("G_idxf", [NEXP, CAP], F32)
    dve.tensor_copy(out=idxf[:], in_=idx[:])
    pt = PS[6]
    for ct in range(4):
        pe.transpose(pt[:, ct * NEXP:(ct + 1) * NEXP], idxf[:, ct * 128:(ct + 1) * 128], identf[0:NEXP, 0:NEXP])
    dve.tensor_copy(out=idxT[:], in_=pt[:, 0:4 * NEXP].rearrange("p (c e) -> p c e", c=4))
    pt = PS[7]
    for ct in range(4):
        pe.transpose(pt[:, ct * NEXP:(ct + 1) * NEXP], top[:, ct * 128:(ct + 1) * 128], identf[0:NEXP, 0:NEXP])
    dve.tensor_copy(out=gT[:], in_=pt[:, 0:4 * NEXP].rearrange("p (c e) -> p c e", c=4))
    if DBG.get("idx") is not None:
        sp.dma_start(out=DBG["idx"][:, :], in_=idxT[:].rearrange("p c e -> p (c e)"))
        sp.dma_start(out=DBG["g"][:, :], in_=gT[:].rearrange("p c e -> p (c e)"))
    Xe = [kb.sb("G_Xe%d" % i, [128, 4, D], BF16) for i in range(2)]
    XeT = [kb.sb("G_XeT%d" % i, [128, 8, CAP], BF16) for i in range(2)]
    hT = kb.sb("G_hT", [128, 16, CAP], BF16)
    sg = [kb.sb("G_sg%d" % i, [128, CAP], F32) for i in range(2)]
    yg = [kb.sb("G_yg%d" % i, [128, D], F32) for i in range(2)]
    xs_tok = kb.token("xs_tok")
    def gather(e):
        X = Xe[e % 2]
        for ct in range(4):
            pool.indirect_dma_start(out=X[:, ct, :], out_offset=None, in_=hn_d[:, :],
                                    in_offset=bass.IndirectOffsetOnAxis(ap=idxT[:, ct, e:e + 1].ap, axis=0),
                                    _reads=[idxT])
        return X
    yi = 0
    Xn = gather(0)
    for e in range(NEXP):
        X = Xn
        if e + 1 < NEXP:
            Xn = gather(e + 1)
        XT = XeT[e % 2]
        for ct in range(4):
            pv = bfview(PS[ct % 2])
            for c in range(8):
                pe.transpose(pv[:, c * 128:(c + 1) * 128], X[:, ct, c * 128:(c + 1) * 128], identb[:])
            act.activation(out=XT[:, :, ct * 128:(ct + 1) * 128], in_=pv.rearrange("p (c t) -> p c t", c=8), func=AF.Copy)
        for fj in range(16):
            half, fl = fj // 8, fj % 8
            wga, wup = ws[2 * half], ws[2 * half + 1]
            pa = PS[2 + (fj % 2) * 2]
            pb_ = PS[3 + (fj % 2) * 2]
            for c in range(8):
                pe.matmul(pa[:], lhsT=wga[:, c, fl * 128:(fl + 1) * 128], rhs=XT[:, c, :], start=(c == 0), stop=(c == 7))
            for c in range(8):
                pe.matmul(pb_[:], lhsT=wup[:, c, fl * 128:(fl + 1) * 128], rhs=XT[:, c, :], start=(c == 0), stop=(c == 7))
            sgt = sg[fj % 2]
            act.activation(out=sgt[:], in_=pa[:], func=AF.Sigmoid)
            dve.tensor_tensor(out=sgt[:], in0=sgt[:], in1=pa[:], op=ALU.mult)
            dve.tensor_tensor(out=hT[:, fj, :], in0=sgt[:], in1=pb_[:], op=ALU.mult)
            if fl == 7 and e + 1 < NEXP:
                load_gu(e + 1, 2 * half)
                load_gu(e + 1, 2 * half + 1)
        if e == 0 and DBG.get("X") is not None:
            sp.dma_start(out=DBG["X"][:, :], in_=X[:].rearrange("p c d -> p (c d)"))
            sp.dma_start(out=DBG["hT"][:, :], in_=hT[:].rearrange("p c d -> p (c d)"))
        for ct in range(4):
            Y = yg[yi % 2]
            yi += 1
            for half in range(2):
                pt = PS[6 + half]
                for fj in range(16):
                    pe.matmul(pt[:], lhsT=hT[:, fj, ct * 128:(ct + 1) * 128], rhs=ws[4 + fj // 8][:, fj % 8, half * 512:(half + 1) * 512],
                              start=(fj == 0), stop=(fj == 15))
                dve.tensor_scalar(out=Y[:, half * 512:(half + 1) * 512], in0=pt[:], scalar1=gT[:, ct, e:e + 1], scalar2=None, op0=ALU.mult)
            if e == 0 and ct == 0 and DBG.get("yg") is not None:
                sp.dma_start(out=DBG["yg"][:, :], in_=Y[:])
            pool.indirect_dma_start(out=xs[:, :], out_offset=bass.IndirectOffsetOnAxis(ap=idxT[:, ct, e:e + 1].ap, axis=0),
                                    in_=Y[:], in_offset=None, compute_op=ALU.add,
                                    _reads=[idxT], _writes=[xs_tok])
        if e + 1 < NEXP:
            load_wd(e + 1, 0)
            load_wd(e + 1, 1)
    kb.end_phase()


def rope_tables():
    t = np.arange(S)
    row = (t // 64).astype(np.float32)
    col = (t % 64).astype(np.float32)
    inv = (10000.0 ** (-(np.arange(16, dtype=np.float32)) / 16)).astype(np.float32)
    ang = np.concatenate([row[:, None] * inv, col[:, None] * inv], axis=-1).astype(np.float32)
    return np.cos(ang).astype(np.float32), np.sin(ang).astype(np.float32)


def pool_invcount():
    t = np.arange(S)
    tab = np.zeros((256, S), np.float32)
    for g, win in enumerate((2, 4, 8, 16)):
        lo = np.clip(t - win // 2, 0, S)
        hi = np.clip(t + win // 2, 0, S)
        tab[g * 64:(g + 1) * 64, :] = (1.0 / (hi - lo).astype(np.float32))[None, :]
    return tab


def consts():
    c, s = rope_tables()
    tri = np.zeros((128, 2, 128), np.float32)
    si = np.arange(128)[:, None]
    ji = np.arange(128)[None, :]
    tri[:, 0, :] = (si <= ji)
    tri[:, 1, :] = (si >= ji)
    return {"c_cos": c, "c_sin": s, "c_identf": np.eye(128, dtype=np.float32), "c_invc": pool_invcount(),
            "c_tri": tri.reshape(128, 256)}


_NC_CACHE = {}


def kernel(**inputs):
    if "nc" not in _NC_CACHE:
        _NC_CACHE["nc"] = build()
    nc = _NC_CACHE["nc"]
    cst = consts()
    shared = {k: np.ascontiguousarray(np.asarray(v, dtype=np.float32)) for k, v in inputs.items()
              if k not in ("x", "mem", "mlstm_conv_w")}
    shared["mlstm_conv_wT"] = np.ascontiguousarray(np.asarray(inputs["mlstm_conv_w"], dtype=np.float32).transpose(0, 2, 1))
    shared.update(cst)
    x = np.asarray(inputs["x"], dtype=np.float32)
    mem = np.asarray(inputs["mem"], dtype=np.float32)
    in_maps = []
    for b in range(8):
        m = dict(shared)
        m["x"] = np.ascontiguousarray(x[b])
        m["mem"] = np.ascontiguousarray(mem[b])
        in_maps.append(m)
    res = run_bass_kernel_spmd(nc, in_maps, core_ids=list(range(8)))
    return np.stack([r["out"] for r in res.results], axis=0).astype(np.float32)
```

```python
import numpy as np
from contextlib import ExitStack
import concourse.bass as bass
import concourse.mybir as mybir
from concourse.bass_utils import run_bass_kernel_spmd

F32 = mybir.dt.float32
BF16 = mybir.dt.bfloat16
U32 = mybir.dt.uint32
I32 = mybir.dt.int32
AF = mybir.ActivationFunctionType
ALU = mybir.AluOpType
AX = mybir.AxisListType

S = 4096
D = 1024
NT = S // 128
DEPTH = 4
INC = 2064
EPS = 1e-6
NEXP = 16
CAP = 512
DFF = 2048


class Tile:
    def __init__(self, kb, t, name):
        self.kb = kb
        self.t = t
        self.name = name
        self.last_write = None
        self.readers = {}
        self.dsem = None

    def __getitem__(self, key):
        return TAP(self.t[key], self)

    def ap(self):
        return TAP(self.t[:], self)


class TAP:
    def __init__(self, ap, tile):
        self.ap = ap
        self.tile = tile

    def __getitem__(self, key):
        return TAP(self.ap[key], self.tile)

    def __getattr__(self, name):
        attr = getattr(self.ap, name)
        if callable(attr):
            def f(*a, **kw):
                r = attr(*a, **kw)
                if isinstance(r, bass.AP):
                    return TAP(r, self.tile)
                return r
            return f
        return attr


class Eng:
    def __init__(self, kb, name, eng, sem):
        self.kb = kb
        self.name = name
        self.eng = eng
        self.sem = sem
        self.count = 0
        self.known = {}

    def __getattr__(self, fn):
        def f(*a, **kw):
            return self.kb.emit(self, fn, a, kw)
        return f


class KB:
    def __init__(self, nc, es, n_dma_sems=92):
        self.nc = nc
        self.es = es
        self.sems = {}
        self.engs = {}
        for name, eng in (("pe", nc.tensor), ("dve", nc.vector), ("act", nc.scalar),
                          ("pool", nc.gpsimd), ("sp", nc.sync)):
            sem = es.enter_context(nc.semaphore("S_" + name))
            self.sems[name] = sem
            self.engs[name] = Eng(self, name, eng, name)
        self.pe, self.dve, self.act, self.pool, self.sp = (self.engs[n] for n in ("pe", "dve", "act", "pool", "sp"))
        self.dma_free = []
        self.dma_count = {}
        for i in range(n_dma_sems):
            key = "D%d" % i
            self.sems[key] = es.enter_context(nc.semaphore(key))
            self.dma_count[key] = 0
            self.dma_free.append(key)
        self.phase_tiles = []
        self.phase_stack = None
        self.all_tiles = []
        self.n_inst = 0
        self.n_wait = 0

    def begin_phase(self):
        self.phase_stack = ExitStack()
        self.phase_tiles = []

    def end_phase(self):
        self.barrier()
        for t in self.phase_tiles:
            if t.dsem is not None:
                self.dma_free.append(t.dsem)
                t.dsem = None
        self.phase_stack.close()
        self.phase_stack = None
        self.phase_tiles = []

    def begin_hold(self):
        self.hold_stack = ExitStack()
        self.hold_tiles = []

    def end_hold(self):
        for t in self.hold_tiles:
            if t.dsem is not None:
                self.dma_free.append(t.dsem)
                t.dsem = None
        self.hold_stack.close()
        self.hold_stack = None
        self.hold_tiles = []

    def sb(self, name, shape, dtype, glob=False, hold=False):
        st = self.es if glob else (self.hold_stack if hold else self.phase_stack)
        self.n_alloc = getattr(self, "n_alloc", 0) + 1
        name = "%s_%d" % (name, self.n_alloc)
        t = st.enter_context(self.nc.sbuf_tensor(name, list(shape), dtype))
        tl = Tile(self, t, name)
        if hold:
            self.hold_tiles.append(tl)
        elif not glob:
            self.phase_tiles.append(tl)
        self.all_tiles.append(tl)
        return tl

    def ps(self, name, shape, dtype):
        t = self.es.enter_context(self.nc.psum_tensor(name, list(shape), dtype))
        tl = Tile(self, t, name)
        self.all_tiles.append(tl)
        return tl

    def token(self, name):
        tl = Tile(self, None, name)
        self.all_tiles.append(tl)
        return tl

    def tile_dsem(self, tile):
        if tile.dsem is None:
            tile.dsem = self.dma_free.pop()
        return tile.dsem

    def emit(self, E, fn, args, kw):
        reads, writes = [], []
        kw = dict(kw)
        xr = kw.pop("_reads", [])
        xw = kw.pop("_writes", [])

        def unwrap(v, is_out):
            if isinstance(v, TAP):
                (writes if is_out else reads).append(v.tile)
                return v.ap
            return v
        a2 = [unwrap(a, (i == 0 and fn in ("matmul", "transpose"))) for i, a in enumerate(args)]
        kw2 = {k_: unwrap(v, k_ in ("out", "accum_out", "ap", "out_ap")) for k_, v in kw.items()}
        reads.extend(xr)
        writes.extend(xw)
        is_dma = fn in ("dma_start", "indirect_dma_start")
        deps = {}

        def add(ev):
            if ev is None:
                return
            sk, val, src = ev
            if src is E and E.name == "pe":
                return
            if deps.get(sk, 0) < val:
                deps[sk] = val
        for t in reads:
            add(t.last_write)
        for t in writes:
            add(t.last_write)
            for sk, (val, src) in t.readers.items():
                add((sk, val, src))
        for sk, val in deps.items():
            if E.known.get(sk, 0) >= val:
                continue
            E.eng.wait_ge(self.sems[sk], val)
            E.known[sk] = val
            self.n_wait += 1
        inst = getattr(E.eng, fn)(*a2, **kw2)
        self.n_inst += 1
        if is_dma:
            sbt = None
            for t in writes + reads:
                if t.t is not None:
                    sbt = t
                    break
            sk = self.tile_dsem(sbt)
            self.dma_count[sk] += 16
            inst.then_inc(self.sems[sk], 16)
            ev = (sk, self.dma_count[sk], None)
        else:
            E.count += 1
            inst.then_inc(self.sems[E.sem], 1)
            ev = (E.sem, E.count, E)
        for t in writes:
            t.last_write = ev
            t.readers = {}
        for t in reads:
            if t in writes:
                continue
            sk, val, src = ev
            if t.readers.get(sk, (0, None))[0] < val:
                t.readers[sk] = (val, src)
        return inst

    def barrier(self):
        sp = self.sp
        for name in ("pe", "dve", "act", "pool"):
            e = self.engs[name]
            if sp.known.get(name, 0) < e.count:
                sp.eng.wait_ge(self.sems[name], e.count)
                sp.known[name] = e.count
        for sk, c in self.dma_count.items():
            if c > 0 and sp.known.get(sk, 0) < c:
                sp.eng.wait_ge(self.sems[sk], c)
                sp.known[sk] = c
        sp.count += 1
        sp.eng.nop().then_inc(self.sems["sp"], 1)
        for name in ("pe", "dve", "act", "pool"):
            e = self.engs[name]
            e.eng.wait_ge(self.sems["sp"], sp.count)
        for e in self.engs.values():
            for n2, e2 in self.engs.items():
                e.known[n2] = e2.count
            for sk, c in self.dma_count.items():
                e.known[sk] = c
        for t in self.all_tiles:
            t.last_write = None
            t.readers = {}


def bfview(pt):
    return pt[:].bitcast(BF16)


def load_cast(kb, dst, src, eng=None):
    n = src.shape[-1]
    step = 2048
    for c0 in range(0, n, step):
        c1 = min(n, c0 + step)
        kb.pool.dma_start(out=dst[..., c0:c1], in_=src[..., c0:c1])


DBG = {}


def build(depth=DEPTH, debug=(), phases=None, moe_depth=DEPTH, final_norm=True):
    nc = bass.Bass("TRN2", target_bir_lowering=False)
    dbg = set(debug)

    def din(name, shape, dt=F32):
        return nc.dram_tensor(name, list(shape), dt, kind="ExternalInput").ap()

    def dscr(name, shape, dt=F32):
        kind = "ExternalOutput" if name in dbg else "Internal"
        return nc.dram_tensor(name, list(shape), dt, kind=kind).ap()

    x_in = din("x", [S, D])
    mem_in = din("mem", [256, D])
    W = {}
    W["norm_mix_g"] = din("norm_mix_g", [DEPTH, D])
    W["w_in"] = din("w_in", [DEPTH, D, INC])
    W["pool_w"] = din("pool_w", [DEPTH, 4, 64, 64])
    W["pool_scale"] = din("pool_scale", [DEPTH, 256])
    W["mlstm_conv_wT"] = din("mlstm_conv_wT", [DEPTH, 512, 5])
    W["mlstm_gate_b"] = din("mlstm_gate_b", [DEPTH, 16])
    W["mlstm_norm_g"] = din("mlstm_norm_g", [DEPTH, 256])
    W["q_norm_g"] = din("q_norm_g", [DEPTH, 64])
    W["k_norm_g"] = din("k_norm_g", [DEPTH, 64])
    W["w_out"] = din("w_out", [DEPTH, D, D])
    W["norm_mem_g"] = din("norm_mem_g", [DEPTH, D])
    W["ca_wq"] = din("ca_wq", [DEPTH, D, 512])
    W["ca_wkv"] = din("ca_wkv", [DEPTH, D, D])
    W["ca_wo"] = din("ca_wo", [DEPTH, 512, D])
    W["norm_ffn_g"] = din("norm_ffn_g", [DEPTH, D])
    W["router_w"] = din("router_w", [DEPTH, D, NEXP])
    W["expert_w_gu"] = din("expert_w_gu", [moe_depth, NEXP, D, 2 * DFF])
    W["expert_w_down"] = din("expert_w_down", [moe_depth, NEXP, DFF, D])
    W["final_norm_g"] = din("final_norm_g", [D])
    C_cos = din("c_cos", [S, 32])
    C_sin = din("c_sin", [S, 32])
    C_identf = din("c_identf", [128, 128])
    C_invc = din("c_invc", [256, S])
    C_tri = din("c_tri", [128, 256])
    out = nc.dram_tensor("out", [S, D], F32, kind="ExternalOutput").ap()

    xs = dscr("xs", [S, D])
    featT = dscr("featT", [768, S])
    mv_d = dscr("mv_d", [S, 256], BF16)
    mo_d = dscr("mo_d", [S, 256], BF16)
    gates_d = dscr("gates_d", [S, 16])
    qT_d = dscr("qT_d", [4, 128, S], BF16)
    kT_d = dscr("kT_d", [2, 128, S], BF16)
    av_d = dscr("av_d", [S, 128], BF16)
    yT_d = dscr("yT_d", [D, S], BF16)
    hn_d = dscr("hn_d", [S, D], BF16)
    aff_d = dscr("aff_d", [NEXP, S])
    qk_d = dscr("qk_d", [4, 128, S], BF16)

    DBG.clear()
    if "dbg_X" in dbg:
        DBG["X"] = dscr("dbg_X", [128, 4 * D], BF16)
        DBG["hT"] = dscr("dbg_hT", [128, 16 * CAP], BF16)
        DBG["yg"] = dscr("dbg_yg", [128, D], F32)
    if "dbg_idx" in dbg:
        DBG["idx"] = dscr("dbg_idx", [128, 64], U32)
        DBG["g"] = dscr("dbg_g", [128, 64], F32)
    es = ExitStack()
    with es:
        kb = KB(nc, es)
        pe, dve, act, pool, sp = kb.pe, kb.dve, kb.act, kb.pool, kb.sp
        PS = [kb.ps("psum%d" % i, [128, 512], F32) for i in range(8)]
        identf = kb.sb("identf", [128, 128], F32, glob=True)
        identb = kb.sb("identb", [128, 128], BF16, glob=True)
        idxT = kb.sb("G_idxT", [128, 4, NEXP], U32, glob=True)
        gT = kb.sb("G_gT", [128, 4, NEXP], F32, glob=True)
        sp.dma_start(out=identf[:], in_=C_identf[:, :])
        pool.dma_start(out=identb[:], in_=C_identf[:, :])

        for layer in range(depth):
            if phases is None or "A" in phases:
                phase_A(kb, nc, layer, W, PS, identb, (x_in if layer == 0 else xs), featT, mv_d, mo_d, gates_d, qT_d, kT_d, av_d, C_cos, C_sin)
            if phases is None or "B" in phases:
                phase_B(kb, nc, layer, W, PS, featT, yT_d, C_invc)
            fuse_c0 = phases is None
            if phases is None or "D" in phases:
                phase_D(kb, nc, layer, PS, qT_d, kT_d, av_d, yT_d,
                        c0=({"W": W, "featT": featT, "qk_d": qk_d} if fuse_c0 else None))
            if phases is None or "C" in phases:
                phase_C(kb, nc, layer, W, PS, identb, featT, mv_d, mo_d, gates_d, qk_d, yT_d, C_tri, skip_c0=fuse_c0)
            if phases is None or "E" in phases:
                kb.begin_hold()
                efw = {"wo": kb.sb("E_wo", [128, 8, D], BF16, hold=True), "wq": kb.sb("E_wq", [128, 8, 512], BF16, hold=True),
                       "wkv": kb.sb("E_wkv", [128, 8, D], BF16, hold=True), "cwo": kb.sb("E_cwo", [128, 4, D], BF16, hold=True)}
                for c in range(8):
                    kb.pool.dma_start(out=efw["wo"][:, c, :], in_=W["w_out"][layer][c * 128:(c + 1) * 128, :])
                    kb.pool.dma_start(out=efw["wq"][:, c, :], in_=W["ca_wq"][layer][c * 128:(c + 1) * 128, :])
                    kb.pool.dma_start(out=efw["wkv"][:, c, :], in_=W["ca_wkv"][layer][c * 128:(c + 1) * 128, :])
                for c in range(4):
                    kb.pool.dma_start(out=efw["cwo"][:, c, :], in_=W["ca_wo"][layer][c * 128:(c + 1) * 128, :])
                phase_EF(kb, nc, layer, W, PS, identb, (x_in if layer == 0 else xs), xs, yT_d, mem_in, efw,
                         g1args={"identf": identf, "hn_d": hn_d, "aff_d": aff_d})
                kb.end_hold()
            if phases is None or "G" in phases:
                phase_G(kb, nc, layer, W, PS, identb, identf, xs, hn_d, idxT, gT, aff_d)

        kb.begin_phase()
        cp = [kb.sb("cpo%d" % i, [128, 4, D], F32) for i in range(2)]
        co = [kb.sb("cpq%d" % i, [128, 4, D], F32) for i in range(2)]
        fst = [kb.sb("fst%d" % i, [128, 4], F32) for i in range(2)]
        fjunk = kb.sb("fjunk", [128, D], BF16)
        fg = kb.sb("fg", [128, D], F32)
        sp.dma_start(out=fg[:], in_=W["final_norm_g"].partition_broadcast(128))
        for blk in range(8):
            t = cp[blk % 2]
            o = co[blk % 2]
            stt = fst[blk % 2]
            rows = slice(blk * 512, (blk + 1) * 512)
            sp.dma_start(out=t[:], in_=xs[rows, :].rearrange("(j p) d -> p j d", p=128))
            if final_norm:
                for j in range(4):
                    act.activation(out=fjunk[:], in_=t[:, j, :], func=AF.Square, accum_out=stt[:, j:j + 1])
                act.activation(out=stt[:], in_=stt[:], func=AF.Sqrt, scale=1.0 / D, bias=EPS)
                dve.reciprocal(out=stt[:], in_=stt[:])
                for j in range(4):
                    dve.scalar_tensor_tensor(out=o[:, j, :], in0=t[:, j, :], scalar=stt[:, j:j + 1], in1=fg[:], op0=ALU.mult, op1=ALU.mult)
                sp.dma_start(out=out[rows, :].rearrange("(j p) d -> p j d", p=128), in_=o[:])
            else:
                sp.dma_start(out=out[rows, :].rearrange("(j p) d -> p j d", p=128), in_=t[:])
        kb.end_phase()
        print("instructions", kb.n_inst, "waits", kb.n_wait)
    return nc


def phase_A(kb, nc, layer, W, PS, identb, xs, featT, mv_d, mo_d, gates_d, qT_d, kT_d, av_d, C_cos, C_sin):
    pe, dve, act, pool, sp = kb.pe, kb.dve, kb.act, kb.pool, kb.sp
    kb.begin_phase()
    w = kb.sb("A_w", [128, 8, INC], BF16)
    wsrc = W["w_in"][layer].rearrange("(c p) n -> p c n", p=128)
    for c in range(8):
        pool.dma_start(out=w[:, c, 0:1280], in_=wsrc[:, c, 0:1280])
        pool.dma_start(out=w[:, c, 1280:1792], in_=wsrc[:, c, 1296:1808])
        pool.dma_start(out=w[:, c, 1792:1808], in_=wsrc[:, c, 1280:1296])
        pool.dma_start(out=w[:, c, 1808:2064], in_=wsrc[:, c, 1808:2064])
    g_bc = kb.sb("A_g", [128, D], F32)
    sp.dma_start(out=g_bc[:], in_=W["norm_mix_g"][layer].partition_broadcast(128))
    gq = kb.sb("A_gq", [128, 64], F32)
    gk = kb.sb("A_gk", [128, 64], F32)
    gb = kb.sb("A_gb", [128, 16], F32)
    sp.dma_start(out=gq[:], in_=W["q_norm_g"][layer].partition_broadcast(128))
    sp.dma_start(out=gk[:], in_=W["k_norm_g"][layer].partition_broadcast(128))
    sp.dma_start(out=gb[:], in_=W["mlstm_gate_b"][layer].partition_broadcast(128))
    dve.tensor_scalar(out=gq[:], in0=gq[:], scalar1=0.125, scalar2=None, op0=ALU.mult)
    cos_t = kb.sb("A_cos", [128, NT, 32], F32)
    sin_t = kb.sb("A_sin", [128, NT, 32], F32)
    sp.dma_start(out=cos_t[:], in_=C_cos.rearrange("(j p) f -> p j f", p=128))
    sp.dma_start(out=sin_t[:], in_=C_sin.rearrange("(j p) f -> p j f", p=128))

    xb = [kb.sb("A_x%d" % i, [128, 4, D], F32) for i in range(2)]
    hn = [kb.sb("A_hn%d" % i, [128, D], BF16) for i in range(2)]
    junk = kb.sb("A_junk", [128, D], BF16)
    st = [kb.sb("A_st%d" % i, [128, 4], F32) for i in range(2)]
    hnT = [kb.sb("A_hnT%d" % i, [128, 8, 512], BF16) for i in range(2)]
    fst = [kb.sb("A_fst%d" % i, [128, 512], F32) for i in range(2)]
    vo = [kb.sb("A_vo%d" % i, [128, 512], BF16) for i in range(2)]
    g2 = [kb.sb("A_g2%d" % i, [128, 272], F32) for i in range(2)]
    gt = [kb.sb("A_gt%d" % i, [128, 16], F32) for i in range(2)]
    avs = [kb.sb("A_av%d" % i, [128, 128], BF16) for i in range(2)]
    qs = [kb.sb("A_qs%d" % i, [128, 512], F32) for i in range(2)]
    sq = kb.sb("A_sq", [128, 512], F32)
    ss8 = kb.sb("A_ss8", [128, 8], F32)
    tmp = [kb.sb("A_tmp%d" % i, [128, 256], F32) for i in range(4)]
    qr = [kb.sb("A_qr%d" % i, [128, 512], BF16) for i in range(2)]
    kr = [kb.sb("A_kr%d" % i, [128, 2, 2, 64], BF16) for i in range(2)]
    qTb = [kb.sb("A_qTb%d" % i, [128, 4, 512], BF16) for i in range(2)]
    kTb = [kb.sb("A_kTb%d" % i, [128, 2, 512], BF16) for i in range(2)]

    sqk = kb.sb("A_sqk", [128, 128], F32)
    ss8k = kb.sb("A_ss8k", [128, 2], F32)
    tmpk = [kb.sb("A_tmpk%d" % i, [128, 64], F32) for i in range(4)]

    def hnr_a(src, nh, sqb, ssb):
        n = nh * 64
        dve.tensor_tensor(out=sqb[:, 0:n], in0=src, in1=src, op=ALU.mult)
        dve.tensor_reduce(out=ssb[:, 0:nh], in_=sqb[:, 0:n].rearrange("p (h d) -> p h d", d=64), axis=AX.X, op=ALU.add)

    def hnr_s(nh, ssb):
        act.activation(out=ssb[:, 0:nh], in_=ssb[:, 0:nh], func=AF.Sqrt, scale=1.0 / 64, bias=EPS)

    def hnr_b(src, nh, g, j_tile, dst_views, sqb, ssb, tmps):
        n = nh * 64
        dve.reciprocal(out=ssb[:, 0:nh], in_=ssb[:, 0:nh])
        s3 = src.rearrange("p (h d) -> p h d", d=64)
        q3 = sqb[:, 0:n].rearrange("p (h d) -> p h d", d=64)
        dve.tensor_tensor(out=q3, in0=s3, in1=ssb[:, 0:nh].unsqueeze(2).to_broadcast([128, nh, 64]), op=ALU.mult)
        dve.tensor_tensor(out=q3, in0=q3, in1=g[:].unsqueeze(1).to_broadcast([128, nh, 64]), op=ALU.mult)
        q4 = sqb[:, 0:n].rearrange("p (h i two) -> p h i two", h=nh, two=2)
        x0 = q4[:, :, :, 0]
        x1 = q4[:, :, :, 1]
        cb = cos_t[:, j_tile, :].unsqueeze(1).to_broadcast([128, nh, 32])
        sb_ = sin_t[:, j_tile, :].unsqueeze(1).to_broadcast([128, nh, 32])
        m = nh * 32
        tv = [t[:, 0:m].rearrange("p (h i) -> p h i", h=nh) for t in tmps]
        dve.tensor_tensor(out=tv[0], in0=x0, in1=cb, op=ALU.mult)
        dve.tensor_tensor(out=tv[1], in0=x1, in1=sb_, op=ALU.mult)
        dve.tensor_tensor(out=tv[2], in0=x0, in1=sb_, op=ALU.mult)
        dve.tensor_tensor(out=tv[3], in0=x1, in1=cb, op=ALU.mult)
        for dv in dst_views:
            dve.tensor_tensor(out=dv[:, :, :, 0], in0=tv[0], in1=tv[1], op=ALU.subtract)
            dve.tensor_tensor(out=dv[:, :, :, 1], in0=tv[2], in1=tv[3], op=ALU.add)

    def prologue(blk):
        rows = slice(blk * 512, (blk + 1) * 512)
        X = xb[blk % 2]
        HT = hnT[blk % 2]
        sp.dma_start(out=X[:], in_=xs[rows, :].rearrange("(j p) d -> p j d", p=128))
        stt = st[blk % 2]
        for j in range(4):
            act.activation(out=junk[:], in_=X[:, j, :], func=AF.Square, accum_out=stt[:, j:j + 1])
        act.activation(out=stt[:], in_=stt[:], func=AF.Sqrt, scale=1.0 / D, bias=EPS)
        dve.reciprocal(out=stt[:], in_=stt[:])
        for j in range(4):
            H = hn[j % 2]
            dve.scalar_tensor_tensor(out=H[:], in0=X[:, j, :], scalar=stt[:, j:j + 1], in1=g_bc[:], op0=ALU.mult, op1=ALU.mult)
            pt = PS[j % 2]
            pv = bfview(pt)
            for c in range(8):
                pe.transpose(pv[:, c * 128:(c + 1) * 128], H[:, c * 128:(c + 1) * 128], identb[:])
            act.activation(out=HT[:, :, j * 128:(j + 1) * 128], in_=pv.rearrange("p (c t) -> p c t", c=8), func=AF.Copy)

    def mainA(blk):
        rows = slice(blk * 512, (blk + 1) * 512)
        HT = hnT[blk % 2]
        for ch in range(6):
            pt = PS[2 + ch % 2]
            for c in range(8):
                pe.matmul(pt[:], lhsT=w[:, c, ch * 128:(ch + 1) * 128], rhs=HT[:, c, :], start=(c == 0), stop=(c == 7))
            f = fst[ch % 2]
            act.activation(out=f[:], in_=pt[:], func=AF.Copy)
            sp.dma_start(out=featT[ch * 128:(ch + 1) * 128, rows], in_=f[:])
        QT = qTb[blk % 2]
        KT = kTb[blk % 2]

        def part1(j):
            jt = blk * 4 + j
            trow = slice(jt * 128, (jt + 1) * 128)
            p1, p2, p3 = PS[4], PS[5], PS[7]
            for c in range(8):
                pe.matmul(p1[:], lhsT=HT[:, c, j * 128:(j + 1) * 128], rhs=w[:, c, 768:1280], start=(c == 0), stop=(c == 7))
            for c in range(8):
                pe.matmul(p2[:, 0:272], lhsT=HT[:, c, j * 128:(j + 1) * 128], rhs=w[:, c, 1792:2064], start=(c == 0), stop=(c == 7))
            for c in range(8):
                pe.matmul(p3[:], lhsT=HT[:, c, j * 128:(j + 1) * 128], rhs=w[:, c, 1280:1792], start=(c == 0), stop=(c == 7))
            V = vo[j % 2]
            G2 = g2[j % 2]
            Q = qs[j % 2]
            GT = gt[j % 2]
            AV = avs[j % 2]
            KR = kr[j % 2]
            QR = qr[j % 2]
            act.activation(out=G2[:], in_=p2[:, 0:272], func=AF.Copy)
            act.activation(out=Q[:], in_=p3[:], func=AF.Copy)
            act.activation(out=V[:, 0:256], in_=p1[:, 0:256], func=AF.Copy)
            act.activation(out=V[:, 256:512], in_=p1[:, 256:512], func=AF.Sigmoid)
            sp.dma_start(out=mv_d[trow, :], in_=V[:, 0:256])
            sp.dma_start(out=mo_d[trow, :], in_=V[:, 256:512])
            dve.tensor_tensor(out=GT[:], in0=G2[:, 0:16], in1=gb[:], op=ALU.add)
            dve.tensor_copy(out=AV[:], in_=G2[:, 144:272])
            sp.dma_start(out=av_d[trow, :], in_=AV[:])
            hnr_a(G2[:, 16:144], 2, sqk, ss8k)
            hnr_a(Q[:], 8, sq, ss8)
            act.activation(out=GT[:, 8:16], in_=GT[:, 8:16], func=AF.Exp, scale=-1.0)
            act.activation(out=GT[:, 8:16], in_=GT[:, 8:16], func=AF.Ln, bias=1.0)
            hnr_s(2, ss8k)
            hnr_s(8, ss8)
            dve.tensor_scalar(out=GT[:, 8:16], in0=GT[:, 8:16], scalar1=-1.0, scalar2=None, op0=ALU.mult)
            sp.dma_start(out=gates_d[trow, :], in_=GT[:])
            hnr_b(G2[:, 16:144], 2, gk, jt,
                  [KR[:, :, 0, :].rearrange("p h (i two) -> p h i two", two=2),
                   KR[:, :, 1, :].rearrange("p h (i two) -> p h i two", two=2)], sqk, ss8k, tmpk)
            hnr_b(Q[:], 8, gq, jt, [QR[:].rearrange("p (h i two) -> p h i two", h=8, two=2)], sq, ss8, tmp)

        def part2(j):
            KR = kr[j % 2]
            QR = qr[j % 2]
            pv = bfview(PS[6])
            for kv in range(2):
                pe.transpose(pv[:, kv * 128:(kv + 1) * 128], KR[:, kv, :, :].rearrange("p a d -> p (a d)"), identb[:])
            for c in range(4):
                pe.transpose(pv[:, 256 + c * 128:256 + (c + 1) * 128], QR[:, c * 128:(c + 1) * 128], identb[:])
            act.activation(out=KT[:, :, j * 128:(j + 1) * 128], in_=pv[:, 0:256].rearrange("p (c t) -> p c t", c=2), func=AF.Copy)
            act.activation(out=QT[:, :, j * 128:(j + 1) * 128], in_=pv[:, 256:768].rearrange("p (c t) -> p c t", c=4), func=AF.Copy)
        for j in range(4):
            part1(j)
            if j >= 1:
                part2(j - 1)
        part2(3)
        sp.dma_start(out=qT_d[:, :, rows].rearrange("c p t -> p c t"), in_=QT[:])
        sp.dma_start(out=kT_d[:, :, rows].rearrange("c p t -> p c t"), in_=KT[:])

    prologue(0)
    for blk in range(8):
        if blk + 1 < 8:
            prologue(blk + 1)
        mainA(blk)
    kb.end_phase()


def phase_B(kb, nc, layer, W, PS, featT, yT_d, C_invc):
    pe, dve, act, pool, sp = kb.pe, kb.dve, kb.act, kb.pool, kb.sp
    kb.begin_phase()
    PADL = 16
    WID = S + 32
    U = kb.sb("B_U", [128, 2, WID], F32)
    dve.memset(U[:, :, 0:PADL], 0.0)
    dve.memset(U[:, :, PADL + S:WID], 0.0)
    for c in range(2):
        sp.dma_start(out=U[:, c, PADL:PADL + S], in_=featT[c * 128:(c + 1) * 128, :])
    invc = kb.sb("B_invc", [128, 2, S], F32)
    sp.dma_start(out=invc[:], in_=C_invc.rearrange("(c p) t -> p c t", p=128))
    BD = kb.sb("B_BD", [128, 2, 128], BF16)
    dve.memset(BD[:], 0.0)
    for c in range(2):
        pool.dma_start(out=BD[0:64, c, 0:64], in_=W["pool_w"][layer][2 * c])
        pool.dma_start(out=BD[64:128, c, 64:128], in_=W["pool_w"][layer][2 * c + 1])
    psc = kb.sb("B_psc", [128, 2], F32)
    for c in range(2):
        sp.dma_start(out=psc[:, c:c + 1], in_=W["pool_scale"][layer][c * 128:(c + 1) * 128].rearrange("(p o) -> p o", o=1))
    P2a = kb.sb("B_P2a", [128, WID], F32)
    P2b = kb.sb("B_P2b", [128, WID], F32)
    P4b = kb.sb("B_P4b", [128, WID], F32)
    P8b = kb.sb("B_P8b", [128, WID], F32)
    Ss = kb.sb("B_S", [128, 2, S], F32)
    dT = kb.sb("B_dT", [128, 2, S], BF16)

    def rng(t, lo, hi, sh=0):
        return slice(PADL + lo + sh, PADL + hi + sh)
    lo = -8
    u0, u1 = U[:, 0, :], U[:, 1, :]
    pool.tensor_tensor(out=Ss[0:64, 0, :], in0=U[0:64, 0, rng(0, 0, S, -1)], in1=U[0:64, 0, rng(0, 0, S)], op=ALU.add)
    dve.tensor_tensor(out=P2a[64:128, rng(0, lo, S + 12)], in0=U[64:128, 0, rng(0, lo, S + 12)], in1=U[64:128, 0, rng(0, lo, S + 12, 1)], op=ALU.add)
    dve.tensor_tensor(out=Ss[64:128, 0, :], in0=P2a[64:128, rng(0, 0, S, -2)], in1=P2a[64:128, rng(0, 0, S)], op=ALU.add)
    pool.tensor_tensor(out=P2b[:, rng(0, lo, S + 12)], in0=U[:, 1, rng(0, lo, S + 12)], in1=U[:, 1, rng(0, lo, S + 12, 1)], op=ALU.add)
    pool.tensor_tensor(out=P4b[:, rng(0, lo, S + 8)], in0=P2b[:, rng(0, lo, S + 8)], in1=P2b[:, rng(0, lo, S + 8, 2)], op=ALU.add)
    dve.tensor_tensor(out=Ss[0:64, 1, :], in0=P4b[0:64, rng(0, 0, S, -4)], in1=P4b[0:64, rng(0, 0, S)], op=ALU.add)
    pool.tensor_tensor(out=P8b[64:128, rng(0, lo, S + 4)], in0=P4b[64:128, rng(0, lo, S + 4)], in1=P4b[64:128, rng(0, lo, S + 4, 4)], op=ALU.add)
    dve.tensor_tensor(out=Ss[64:128, 1, :], in0=P8b[64:128, rng(0, 0, S, -8)], in1=P8b[64:128, rng(0, 0, S)], op=ALU.add)
    for c in range(2):
        dve.tensor_tensor(out=Ss[:, c, :], in0=Ss[:, c, :], in1=invc[:, c, :], op=ALU.mult)
        dve.tensor_tensor(out=dT[:, c, :], in0=Ss[:, c, :], in1=U[:, c, PADL:PADL + S], op=ALU.subtract)
    yst = [kb.sb("B_y%d" % i, [128, 512], BF16) for i in range(2)]
    k = 0
    for blk in range(8):
        for c in range(2):
            pt = PS[k % 2]
            pe.matmul(pt[:], lhsT=BD[:, c, :], rhs=dT[:, c, blk * 512:(blk + 1) * 512], start=True, stop=True)
            Y = yst[k % 2]
            k += 1
            act.activation(out=Y[:], in_=pt[:], func=AF.Copy, scale=psc[:, c:c + 1])
            sp.dma_start(out=yT_d[c * 128:(c + 1) * 128, blk * 512:(blk + 1) * 512], in_=Y[:])
    kb.end_phase()


def phase_C(kb, nc, layer, W, PS, identb, featT, mv_d, mo_d, gates_d, qk_d, yT_d, C_tri, skip_c0=False):
    pe, dve, act, pool, sp = kb.pe, kb.dve, kb.act, kb.pool, kb.sp
    if not skip_c0:
        kb.begin_phase()
        cw = kb.sb("C_cw", [128, 4, 5], F32)
        sp.dma_start(out=cw[:], in_=W["mlstm_conv_wT"][layer].rearrange("(c p) j -> p c j", p=128))
        Xp = [kb.sb("C_Xp%d" % i, [128, S + 4], F32) for i in range(2)]
        acc = [kb.sb("C_acc%d" % i, [128, S], F32) for i in range(2)]
        sgm = [kb.sb("C_sgm%d" % i, [128, S], F32) for i in range(2)]
        qko = [kb.sb("C_qko%d" % i, [128, S], BF16) for i in range(2)]
        for i in range(2):
            dve.memset(Xp[i][:, 0:2], 0.0)
            dve.memset(Xp[i][:, S + 2:S + 4], 0.0)
        def loadC0(ci):
            sp.dma_start(out=Xp[ci % 2][:, 2:S + 2], in_=featT[256 + ci * 128:256 + (ci + 1) * 128, :])
        loadC0(0)
        for ci in range(4):
            X = Xp[ci % 2]
            A_ = acc[ci % 2]
            G_ = sgm[ci % 2]
            O_ = qko[ci % 2]
            if ci + 1 < 4:
                loadC0(ci + 1)
            dve.tensor_scalar(out=A_[:], in0=X[:, 0:S], scalar1=cw[:, ci, 0:1], scalar2=None, op0=ALU.mult)
            for j in range(1, 5):
                dve.scalar_tensor_tensor(out=A_[:], in0=X[:, j:j + S], scalar=cw[:, ci, j:j + 1], in1=A_[:], op0=ALU.mult, op1=ALU.add)
            act.activation(out=G_[:], in_=A_[:], func=AF.Sigmoid)
            dve.scalar_tensor_tensor(out=O_[:], in0=A_[:], scalar=(1.0 if ci < 2 else 0.125), in1=G_[:], op0=ALU.mult, op1=ALU.mult)
            sp.dma_start(out=qk_d[ci], in_=O_[:])
        kb.end_phase()
    import os
    CSTOP = int(os.environ.get("CSTOP", "9"))
    if CSTOP <= 0:
        return
    kb.begin_phase()
    QK = kb.sb("C_QK", [128, 4, S], BF16)
    sp.dma_start(out=QK[:], in_=qk_d.rearrange("c p t -> p c t"))
    TRI = kb.sb("C_TRI", [128, 2, 128], F32)
    sp.dma_start(out=TRI[:], in_=C_tri.rearrange("p (a j) -> p a j", a=2))
    ones = kb.sb("C_ones", [128, 128], F32)
    dve.memset(ones[:], 1.0)
    G = kb.sb("C_G", [128, NT, 16], F32)
    sp.dma_start(out=G[:], in_=gates_d.rearrange("(c p) n -> p c n", p=128))
    ybf = kb.sb("C_ybf", [128, NT, 256], BF16)
    sp.dma_start(out=ybf[:], in_=mv_d.rearrange("(c p) n -> p c n", p=128))
    Va = kb.sb("C_Va", [128, NT, 4, 65], BF16)
    dve.memset(Va[:, :, :, 64:65], 1.0)
    dve.tensor_copy(out=Va[:, :, :, 0:64], in_=ybf[:].rearrange("p c (h d) -> p c h d", h=4))
    Kt = kb.sb("C_Kt", [128, NT, 256], BF16)
    for cg in range(8):
        pv = bfview(PS[cg % 2])
        for cl in range(4):
            c = cg * 4 + cl
            for hp in range(2):
                pe.transpose(pv[:, (cl * 2 + hp) * 128:(cl * 2 + hp + 1) * 128], QK[:, 2 + hp, c * 128:(c + 1) * 128], identb[:])
        act.activation(out=Kt[:, cg * 4:(cg + 1) * 4, :], in_=pv.rearrange("p (c n) -> p c n", c=4), func=AF.Copy)
    A = [kb.sb("C_A%d" % i, [128, NT, 4], F32) for i in range(2)]
    A2 = [kb.sb("C_A2%d" % i, [128, NT, 4], F32) for i in range(2)]
    Bc = [kb.sb("C_B%d" % i, [128, NT, 4], F32) for i in range(2)]
    Fc = [kb.sb("C_F%d" % i, [128, NT, 4], F32) for i in range(2)]
    at = kb.sb("C_at", [128, NT, 4], F32)
    lfc = kb.sb("C_lfc", [128, 2, NT * 4], F32)
    for d_ in range(2):
        dve.tensor_copy(out=lfc[:, d_, :].rearrange("p (c h) -> p c h", h=4), in_=G[:, :, 8 + 4 * d_:12 + 4 * d_])
    for d_ in range(2):
        lfv = lfc[:, d_, :]
        liv = G[:, :, 4 * d_:4 * d_ + 4]
        pc, ptot = PS[6], PS[7]
        pe.matmul(pc[:, 0:128], lhsT=TRI[:, d_, :], rhs=lfv, start=True, stop=True)
        pe.matmul(ptot[:, 0:128], lhsT=ones[:], rhs=lfv, start=True, stop=True)
        pc3 = pc[:, 0:128].rearrange("p (c h) -> p c h", h=4)
        pt3 = ptot[:, 0:128].rearrange("p (c h) -> p c h", h=4)
        dve.tensor_tensor(out=at[:], in0=liv, in1=pc3, op=ALU.subtract)
        act.activation(out=A[d_][:], in_=at[:], func=AF.Exp)
        dve.tensor_tensor(out=at[:], in0=at[:], in1=pt3, op=ALU.add)
        act.activation(out=A2[d_][:], in_=at[:], func=AF.Exp)
        act.activation(out=Bc[d_][:], in_=pc3, func=AF.Exp)
        act.activation(out=Fc[d_][:], in_=pt3, func=AF.Exp)
    if CSTOP <= 1:
        kb.end_phase()
        return
    Hd = [kb.sb("C_H%d" % i, [128, NT, 256], F32) for i in range(2)]
    Zf = [kb.sb("C_Zf%d" % i, [128, 4, 65], F32) for i in range(2)]
    Zb = [[kb.sb("C_Zb%d_%d" % (i, j), [128, 4, 65], BF16) for j in range(2)] for i in range(2)]
    for d_ in range(2):
        dve.memset(Zf[d_][:], 0.0)
        dve.memset(Zb[d_][0][:], 0.0)
    VWr = [kb.sb("C_VW%d" % i, [128, 2, 4, 65], BF16) for i in range(4)]
    SMr = [kb.sb("C_SM%d" % i, [128, 2, 2, 128], BF16) for i in range(4)]
    t4r = [kb.sb("C_t4%d" % i, [128, 4], F32) for i in range(4)]
    for it in range(NT):
        cc = [it, NT - 1 - it]
        VWs = [VWr[(2 * it + d_) % 4] for d_ in range(2)]
        SMs = [SMr[(2 * it + d_) % 4] for d_ in range(2)]
        t4s = [t4r[(2 * it + d_) % 4] for d_ in range(2)]
        pss = [[PS[0], PS[1]], [PS[2], PS[3]]]
        psn = [PS[4], PS[5]]
        psu = [PS[6], PS[7]]
        for d_ in range(2):
            c = cc[d_]
            VW = VWs[d_]
            dve.tensor_tensor(out=VW[:, 0], in0=Va[:, c], in1=A[d_][:, c, :].unsqueeze(2).to_broadcast([128, 4, 65]), op=ALU.mult)
            dve.tensor_tensor(out=VW[:, 1], in0=Va[:, c], in1=A2[d_][:, c, :].unsqueeze(2).to_broadcast([128, 4, 65]), op=ALU.mult)
        for d_ in range(2):
            c = cc[d_]
            cs = slice(c * 128, (c + 1) * 128)
            for h in range(4):
                pb = (h % 2) * 64
                pe.matmul(pss[d_][h % 2][:, (h // 2) * 128:(h // 2 + 1) * 128], lhsT=QK[pb:pb + 64, 2 + h // 2, cs], rhs=QK[pb:pb + 64, h // 2, cs], start=True, stop=True)
        for d_ in range(2):
            for hh in range(2):
                dve.tensor_tensor(out=SMs[d_][:, hh], in0=pss[d_][hh][:, 0:256].rearrange("p (h j) -> p h j", h=2),
                                  in1=TRI[:, d_, :].unsqueeze(1).to_broadcast([128, 2, 128]), op=ALU.mult)
        for d_ in range(2):
            c = cc[d_]
            cs = slice(c * 128, (c + 1) * 128)
            Zc = Zb[d_][it % 2]
            for h in range(4):
                pe.matmul(psn[d_][:, h * 65:(h + 1) * 65], lhsT=SMs[d_][:, h % 2, h // 2, :], rhs=VWs[d_][:, 0, h, :], start=True, stop=False)
                pe.matmul(psn[d_][:, h * 65:(h + 1) * 65], lhsT=QK[:, h // 2, cs], rhs=Zc[:, h, :], start=False, stop=True)
            for hp in range(2):
                pe.matmul(psu[d_][:, hp * 130:(hp + 1) * 130], lhsT=Kt[:, c, hp * 128:(hp + 1) * 128],
                          rhs=VWs[d_][:, 1, 2 * hp:2 * hp + 2, :].rearrange("p a b -> p (a b)"), start=True, stop=True)
        for d_ in range(2):
            c = cc[d_]
            for hp in range(2):
                for hh in range(2):
                    rows = slice(hh * 64, (hh + 1) * 64)
                    h = 2 * hp + hh
                    dve.scalar_tensor_tensor(out=Zf[d_][rows, h, :], in0=Zf[d_][rows, h, :], scalar=Fc[d_][rows, c, h:h + 1],
                                             in1=psu[d_][rows, hp * 130 + hh * 65:hp * 130 + (hh + 1) * 65], op0=ALU.mult, op1=ALU.add)
            act.activation(out=Zb[d_][(it + 1) % 2][:], in_=Zf[d_][:], func=AF.Copy)
        n3s = [psn[d_][:, 0:260].rearrange("p (h e) -> p h e", h=4) for d_ in range(2)]
        for d_ in range(2):
            dve.tensor_tensor(out=t4s[d_][:], in0=n3s[d_][:, :, 64], in1=Bc[d_][:, cc[d_], :], op=ALU.mult)
        for d_ in range(2):
            act.activation(out=t4s[d_][:], in_=t4s[d_][:], func=AF.Abs)
        for d_ in range(2):
            dve.tensor_scalar(out=t4s[d_][:], in0=t4s[d_][:], scalar1=1.0, scalar2=None, op0=ALU.max)
            dve.reciprocal(out=t4s[d_][:], in_=t4s[d_][:])
            dve.tensor_tensor(out=t4s[d_][:], in0=t4s[d_][:], in1=Bc[d_][:, cc[d_], :], op=ALU.mult)
            dve.tensor_tensor(out=Hd[d_][:, cc[d_], :].rearrange("p (h e) -> p h e", h=4), in0=n3s[d_][:, :, 0:64],
                              in1=t4s[d_][:].unsqueeze(2).to_broadcast([128, 4, 64]), op=ALU.mult)
    if CSTOP <= 2:
        kb.end_phase()
        return
    H0, H1 = Hd
    ng = kb.sb("C_ng", [128, 256], F32)
    sp.dma_start(out=ng[:], in_=W["mlstm_norm_g"][layer].partition_broadcast(128))
    mo = kb.sb("C_mo", [128, NT, 256], BF16)
    sp.dma_start(out=mo[:], in_=mo_d.rearrange("(c p) n -> p c n", p=128))
    ss = kb.sb("C_ss", [128, NT, 4], F32)
    for half in range(2):
        cs = slice(half * 16, (half + 1) * 16)
        dve.tensor_tensor(out=H0[:, cs, :], in0=H0[:, cs, :], in1=H1[:, cs, :], op=ALU.add)
        pool.tensor_tensor(out=H1[:, cs, :], in0=H0[:, cs, :], in1=H0[:, cs, :], op=ALU.mult)
        dve.tensor_reduce(out=ss[:, cs, :], in_=H1[:, cs, :].rearrange("p c (h e) -> p c h e", h=4), axis=AX.X, op=ALU.add)
    act.activation(out=ss[:], in_=ss[:], func=AF.Sqrt, scale=1.0 / 64, bias=EPS)
    dve.reciprocal(out=ss[:], in_=ss[:])
    for half in range(2):
        cs = slice(half * 16, (half + 1) * 16)
        dve.tensor_tensor(out=H0[:, cs, :].rearrange("p c (h e) -> p c h e", h=4), in0=H0[:, cs, :].rearrange("p c (h e) -> p c h e", h=4),
                          in1=ss[:, cs, :].unsqueeze(3).to_broadcast([128, 16, 4, 64]), op=ALU.mult)
        pool.tensor_tensor(out=H0[:, cs, :], in0=H0[:, cs, :], in1=ng[:].unsqueeze(1).to_broadcast([128, 16, 256]), op=ALU.mult)
        dve.tensor_tensor(out=ybf[:, cs, :], in0=H0[:, cs, :], in1=mo[:, cs, :], op=ALU.mult)
    yst = [kb.sb("C_yst%d" % i, [128, 1024], BF16) for i in range(2)]
    k = 0
    for hp in range(2):
        for cg in range(4):
            pv = bfview(PS[k % 2])
            Y = yst[k % 2]
            k += 1
            for cl in range(8):
                c = cg * 8 + cl
                pe.transpose(pv[:, cl * 128:(cl + 1) * 128], ybf[:, c, hp * 128:(hp + 1) * 128], identb[:])
            act.activation(out=Y[:], in_=pv, func=AF.Copy)
            sp.dma_start(out=yT_d[256 + hp * 128:256 + (hp + 1) * 128, cg * 1024:(cg + 1) * 1024], in_=Y[:])
    kb.end_phase()


def phase_D(kb, nc, layer, PS, qT_d, kT_d, av_d, yT_d, c0=None):
    pe, dve, act, pool, sp = kb.pe, kb.dve, kb.act, kb.pool, kb.sp
    kb.begin_phase()
    kT2 = kb.sb("D_kT", [128, 2, S], BF16)
    sp.dma_start(out=kT2[:], in_=kT_d.rearrange("c p t -> p c t"))
    Va = kb.sb("D_Va", [128, NT, 2, 128], BF16)
    dve.memset(Va[:, :, :, 64:128], 1.0)
    Vst = kb.sb("D_Vst", [128, NT, 128], BF16)
    sp.dma_start(out=Vst[:], in_=av_d.rearrange("(c p) n -> p c n", p=128))
    dve.tensor_copy(out=Va[:, :, :, 0:64], in_=Vst[:].rearrange("p c (h d) -> p c h d", h=2))
    Qe = [kb.sb("D_Qe%d" % i, [128, S], BF16) for i in range(2)]
    Qo = [kb.sb("D_Qo%d" % i, [128, S], BF16) for i in range(2)]
    for i in range(2):
        pool.memset(Qe[i][64:128, :], 0.0)
        pool.memset(Qo[i][0:64, :], 0.0)

    def load_q(pr):
        sp.dma_start(out=Qe[pr % 2][0:64, :], in_=qT_d[pr, 0:64, :])
        sp.dma_start(out=Qo[pr % 2][64:128, :], in_=qT_d[pr, 64:128, :])
    P = [kb.sb("D_P%d" % i, [128, 512], BF16) for i in range(4)]
    rd = [kb.sb("D_rd%d" % i, [64, 512], F32) for i in range(2)]
    yo = [kb.sb("D_yo%d" % i, [64, 512], BF16) for i in range(2)]
    SB = PS[0:4]
    OB = PS[4:6]
    steps = []
    for pr in range(4):
        for hh in range(2):
            for qb in range(8):
                for kc in range(NT):
                    steps.append((pr, hh, qb, kc))
    load_q(0)

    def issue_S(i):
        pr, hh, qb, kc = steps[i]
        kv = pr // 2
        if hh == 0 and qb == 0 and kc == 0 and pr + 1 < 4:
            load_q(pr + 1)
        Qt = (Qe if hh == 0 else Qo)[pr % 2]
        pe.matmul(SB[i % 4][:], lhsT=kT2[:, kv, kc * 128:(kc + 1) * 128],
                  rhs=Qt[:, qb * 512:(qb + 1) * 512], start=True, stop=True)
    sched = {}
    if c0 is not None:
        Wc, featT_, qk_d_ = c0["W"], c0["featT"], c0["qk_d"]
        cw = kb.sb("C_cw", [128, 4, 5], F32)
        sp.dma_start(out=cw[:], in_=Wc["mlstm_conv_wT"][layer].rearrange("(c p) j -> p c j", p=128))
        Xc = kb.sb("C_Xp", [128, S + 4], F32)
        Ac = kb.sb("C_acc", [128, S], F32)
        Gc = kb.sb("C_sgm", [128, S], F32)
        Oc = kb.sb("C_qko", [128, S], BF16)
        dve.memset(Xc[:, 0:2], 0.0)
        dve.memset(Xc[:, S + 2:S + 4], 0.0)

        def c0_a(ci):
            sp.dma_start(out=Xc[:, 2:S + 2], in_=featT_[256 + ci * 128:256 + (ci + 1) * 128, :])
            dve.tensor_scalar(out=Ac[:], in0=Xc[:, 0:S], scalar1=cw[:, ci, 0:1], scalar2=None, op0=ALU.mult)
            for j in range(1, 5):
                dve.scalar_tensor_tensor(out=Ac[:], in0=Xc[:, j:j + S], scalar=cw[:, ci, j:j + 1], in1=Ac[:], op0=ALU.mult, op1=ALU.add)

        def c0_b(ci):
            act.activation(out=Gc[:], in_=Ac[:], func=AF.Sigmoid)

        def c0_c(ci):
            dve.scalar_tensor_tensor(out=Oc[:], in0=Ac[:], scalar=(1.0 if ci < 2 else 0.125), in1=Gc[:], op0=ALU.mult, op1=ALU.mult)
            sp.dma_start(out=qk_d_[ci], in_=Oc[:])
        for ci in range(4):
            base = 40 + ci * 500
            sched[base] = (c0_a, ci)
            sched[base + 170] = (c0_b, ci)
            sched[base + 340] = (c0_c, ci)
    LOOK = 2
    for i in range(min(LOOK, len(steps))):
        issue_S(i)
    ob_i = 0
    for i, (pr, hh, qb, kc) in enumerate(steps):
        if i in sched:
            sched[i][0](sched[i][1])
        if i + LOOK < len(steps):
            issue_S(i + LOOK)
        kv = pr // 2
        Pt = P[i % 4]
        act.activation(out=Pt[:], in_=SB[i % 4][:], func=AF.Exp)
        O = OB[ob_i % 2]
        pe.matmul(O[:], lhsT=Va[:, kc, kv, :], rhs=Pt[:], start=(kc == 0), stop=(kc == NT - 1))
        if kc == NT - 1:
            h = 2 * pr + hh
            R = rd[ob_i % 2]
            Y = yo[ob_i % 2]
            dve.reciprocal(out=R[:], in_=O[64:128, :])
            dve.tensor_tensor(out=Y[:], in0=O[0:64, :], in1=R[:], op=ALU.mult)
            sp.dma_start(out=yT_d[512 + h * 64:512 + (h + 1) * 64, qb * 512:(qb + 1) * 512], in_=Y[:])
            ob_i += 1
    kb.end_phase()


def phase_EF(kb, nc, layer, W, PS, identb, xsrc, xs, yT_d, mem_in, efw, g1args=None):
    pe, dve, act, pool, sp = kb.pe, kb.dve, kb.act, kb.pool, kb.sp
    kb.begin_phase()
    wo, wq, wkv, cwo = efw["wo"], efw["wq"], efw["wkv"], efw["cwo"]
    g1_block = g1_finish = None
    if g1args is not None:
        g1_block, g1_finish = make_g1(kb, nc, layer, W, PS, g1args["identf"], g1args["hn_d"], g1args["aff_d"])
    g_bc = kb.sb("E_g", [128, D], F32)
    sp.dma_start(out=g_bc[:], in_=W["norm_mem_g"][layer].partition_broadcast(128))
    ones = kb.sb("E_ones", [128, 128], BF16)
    dve.memset(ones[:], 1.0)
    memb = kb.sb("E_memb", [128, 2, D], BF16)
    for mc in range(2):
        pool.dma_start(out=memb[:, mc, :], in_=mem_in[mc * 128:(mc + 1) * 128, :])
    memT = kb.sb("E_memT", [128, 8, 256], BF16)
    for mc in range(2):
        pv = bfview(PS[mc])
        for c in range(8):
            pe.transpose(pv[:, c * 128:(c + 1) * 128], memb[:, mc, c * 128:(c + 1) * 128], identb[:])
        act.activation(out=memT[:, :, mc * 128:(mc + 1) * 128], in_=pv.rearrange("p (c t) -> p c t", c=8), func=AF.Copy)
    kcT = kb.sb("E_kcT", [128, 4, 256], BF16)
    for h in range(4):
        pt = PS[2 + h % 2]
        for c in range(8):
            pe.matmul(pt[:, 0:256], lhsT=wkv[:, c, h * 128:(h + 1) * 128], rhs=memT[:, c, :], start=(c == 0), stop=(c == 7))
        act.activation(out=kcT[:, h, :], in_=pt[:, 0:256], func=AF.Copy)
    Vc = kb.sb("E_Vc", [128, 2, 512], BF16)
    for mc in range(2):
        pt = PS[4 + mc]
        for c in range(8):
            pe.matmul(pt[:], lhsT=memT[:, c, mc * 128:(mc + 1) * 128], rhs=wkv[:, c, 512:1024], start=(c == 0), stop=(c == 7))
        act.activation(out=Vc[:, mc, :], in_=pt[:], func=AF.Copy)

    xb = [kb.sb("E_x%d" % i, [128, 4, D], F32) for i in range(2)]
    Yb = [kb.sb("E_Y%d" % i, [128, 8, 512], BF16) for i in range(2)]
    hn = [kb.sb("E_hn%d" % i, [128, D], BF16) for i in range(2)]
    junk = kb.sb("E_junk", [128, D], BF16)
    st = [kb.sb("E_st%d" % i, [128, 4], F32) for i in range(2)]
    hnT = [kb.sb("E_hnT%d" % i, [128, 8, 512], BF16) for i in range(2)]
    qcT = [kb.sb("E_qcT%d" % i, [128, 4, 512], BF16) for i in range(2)]
    PT = [kb.sb("E_PT%d" % i, [128, 2, 512], BF16) for i in range(2)]
    rden = [kb.sb("E_rden%d" % i, [128, 512], F32) for i in range(2)]
    oT = [kb.sb("E_oT%d" % i, [128, 4, 512], BF16) for i in range(2)]
    SC = float(128 ** -0.5)
    def loadE(blk):
        rows_ = slice(blk * 512, (blk + 1) * 512)
        sp.dma_start(out=xb[blk % 2][:], in_=xsrc[rows_, :].rearrange("(j p) d -> p j d", p=128))
        sp.dma_start(out=Yb[blk % 2][:], in_=yT_d[:, rows_].rearrange("(c p) t -> p c t", p=128))

    def stage1(blk):
        X = xb[blk % 2]
        Y = Yb[blk % 2]
        HT = hnT[blk % 2]
        stt = st[blk % 2]
        for j in range(4):
            for half in range(2):
                pt = PS[half]
                for c in range(8):
                    pe.matmul(pt[:], lhsT=Y[:, c, j * 128:(j + 1) * 128], rhs=wo[:, c, half * 512:(half + 1) * 512], start=(c == 0), stop=(c == 7))
                dve.tensor_tensor(out=X[:, j, half * 512:(half + 1) * 512], in0=X[:, j, half * 512:(half + 1) * 512], in1=pt[:], op=ALU.add)
            act.activation(out=junk[:], in_=X[:, j, :], func=AF.Square, accum_out=stt[:, j:j + 1])
        act.activation(out=stt[:], in_=stt[:], func=AF.Sqrt, scale=1.0 / D, bias=EPS)
        dve.reciprocal(out=stt[:], in_=stt[:])
        for j in range(4):
            H = hn[j % 2]
            dve.scalar_tensor_tensor(out=H[:], in0=X[:, j, :], scalar=stt[:, j:j + 1], in1=g_bc[:], op0=ALU.mult, op1=ALU.mult)
            pt = PS[2 + j % 2]
            pv = bfview(pt)
            for c in range(8):
                pe.transpose(pv[:, c * 128:(c + 1) * 128], H[:, c * 128:(c + 1) * 128], identb[:])
            act.activation(out=HT[:, :, j * 128:(j + 1) * 128], in_=pv.rearrange("p (c t) -> p c t", c=8), func=AF.Copy)

    def stage2(blk):
        rows = slice(blk * 512, (blk + 1) * 512)
        X = xb[blk % 2]
        HT = hnT[blk % 2]
        QC = qcT[blk % 2]
        for h in range(4):
            pt = PS[4 + h % 2]
            for c in range(8):
                pe.matmul(pt[:], lhsT=wq[:, c, h * 128:(h + 1) * 128], rhs=HT[:, c, :], start=(c == 0), stop=(c == 7))
            act.activation(out=QC[:, h, :], in_=pt[:], func=AF.Copy)
        OT = oT[blk % 2]
        for h in range(4):
            Pt = PT[h % 2]
            for mc in range(2):
                pt = PS[6 + mc]
                pe.matmul(pt[:], lhsT=kcT[:, h, mc * 128:(mc + 1) * 128], rhs=QC[:, h, :], start=True, stop=True)
                act.activation(out=Pt[:, mc, :], in_=pt[:], func=AF.Exp, scale=SC)
            po = PS[4]
            pd = PS[5]
            for mc in range(2):
                pe.matmul(po[:], lhsT=Vc[:, mc, h * 128:(h + 1) * 128], rhs=Pt[:, mc, :], start=(mc == 0), stop=(mc == 1))
            for mc in range(2):
                pe.matmul(pd[:], lhsT=ones[:], rhs=Pt[:, mc, :], start=(mc == 0), stop=(mc == 1))
            R = rden[h % 2]
            dve.reciprocal(out=R[:], in_=pd[:])
            dve.tensor_tensor(out=OT[:, h, :], in0=po[:], in1=R[:], op=ALU.mult)
        for j in range(4):
            for half in range(2):
                pt = PS[6 + half]
                for c in range(4):
                    pe.matmul(pt[:], lhsT=OT[:, c, j * 128:(j + 1) * 128], rhs=cwo[:, c, half * 512:(half + 1) * 512], start=(c == 0), stop=(c == 3))
                dve.tensor_tensor(out=X[:, j, half * 512:(half + 1) * 512], in0=X[:, j, half * 512:(half + 1) * 512], in1=pt[:], op=ALU.add)
        sp.dma_start(out=xs[rows, :].rearrange("(j p) d -> p j d", p=128), in_=X[:])
        if g1_block is not None:
            g1_block(X, blk)
    loadE(0)
    loadE(1)
    stage1(0)
    for blk in range(8):
        if blk + 1 < 8:
            stage1(blk + 1)
        stage2(blk)
        if blk + 2 < 8:
            loadE(blk + 2)
    if g1_finish is not None:
        g1_finish()
    kb.end_phase()


def make_g1(kb, nc, layer, W, PS, identf, hn_d, aff_d):
    pe, dve, act, pool, sp = kb.pe, kb.dve, kb.act, kb.pool, kb.sp
    g_bc = kb.sb("G_g", [128, D], F32)
    sp.dma_start(out=g_bc[:], in_=W["norm_ffn_g"][layer].partition_broadcast(128))
    rw = kb.sb("G_rw", [128, 8, NEXP], F32)
    sp.dma_start(out=rw[:], in_=W["router_w"][layer].rearrange("(c p) e -> p c e", p=128))
    Hf = [kb.sb("G_Hf%d" % i, [128, D], F32) for i in range(2)]
    Hb = [kb.sb("G_Hb%d" % i, [128, D], BF16) for i in range(2)]
    HT = [kb.sb("G_HT%d" % i, [128, 8, 128], F32) for i in range(2)]
    junk = kb.sb("G_junk", [128, D], BF16)
    st = [kb.sb("G_st%d" % i, [128, 4], F32) for i in range(2)]
    LG = kb.sb("G_LG", [128, NT, NEXP], F32)
    mx = kb.sb("G_mx", [128, NT], F32)
    wst = [kb.sb("G_wst%d" % i, [NEXP, 512], F32) for i in range(2)]

    def block_fn(X, blk):
        stt = st[blk % 2]
        for j in range(4):
            act.activation(out=junk[:], in_=X[:, j, :], func=AF.Square, accum_out=stt[:, j:j + 1])
        act.activation(out=stt[:], in_=stt[:], func=AF.Sqrt, scale=1.0 / D, bias=EPS)
        dve.reciprocal(out=stt[:], in_=stt[:])
        for j in range(4):
            jt = blk * 4 + j
            H = Hf[j % 2]
            dve.scalar_tensor_tensor(out=H[:], in0=X[:, j, :], scalar=stt[:, j:j + 1], in1=g_bc[:], op0=ALU.mult, op1=ALU.mult)
            B_ = Hb[j % 2]
            act.activation(out=B_[:], in_=H[:], func=AF.Copy)
            sp.dma_start(out=hn_d[jt * 128:(jt + 1) * 128, :], in_=B_[:])
            T = HT[j % 2]
            for half in range(2):
                pt = PS[half]
                for c in range(4):
                    cc = half * 4 + c
                    pe.transpose(pt[:, c * 128:(c + 1) * 128], H[:, cc * 128:(cc + 1) * 128], identf[:])
                act.activation(out=T[:, half * 4:(half + 1) * 4, :], in_=pt[:].rearrange("p (c t) -> p c t", c=4), func=AF.Copy)
            pl = PS[2 + j % 2]
            for c in range(8):
                pe.matmul(pl[:, 0:NEXP], lhsT=T[:, c, :], rhs=rw[:, c, :], start=(c == 0), stop=(c == 7))
            dve.tensor_copy(out=LG[:, jt, :], in_=pl[:, 0:NEXP])

    def finish_fn():
        dve.tensor_reduce(out=mx[:], in_=LG[:], axis=AX.X, op=ALU.max)
        dve.tensor_tensor(out=LG[:], in0=LG[:], in1=mx[:].unsqueeze(2).to_broadcast([128, NT, NEXP]), op=ALU.subtract)
        act.activation(out=LG[:], in_=LG[:], func=AF.Exp)
        dve.tensor_reduce(out=mx[:], in_=LG[:], axis=AX.X, op=ALU.add)
        dve.reciprocal(out=mx[:], in_=mx[:])
        dve.tensor_tensor(out=LG[:], in0=LG[:], in1=mx[:].unsqueeze(2).to_broadcast([128, NT, NEXP]), op=ALU.mult)
        for blk in range(8):
            pt = PS[4 + blk % 2]
            for j in range(4):
                jt = blk * 4 + j
                pe.transpose(pt[0:NEXP, j * 128:(j + 1) * 128], LG[:, jt, :], identf[:])
            wt = wst[blk % 2]
            act.activation(out=wt[:], in_=pt[0:NEXP, :], func=AF.Copy)
            sp.dma_start(out=aff_d[:, blk * 512:(blk + 1) * 512], in_=wt[:])
    return block_fn, finish_fn


def phase_G(kb, nc, layer, W, PS, identb, identf, xs, hn_d, idxT, gT, aff_d):
    pe, dve, act, pool, sp = kb.pe, kb.dve, kb.act, kb.pool, kb.sp
    kb.begin_phase()
    ws = [kb.sb("G_w%d" % i, [128, 8, 1024], BF16) for i in range(6)]
    wgu = W["expert_w_gu"][layer]
    wdn = W["expert_w_down"][layer]

    def load_gu(e, p):
        half, g = p // 2, p % 2
        pool.dma_start(out=ws[p][:], in_=wgu[e][:, g * DFF + half * 1024:g * DFF + (half + 1) * 1024].rearrange("(c p) f -> p c f", p=128))

    def load_wd(e, p):
        pool.dma_start(out=ws[4 + p][:], in_=wdn[e][p * 1024:(p + 1) * 1024, :].rearrange("(c p) n -> p c n", p=128))
    for p in range(4):
        load_gu(0, p)
    for p in range(2):
        load_wd(0, p)
    work = kb.sb("G_work", [NEXP, S], F32)
    sp.dma_start(out=work[:], in_=aff_d[:, :])
    top = kb.sb("G_top", [NEXP, CAP], F32)
    idx = kb.sb("G_idx", [NEXP, CAP], U32)
    for r in range(CAP // 8):
        sl = slice(r * 8, (r + 1) * 8)
        dve.max(out=top[:, sl], in_=work[:])
        dve.max_index(out=idx[:, sl], in_max=top[:, sl], in_values=work[:])
        dve.match_replace(out=work[:], in_to_replace=top[:, sl], in_values=work[:], imm_value=-1.0)
    idxf = kb.sb("G_idxf", [NEXP, CAP], F32)
    dve.tensor_copy(out=idxf[:], in_=idx[:])
    pt = PS[6]
    for ct in range(4):
        pe.transpose(pt[:, ct * NEXP:(ct + 1) * NEXP], idxf[:, ct * 128:(ct + 1) * 128], identf[0:NEXP, 0:NEXP])
    dve.tensor_copy(out=idxT[:], in_=pt[:, 0:4 * NEXP].rearrange("p (c e) -> p c e", c=4))
    pt = PS[7]
    for ct in range(4):
        pe.transpose(pt[:, ct * NEXP:(ct + 1) * NEXP], top[:, ct * 128:(ct + 1) * 128], identf[0:NEXP, 0:NEXP])
    dve.tensor_copy(out=gT[:], in_=pt[:, 0:4 * NEXP].rearrange("p (c e) -> p c e", c=4))
    if DBG.get("idx") is not None:
        sp.dma_start(out=DBG["idx"][:, :], in_=idxT[:].rearrange("p c e -> p (c e)"))
        sp.dma_start(out=DBG["g"][:, :], in_=gT[:].rearrange("p c e -> p (c e)"))
    Xe = [kb.sb("G_Xe%d" % i, [128, 4, D], BF16) for i in range(2)]
    XeT = [kb.sb("G_XeT%d" % i, [128, 8, CAP], BF16) for i in range(2)]
    hT = kb.sb("G_hT", [128, 16, CAP], BF16)
    sg = [kb.sb("G_sg%d" % i, [128, CAP], F32) for i in range(2)]
    yg = [kb.sb("G_yg%d" % i, [128, D], F32) for i in range(2)]
    xs_tok = kb.token("xs_tok")
    def gather(e):
        X = Xe[e % 2]
        for ct in range(4):
            pool.indirect_dma_start(out=X[:, ct, :], out_offset=None, in_=hn_d[:, :],
                                    in_offset=bass.IndirectOffsetOnAxis(ap=idxT[:, ct, e:e + 1].ap, axis=0),
                                    _reads=[idxT])
        return X
    yi = 0
    Xn = gather(0)
    for e in range(NEXP):
        X = Xn
        if e + 1 < NEXP:
            Xn = gather(e + 1)
        XT = XeT[e % 2]
        for ct in range(4):
            pv = bfview(PS[ct % 2])
            for c in range(8):
                pe.transpose(pv[:, c * 128:(c + 1) * 128], X[:, ct, c * 128:(c + 1) * 128], identb[:])
            act.activation(out=XT[:, :, ct * 128:(ct + 1) * 128], in_=pv.rearrange("p (c t) -> p c t", c=8), func=AF.Copy)
        for fj in range(16):
            half, fl = fj // 8, fj % 8
            wga, wup = ws[2 * half], ws[2 * half + 1]
            pa = PS[2 + (fj % 2) * 2]
            pb_ = PS[3 + (fj % 2) * 2]
            for c in range(8):
                pe.matmul(pa[:], lhsT=wga[:, c, fl * 128:(fl + 1) * 128], rhs=XT[:, c, :], start=(c == 0), stop=(c == 7))
            for c in range(8):
                pe.matmul(pb_[:], lhsT=wup[:, c, fl * 128:(fl + 1) * 128], rhs=XT[:, c, :], start=(c == 0), stop=(c == 7))
            sgt = sg[fj % 2]
            act.activation(out=sgt[:], in_=pa[:], func=AF.Sigmoid)
            dve.tensor_tensor(out=sgt[:], in0=sgt[:], in1=pa[:], op=ALU.mult)
            dve.tensor_tensor(out=hT[:, fj, :], in0=sgt[:], in1=pb_[:], op=ALU.mult)
            if fl == 7 and e + 1 < NEXP:
                load_gu(e + 1, 2 * half)
                load_gu(e + 1, 2 * half + 1)
        if e == 0 and DBG.get("X") is not None:
            sp.dma_start(out=DBG["X"][:, :], in_=X[:].rearrange("p c d -> p (c d)"))
            sp.dma_start(out=DBG["hT"][:, :], in_=hT[:].rearrange("p c d -> p (c d)"))
        for ct in range(4):
            Y = yg[yi % 2]
            yi += 1
            for half in range(2):
                pt = PS[6 + half]
                for fj in range(16):
                    pe.matmul(pt[:], lhsT=hT[:, fj, ct * 128:(ct + 1) * 128], rhs=ws[4 + fj // 8][:, fj % 8, half * 512:(half + 1) * 512],
                              start=(fj == 0), stop=(fj == 15))
                dve.tensor_scalar(out=Y[:, half * 512:(half + 1) * 512], in0=pt[:], scalar1=gT[:, ct, e:e + 1], scalar2=None, op0=ALU.mult)
            if e == 0 and ct == 0 and DBG.get("yg") is not None:
                sp.dma_start(out=DBG["yg"][:, :], in_=Y[:])
            pool.indirect_dma_start(out=xs[:, :], out_offset=bass.IndirectOffsetOnAxis(ap=idxT[:, ct, e:e + 1].ap, axis=0),
                                    in_=Y[:], in_offset=None, compute_op=ALU.add,
                                    _reads=[idxT], _writes=[xs_tok])
        if e + 1 < NEXP:
            load_wd(e + 1, 0)
            load_wd(e + 1, 1)
    kb.end_phase()


def rope_tables():
    t = np.arange(S)
    row = (t // 64).astype(np.float32)
    col = (t % 64).astype(np.float32)
    inv = (10000.0 ** (-(np.arange(16, dtype=np.float32)) / 16)).astype(np.float32)
    ang = np.concatenate([row[:, None] * inv, col[:, None] * inv], axis=-1).astype(np.float32)
    return np.cos(ang).astype(np.float32), np.sin(ang).astype(np.float32)


def pool_invcount():
    t = np.arange(S)
    tab = np.zeros((256, S), np.float32)
    for g, win in enumerate((2, 4, 8, 16)):
        lo = np.clip(t - win // 2, 0, S)
        hi = np.clip(t + win // 2, 0, S)
        tab[g * 64:(g + 1) * 64, :] = (1.0 / (hi - lo).astype(np.float32))[None, :]
    return tab


def consts():
    c, s = rope_tables()
    tri = np.zeros((128, 2, 128), np.float32)
    si = np.arange(128)[:, None]
    ji = np.arange(128)[None, :]
    tri[:, 0, :] = (si <= ji)
    tri[:, 1, :] = (si >= ji)
    return {"c_cos": c, "c_sin": s, "c_identf": np.eye(128, dtype=np.float32), "c_invc": pool_invcount(),
            "c_tri": tri.reshape(128, 256)}


_NC_CACHE = {}


def kernel(**inputs):
    if "nc" not in _NC_CACHE:
        _NC_CACHE["nc"] = build()
    nc = _NC_CACHE["nc"]
    cst = consts()
    shared = {k: np.ascontiguousarray(np.asarray(v, dtype=np.float32)) for k, v in inputs.items()
              if k not in ("x", "mem", "mlstm_conv_w")}
    shared["mlstm_conv_wT"] = np.ascontiguousarray(np.asarray(inputs["mlstm_conv_w"], dtype=np.float32).transpose(0, 2, 1))
    shared.update(cst)
    x = np.asarray(inputs["x"], dtype=np.float32)
    mem = np.asarray(inputs["mem"], dtype=np.float32)
    in_maps = []
    for b in range(8):
        m = dict(shared)
        m["x"] = np.ascontiguousarray(x[b])
        m["mem"] = np.ascontiguousarray(mem[b])
        in_maps.append(m)
    res = run_bass_kernel_spmd(nc, in_maps, core_ids=list(range(8)))
    return np.stack([r["out"] for r in res.results], axis=0).astype(np.float32)
```

```python
import numpy as np
from contextlib import ExitStack
import concourse.bass as bass
import concourse.mybir as mybir
from concourse.bass_utils import run_bass_kernel_spmd

F32 = mybir.dt.float32
BF16 = mybir.dt.bfloat16
U32 = mybir.dt.uint32
I32 = mybir.dt.int32
AF = mybir.ActivationFunctionType
ALU = mybir.AluOpType
AX = mybir.AxisListType

S = 4096
D = 1024
NT = S // 128
DEPTH = 4
INC = 2064
EPS = 1e-6
NEXP = 16
CAP = 512
DFF = 2048


class Tile:
    def __init__(self, kb, t, name):
        self.kb = kb
        self.t = t
        self.name = name
        self.last_write = None
        self.readers = {}
        self.dsem = None

    def __getitem__(self, key):
        return TAP(self.t[key], self)

    def ap(self):
        return TAP(self.t[:], self)


class TAP:
    def __init__(self, ap, tile):
        self.ap = ap
        self.tile = tile

    def __getitem__(self, key):
        return TAP(self.ap[key], self.tile)

    def __getattr__(self, name):
        attr = getattr(self.ap, name)
        if callable(attr):
            def f(*a, **kw):
                r = attr(*a, **kw)
                if isinstance(r, bass.AP):
                    return TAP(r, self.tile)
                return r
            return f
        return attr


class Eng:
    def __init__(self, kb, name, eng, sem):
        self.kb = kb
        self.name = name
        self.eng = eng
        self.sem = sem
        self.count = 0
        self.known = {}

    def __getattr__(self, fn):
        def f(*a, **kw):
            return self.kb.emit(self, fn, a, kw)
        return f


class KB:
    def __init__(self, nc, es, n_dma_sems=92):
        self.nc = nc
        self.es = es
        self.sems = {}
        self.engs = {}
        for name, eng in (("pe", nc.tensor), ("dve", nc.vector), ("act", nc.scalar),
                          ("pool", nc.gpsimd), ("sp", nc.sync)):
            sem = es.enter_context(nc.semaphore("S_" + name))
            self.sems[name] = sem
            self.engs[name] = Eng(self, name, eng, name)
        self.pe, self.dve, self.act, self.pool, self.sp = (self.engs[n] for n in ("pe", "dve", "act", "pool", "sp"))
        self.dma_free = []
        self.dma_count = {}
        for i in range(n_dma_sems):
            key = "D%d" % i
            self.sems[key] = es.enter_context(nc.semaphore(key))
            self.dma_count[key] = 0
            self.dma_free.append(key)
        self.phase_tiles = []
        self.phase_stack = None
        self.all_tiles = []
        self.n_inst = 0
        self.n_wait = 0

    def begin_phase(self):
        self.phase_stack = ExitStack()
        self.phase_tiles = []

    def end_phase(self):
        self.barrier()
        for t in self.phase_tiles:
            if t.dsem is not None:
                self.dma_free.append(t.dsem)
                t.dsem = None
        self.phase_stack.close()
        self.phase_stack = None
        self.phase_tiles = []

    def begin_hold(self):
        self.hold_stack = ExitStack()
        self.hold_tiles = []

    def end_hold(self):
        for t in self.hold_tiles:
            if t.dsem is not None:
                self.dma_free.append(t.dsem)
                t.dsem = None
        self.hold_stack.close()
        self.hold_stack = None
        self.hold_tiles = []

    def sb(self, name, shape, dtype, glob=False, hold=False):
        st = self.es if glob else (self.hold_stack if hold else self.phase_stack)
        self.n_alloc = getattr(self, "n_alloc", 0) + 1
        name = "%s_%d" % (name, self.n_alloc)
        t = st.enter_context(self.nc.sbuf_tensor(name, list(shape), dtype))
        tl = Tile(self, t, name)
        if hold:
            self.hold_tiles.append(tl)
        elif not glob:
            self.phase_tiles.append(tl)
        self.all_tiles.append(tl)
        return tl

    def ps(self, name, shape, dtype):
        t = self.es.enter_context(self.nc.psum_tensor(name, list(shape), dtype))
        tl = Tile(self, t, name)
        self.all_tiles.append(tl)
        return tl

    def token(self, name):
        tl = Tile(self, None, name)
        self.all_tiles.append(tl)
        return tl

    def tile_dsem(self, tile):
        if tile.dsem is None:
            tile.dsem = self.dma_free.pop()
        return tile.dsem

    def emit(self, E, fn, args, kw):
        reads, writes = [], []
        kw = dict(kw)
        xr = kw.pop("_reads", [])
        xw = kw.pop("_writes", [])

        def unwrap(v, is_out):
            if isinstance(v, TAP):
                (writes if is_out else reads).append(v.tile)
                return v.ap
            return v
        a2 = [unwrap(a, (i == 0 and fn in ("matmul", "transpose"))) for i, a in enumerate(args)]
        kw2 = {k_: unwrap(v, k_ in ("out", "accum_out", "ap", "out_ap")) for k_, v in kw.items()}
        reads.extend(xr)
        writes.extend(xw)
        is_dma = fn in ("dma_start", "indirect_dma_start")
        deps = {}

        def add(ev):
            if ev is None:
                return
            sk, val, src = ev
            if src is E and E.name == "pe":
                return
            if deps.get(sk, 0) < val:
                deps[sk] = val
        for t in reads:
            add(t.last_write)
        for t in writes:
            add(t.last_write)
            for sk, (val, src) in t.readers.items():
                add((sk, val, src))
        for sk, val in deps.items():
            if E.known.get(sk, 0) >= val:
                continue
            E.eng.wait_ge(self.sems[sk], val)
            E.known[sk] = val
            self.n_wait += 1
        inst = getattr(E.eng, fn)(*a2, **kw2)
        self.n_inst += 1
        if is_dma:
            sbt = None
            for t in writes + reads:
                if t.t is not None:
                    sbt = t
                    break
            sk = self.tile_dsem(sbt)
            self.dma_count[sk] += 16
            inst.then_inc(self.sems[sk], 16)
            ev = (sk, self.dma_count[sk], None)
        else:
            E.count += 1
            inst.then_inc(self.sems[E.sem], 1)
            ev = (E.sem, E.count, E)
        for t in writes:
            t.last_write = ev
            t.readers = {}
        for t in reads:
            if t in writes:
                continue
            sk, val, src = ev
            if t.readers.get(sk, (0, None))[0] < val:
                t.readers[sk] = (val, src)
        return inst

    def barrier(self):
        sp = self.sp
        for name in ("pe", "dve", "act", "pool"):
            e = self.engs[name]
            if sp.known.get(name, 0) < e.count:
                sp.eng.wait_ge(self.sems[name], e.count)
                sp.known[name] = e.count
        for sk, c in self.dma_count.items():
            if c > 0 and sp.known.get(sk, 0) < c:
                sp.eng.wait_ge(self.sems[sk], c)
                sp.known[sk] = c
        sp.count += 1
        sp.eng.nop().then_inc(self.sems["sp"], 1)
        for name in ("pe", "dve", "act", "pool"):
            e = self.engs[name]
            e.eng.wait_ge(self.sems["sp"], sp.count)
        for e in self.engs.values():
            for n2, e2 in self.engs.items():
                e.known[n2] = e2.count
            for sk, c in self.dma_count.items():
                e.known[sk] = c
        for t in self.all_tiles:
            t.last_write = None
            t.readers = {}


def bfview(pt):
    return pt[:].bitcast(BF16)


def load_cast(kb, dst, src, eng=None):
    n = src.shape[-1]
    step = 2048
    for c0 in range(0, n, step):
        c1 = min(n, c0 + step)
        kb.pool.dma_start(out=dst[..., c0:c1], in_=src[..., c0:c1])


DBG = {}


def build(depth=DEPTH, debug=(), phases=None, moe_depth=DEPTH, final_norm=True):
    nc = bass.Bass("TRN2", target_bir_lowering=False)
    dbg = set(debug)

    def din(name, shape, dt=F32):
        return nc.dram_tensor(name, list(shape), dt, kind="ExternalInput").ap()

    def dscr(name, shape, dt=F32):
        kind = "ExternalOutput" if name in dbg else "Internal"
        return nc.dram_tensor(name, list(shape), dt, kind=kind).ap()

    x_in = din("x", [S, D])
    mem_in = din("mem", [256, D])
    W = {}
    W["norm_mix_g"] = din("norm_mix_g", [DEPTH, D])
    W["w_in"] = din("w_in", [DEPTH, D, INC])
    W["pool_w"] = din("pool_w", [DEPTH, 4, 64, 64])
    W["pool_scale"] = din("pool_scale", [DEPTH, 256])
    W["mlstm_conv_wT"] = din("mlstm_conv_wT", [DEPTH, 512, 5])
    W["mlstm_gate_b"] = din("mlstm_gate_b", [DEPTH, 16])
    W["mlstm_norm_g"] = din("mlstm_norm_g", [DEPTH, 256])
    W["q_norm_g"] = din("q_norm_g", [DEPTH, 64])
    W["k_norm_g"] = din("k_norm_g", [DEPTH, 64])
    W["w_out"] = din("w_out", [DEPTH, D, D])
    W["norm_mem_g"] = din("norm_mem_g", [DEPTH, D])
    W["ca_wq"] = din("ca_wq", [DEPTH, D, 512])
    W["ca_wkv"] = din("ca_wkv", [DEPTH, D, D])
    W["ca_wo"] = din("ca_wo", [DEPTH, 512, D])
    W["norm_ffn_g"] = din("norm_ffn_g", [DEPTH, D])
    W["router_w"] = din("router_w", [DEPTH, D, NEXP])
    W["expert_w_gu"] = din("expert_w_gu", [moe_depth, NEXP, D, 2 * DFF])
    W["expert_w_down"] = din("expert_w_down", [moe_depth, NEXP, DFF, D])
    W["final_norm_g"] = din("final_norm_g", [D])
    C_cos = din("c_cos", [S, 32])
    C_sin = din("c_sin", [S, 32])
    C_identf = din("c_identf", [128, 128])
    C_invc = din("c_invc", [256, S])
    C_tri = din("c_tri", [128, 256])
    out = nc.dram_tensor("out", [S, D], F32, kind="ExternalOutput").ap()

    xs = dscr("xs", [S, D])
    featT = dscr("featT", [768, S])
    mv_d = dscr("mv_d", [S, 256], BF16)
    mo_d = dscr("mo_d", [S, 256], BF16)
    gates_d = dscr("gates_d", [S, 16])
    qT_d = dscr("qT_d", [4, 128, S], BF16)
    kT_d = dscr("kT_d", [2, 128, S], BF16)
    av_d = dscr("av_d", [S, 128], BF16)
    yT_d = dscr("yT_d", [D, S], BF16)
    hn_d = dscr("hn_d", [S, D], BF16)
    aff_d = dscr("aff_d", [NEXP, S])
    qk_d = dscr("qk_d", [4, 128, S], BF16)

    DBG.clear()
    if "dbg_X" in dbg:
        DBG["X"] = dscr("dbg_X", [128, 4 * D], BF16)
        DBG["hT"] = dscr("dbg_hT", [128, 16 * CAP], BF16)
        DBG["yg"] = dscr("dbg_yg", [128, D], F32)
    if "dbg_idx" in dbg:
        DBG["idx"] = dscr("dbg_idx", [128, 64], U32)
        DBG["g"] = dscr("dbg_g", [128, 64], F32)
    es = ExitStack()
    with es:
        kb = KB(nc, es)
        pe, dve, act, pool, sp = kb.pe, kb.dve, kb.act, kb.pool, kb.sp
        PS = [kb.ps("psum%d" % i, [128, 512], F32) for i in range(8)]
        identf = kb.sb("identf", [128, 128], F32, glob=True)
        identb = kb.sb("identb", [128, 128], BF16, glob=True)
        idxT = kb.sb("G_idxT", [128, 4, NEXP], U32, glob=True)
        gT = kb.sb("G_gT", [128, 4, NEXP], F32, glob=True)
        sp.dma_start(out=identf[:], in_=C_identf[:, :])
        pool.dma_start(out=identb[:], in_=C_identf[:, :])

        for layer in range(depth):
            if phases is None or "A" in phases:
                phase_A(kb, nc, layer, W, PS, identb, (x_in if layer == 0 else xs), featT, mv_d, mo_d, gates_d, qT_d, kT_d, av_d, C_cos, C_sin)
            if phases is None or "B" in phases:
                phase_B(kb, nc, layer, W, PS, featT, yT_d, C_invc)
            if phases is None or "C" in phases:
                phase_C(kb, nc, layer, W, PS, identb, featT, mv_d, mo_d, gates_d, qk_d, yT_d, C_tri)
            efw = None
            if phases is None or "E" in phases:
                kb.begin_hold()
                efw = {"wo": kb.sb("E_wo", [128, 8, D], BF16, hold=True), "wq": kb.sb("E_wq", [128, 8, 512], BF16, hold=True),
                       "wkv": kb.sb("E_wkv", [128, 8, D], BF16, hold=True), "cwo": kb.sb("E_cwo", [128, 4, D], BF16, hold=True)}
                for c in range(8):
                    kb.pool.dma_start(out=efw["wo"][:, c, :], in_=W["w_out"][layer][c * 128:(c + 1) * 128, :])
                    kb.pool.dma_start(out=efw["wq"][:, c, :], in_=W["ca_wq"][layer][c * 128:(c + 1) * 128, :])
                    kb.pool.dma_start(out=efw["wkv"][:, c, :], in_=W["ca_wkv"][layer][c * 128:(c + 1) * 128, :])
                for c in range(4):
                    kb.pool.dma_start(out=efw["cwo"][:, c, :], in_=W["ca_wo"][layer][c * 128:(c + 1) * 128, :])
            if phases is None or "D" in phases:
                phase_D(kb, nc, layer, PS, qT_d, kT_d, av_d, yT_d)
            if phases is None or "E" in phases:
                phase_EF(kb, nc, layer, W, PS, identb, (x_in if layer == 0 else xs), xs, yT_d, mem_in, efw,
                         g1args={"identf": identf, "hn_d": hn_d, "aff_d": aff_d})
                kb.end_hold()
            if phases is None or "G" in phases:
                phase_G(kb, nc, layer, W, PS, identb, identf, xs, hn_d, idxT, gT, aff_d)

        kb.begin_phase()
        cp = [kb.sb("cpo%d" % i, [128, 4, D], F32) for i in range(2)]
        co = [kb.sb("cpq%d" % i, [128, 4, D], F32) for i in range(2)]
        fst = [kb.sb("fst%d" % i, [128, 4], F32) for i in range(2)]
        fjunk = kb.sb("fjunk", [128, D], BF16)
        fg = kb.sb("fg", [128, D], F32)
        sp.dma_start(out=fg[:], in_=W["final_norm_g"].partition_broadcast(128))
        for blk in range(8):
            t = cp[blk % 2]
            o = co[blk % 2]
            stt = fst[blk % 2]
            rows = slice(blk * 512, (blk + 1) * 512)
            sp.dma_start(out=t[:], in_=xs[rows, :].rearrange("(j p) d -> p j d", p=128))
            if final_norm:
                for j in range(4):
                    act.activation(out=fjunk[:], in_=t[:, j, :], func=AF.Square, accum_out=stt[:, j:j + 1])
                act.activation(out=stt[:], in_=stt[:], func=AF.Sqrt, scale=1.0 / D, bias=EPS)
                dve.reciprocal(out=stt[:], in_=stt[:])
                for j in range(4):
                    dve.scalar_tensor_tensor(out=o[:, j, :], in0=t[:, j, :], scalar=stt[:, j:j + 1], in1=fg[:], op0=ALU.mult, op1=ALU.mult)
                sp.dma_start(out=out[rows, :].rearrange("(j p) d -> p j d", p=128), in_=o[:])
            else:
                sp.dma_start(out=out[rows, :].rearrange("(j p) d -> p j d", p=128), in_=t[:])
        kb.end_phase()
        print("instructions", kb.n_inst, "waits", kb.n_wait)
    return nc


def phase_A(kb, nc, layer, W, PS, identb, xs, featT, mv_d, mo_d, gates_d, qT_d, kT_d, av_d, C_cos, C_sin):
    pe, dve, act, pool, sp = kb.pe, kb.dve, kb.act, kb.pool, kb.sp
    kb.begin_phase()
    w = kb.sb("A_w", [128, 8, INC], BF16)
    wsrc = W["w_in"][layer].rearrange("(c p) n -> p c n", p=128)
    for c in range(8):
        pool.dma_start(out=w[:, c, 0:1280], in_=wsrc[:, c, 0:1280])
        pool.dma_start(out=w[:, c, 1280:1792], in_=wsrc[:, c, 1296:1808])
        pool.dma_start(out=w[:, c, 1792:1808], in_=wsrc[:, c, 1280:1296])
        pool.dma_start(out=w[:, c, 1808:2064], in_=wsrc[:, c, 1808:2064])
    g_bc = kb.sb("A_g", [128, D], F32)
    sp.dma_start(out=g_bc[:], in_=W["norm_mix_g"][layer].partition_broadcast(128))
    gq = kb.sb("A_gq", [128, 64], F32)
    gk = kb.sb("A_gk", [128, 64], F32)
    gb = kb.sb("A_gb", [128, 16], F32)
    sp.dma_start(out=gq[:], in_=W["q_norm_g"][layer].partition_broadcast(128))
    sp.dma_start(out=gk[:], in_=W["k_norm_g"][layer].partition_broadcast(128))
    sp.dma_start(out=gb[:], in_=W["mlstm_gate_b"][layer].partition_broadcast(128))
    dve.tensor_scalar(out=gq[:], in0=gq[:], scalar1=0.125, scalar2=None, op0=ALU.mult)
    cos_t = kb.sb("A_cos", [128, NT, 32], F32)
    sin_t = kb.sb("A_sin", [128, NT, 32], F32)
    sp.dma_start(out=cos_t[:], in_=C_cos.rearrange("(j p) f -> p j f", p=128))
    sp.dma_start(out=sin_t[:], in_=C_sin.rearrange("(j p) f -> p j f", p=128))

    xb = [kb.sb("A_x%d" % i, [128, 4, D], F32) for i in range(2)]
    hn = [kb.sb("A_hn%d" % i, [128, D], BF16) for i in range(2)]
    junk = kb.sb("A_junk", [128, D], BF16)
    st = [kb.sb("A_st%d" % i, [128, 4], F32) for i in range(2)]
    hnT = [kb.sb("A_hnT%d" % i, [128, 8, 512], BF16) for i in range(2)]
    fst = [kb.sb("A_fst%d" % i, [128, 512], F32) for i in range(2)]
    vo = [kb.sb("A_vo%d" % i, [128, 512], BF16) for i in range(2)]
    g2 = [kb.sb("A_g2%d" % i, [128, 272], F32) for i in range(2)]
    gt = [kb.sb("A_gt%d" % i, [128, 16], F32) for i in range(2)]
    avs = [kb.sb("A_av%d" % i, [128, 128], BF16) for i in range(2)]
    qs = [kb.sb("A_qs%d" % i, [128, 512], F32) for i in range(2)]
    sq = kb.sb("A_sq", [128, 512], F32)
    ss8 = kb.sb("A_ss8", [128, 8], F32)
    tmp = [kb.sb("A_tmp%d" % i, [128, 256], F32) for i in range(4)]
    qr = [kb.sb("A_qr%d" % i, [128, 512], BF16) for i in range(2)]
    kr = [kb.sb("A_kr%d" % i, [128, 2, 2, 64], BF16) for i in range(2)]
    qTb = [kb.sb("A_qTb%d" % i, [128, 4, 512], BF16) for i in range(2)]
    kTb = [kb.sb("A_kTb%d" % i, [128, 2, 512], BF16) for i in range(2)]

    sqk = kb.sb("A_sqk", [128, 128], F32)
    ss8k = kb.sb("A_ss8k", [128, 2], F32)
    tmpk = [kb.sb("A_tmpk%d" % i, [128, 64], F32) for i in range(4)]

    def hnr_a(src, nh, sqb, ssb):
        n = nh * 64
        dve.tensor_tensor(out=sqb[:, 0:n], in0=src, in1=src, op=ALU.mult)
        dve.tensor_reduce(out=ssb[:, 0:nh], in_=sqb[:, 0:n].rearrange("p (h d) -> p h d", d=64), axis=AX.X, op=ALU.add)

    def hnr_s(nh, ssb):
        act.activation(out=ssb[:, 0:nh], in_=ssb[:, 0:nh], func=AF.Sqrt, scale=1.0 / 64, bias=EPS)

    def hnr_b(src, nh, g, j_tile, dst_views, sqb, ssb, tmps):
        n = nh * 64
        dve.reciprocal(out=ssb[:, 0:nh], in_=ssb[:, 0:nh])
        s3 = src.rearrange("p (h d) -> p h d", d=64)
        q3 = sqb[:, 0:n].rearrange("p (h d) -> p h d", d=64)
        dve.tensor_tensor(out=q3, in0=s3, in1=ssb[:, 0:nh].unsqueeze(2).to_broadcast([128, nh, 64]), op=ALU.mult)
        dve.tensor_tensor(out=q3, in0=q3, in1=g[:].unsqueeze(1).to_broadcast([128, nh, 64]), op=ALU.mult)
        q4 = sqb[:, 0:n].rearrange("p (h i two) -> p h i two", h=nh, two=2)
        x0 = q4[:, :, :, 0]
        x1 = q4[:, :, :, 1]
        cb = cos_t[:, j_tile, :].unsqueeze(1).to_broadcast([128, nh, 32])
        sb_ = sin_t[:, j_tile, :].unsqueeze(1).to_broadcast([128, nh, 32])
        m = nh * 32
        tv = [t[:, 0:m].rearrange("p (h i) -> p h i", h=nh) for t in tmps]
        dve.tensor_tensor(out=tv[0], in0=x0, in1=cb, op=ALU.mult)
        dve.tensor_tensor(out=tv[1], in0=x1, in1=sb_, op=ALU.mult)
        dve.tensor_tensor(out=tv[2], in0=x0, in1=sb_, op=ALU.mult)
        dve.tensor_tensor(out=tv[3], in0=x1, in1=cb, op=ALU.mult)
        for dv in dst_views:
            dve.tensor_tensor(out=dv[:, :, :, 0], in0=tv[0], in1=tv[1], op=ALU.subtract)
            dve.tensor_tensor(out=dv[:, :, :, 1], in0=tv[2], in1=tv[3], op=ALU.add)

    def prologue(blk):
        rows = slice(blk * 512, (blk + 1) * 512)
        X = xb[blk % 2]
        HT = hnT[blk % 2]
        sp.dma_start(out=X[:], in_=xs[rows, :].rearrange("(j p) d -> p j d", p=128))
        stt = st[blk % 2]
        for j in range(4):
            act.activation(out=junk[:], in_=X[:, j, :], func=AF.Square, accum_out=stt[:, j:j + 1])
        act.activation(out=stt[:], in_=stt[:], func=AF.Sqrt, scale=1.0 / D, bias=EPS)
        dve.reciprocal(out=stt[:], in_=stt[:])
        for j in range(4):
            H = hn[j % 2]
            dve.scalar_tensor_tensor(out=H[:], in0=X[:, j, :], scalar=stt[:, j:j + 1], in1=g_bc[:], op0=ALU.mult, op1=ALU.mult)
            pt = PS[j % 2]
            pv = bfview(pt)
            for c in range(8):
                pe.transpose(pv[:, c * 128:(c + 1) * 128], H[:, c * 128:(c + 1) * 128], identb[:])
            act.activation(out=HT[:, :, j * 128:(j + 1) * 128], in_=pv.rearrange("p (c t) -> p c t", c=8), func=AF.Copy)

    def mainA(blk):
        rows = slice(blk * 512, (blk + 1) * 512)
        HT = hnT[blk % 2]
        for ch in range(6):
            pt = PS[2 + ch % 2]
            for c in range(8):
                pe.matmul(pt[:], lhsT=w[:, c, ch * 128:(ch + 1) * 128], rhs=HT[:, c, :], start=(c == 0), stop=(c == 7))
            f = fst[ch % 2]
            act.activation(out=f[:], in_=pt[:], func=AF.Copy)
            sp.dma_start(out=featT[ch * 128:(ch + 1) * 128, rows], in_=f[:])
        QT = qTb[blk % 2]
        KT = kTb[blk % 2]

        def part1(j):
            jt = blk * 4 + j
            trow = slice(jt * 128, (jt + 1) * 128)
            p1, p2, p3 = PS[4], PS[5], PS[7]
            for c in range(8):
                pe.matmul(p1[:], lhsT=HT[:, c, j * 128:(j + 1) * 128], rhs=w[:, c, 768:1280], start=(c == 0), stop=(c == 7))
            for c in range(8):
                pe.matmul(p2[:, 0:272], lhsT=HT[:, c, j * 128:(j + 1) * 128], rhs=w[:, c, 1792:2064], start=(c == 0), stop=(c == 7))
            for c in range(8):
                pe.matmul(p3[:], lhsT=HT[:, c, j * 128:(j + 1) * 128], rhs=w[:, c, 1280:1792], start=(c == 0), stop=(c == 7))
            V = vo[j % 2]
            G2 = g2[j % 2]
            Q = qs[j % 2]
            GT = gt[j % 2]
            AV = avs[j % 2]
            KR = kr[j % 2]
            QR = qr[j % 2]
            act.activation(out=G2[:], in_=p2[:, 0:272], func=AF.Copy)
            act.activation(out=Q[:], in_=p3[:], func=AF.Copy)
            act.activation(out=V[:, 0:256], in_=p1[:, 0:256], func=AF.Copy)
            act.activation(out=V[:, 256:512], in_=p1[:, 256:512], func=AF.Sigmoid)
            sp.dma_start(out=mv_d[trow, :], in_=V[:, 0:256])
            sp.dma_start(out=mo_d[trow, :], in_=V[:, 256:512])
            dve.tensor_tensor(out=GT[:], in0=G2[:, 0:16], in1=gb[:], op=ALU.add)
            dve.tensor_copy(out=AV[:], in_=G2[:, 144:272])
            sp.dma_start(out=av_d[trow, :], in_=AV[:])
            hnr_a(G2[:, 16:144], 2, sqk, ss8k)
            hnr_a(Q[:], 8, sq, ss8)
            act.activation(out=GT[:, 8:16], in_=GT[:, 8:16], func=AF.Exp, scale=-1.0)
            act.activation(out=GT[:, 8:16], in_=GT[:, 8:16], func=AF.Ln, bias=1.0)
            hnr_s(2, ss8k)
            hnr_s(8, ss8)
            dve.tensor_scalar(out=GT[:, 8:16], in0=GT[:, 8:16], scalar1=-1.0, scalar2=None, op0=ALU.mult)
            sp.dma_start(out=gates_d[trow, :], in_=GT[:])
            hnr_b(G2[:, 16:144], 2, gk, jt,
                  [KR[:, :, 0, :].rearrange("p h (i two) -> p h i two", two=2),
                   KR[:, :, 1, :].rearrange("p h (i two) -> p h i two", two=2)], sqk, ss8k, tmpk)
            hnr_b(Q[:], 8, gq, jt, [QR[:].rearrange("p (h i two) -> p h i two", h=8, two=2)], sq, ss8, tmp)

        def part2(j):
            KR = kr[j % 2]
            QR = qr[j % 2]
            pv = bfview(PS[6])
            for kv in range(2):
                pe.transpose(pv[:, kv * 128:(kv + 1) * 128], KR[:, kv, :, :].rearrange("p a d -> p (a d)"), identb[:])
            for c in range(4):
                pe.transpose(pv[:, 256 + c * 128:256 + (c + 1) * 128], QR[:, c * 128:(c + 1) * 128], identb[:])
            act.activation(out=KT[:, :, j * 128:(j + 1) * 128], in_=pv[:, 0:256].rearrange("p (c t) -> p c t", c=2), func=AF.Copy)
            act.activation(out=QT[:, :, j * 128:(j + 1) * 128], in_=pv[:, 256:768].rearrange("p (c t) -> p c t", c=4), func=AF.Copy)
        for j in range(4):
            part1(j)
            if j >= 1:
                part2(j - 1)
        part2(3)
        sp.dma_start(out=qT_d[:, :, rows].rearrange("c p t -> p c t"), in_=QT[:])
        sp.dma_start(out=kT_d[:, :, rows].rearrange("c p t -> p c t"), in_=KT[:])

    prologue(0)
    for blk in range(8):
        if blk + 1 < 8:
            prologue(blk + 1)
        mainA(blk)
    kb.end_phase()


def phase_B(kb, nc, layer, W, PS, featT, yT_d, C_invc):
    pe, dve, act, pool, sp = kb.pe, kb.dve, kb.act, kb.pool, kb.sp
    kb.begin_phase()
    PADL = 16
    WID = S + 32
    U = kb.sb("B_U", [128, 2, WID], F32)
    dve.memset(U[:, :, 0:PADL], 0.0)
    dve.memset(U[:, :, PADL + S:WID], 0.0)
    for c in range(2):
        sp.dma_start(out=U[:, c, PADL:PADL + S], in_=featT[c * 128:(c + 1) * 128, :])
    invc = kb.sb("B_invc", [128, 2, S], F32)
    sp.dma_start(out=invc[:], in_=C_invc.rearrange("(c p) t -> p c t", p=128))
    BD = kb.sb("B_BD", [128, 2, 128], BF16)
    dve.memset(BD[:], 0.0)
    for c in range(2):
        pool.dma_start(out=BD[0:64, c, 0:64], in_=W["pool_w"][layer][2 * c])
        pool.dma_start(out=BD[64:128, c, 64:128], in_=W["pool_w"][layer][2 * c + 1])
    psc = kb.sb("B_psc", [128, 2], F32)
    for c in range(2):
        sp.dma_start(out=psc[:, c:c + 1], in_=W["pool_scale"][layer][c * 128:(c + 1) * 128].rearrange("(p o) -> p o", o=1))
    P2a = kb.sb("B_P2a", [128, WID], F32)
    P2b = kb.sb("B_P2b", [128, WID], F32)
    P4b = kb.sb("B_P4b", [128, WID], F32)
    P8b = kb.sb("B_P8b", [128, WID], F32)
    Ss = kb.sb("B_S", [128, 2, S], F32)
    dT = kb.sb("B_dT", [128, 2, S], BF16)

    def rng(t, lo, hi, sh=0):
        return slice(PADL + lo + sh, PADL + hi + sh)
    lo = -8
    u0, u1 = U[:, 0, :], U[:, 1, :]
    pool.tensor_tensor(out=Ss[0:64, 0, :], in0=U[0:64, 0, rng(0, 0, S, -1)], in1=U[0:64, 0, rng(0, 0, S)], op=ALU.add)
    dve.tensor_tensor(out=P2a[64:128, rng(0, lo, S + 12)], in0=U[64:128, 0, rng(0, lo, S + 12)], in1=U[64:128, 0, rng(0, lo, S + 12, 1)], op=ALU.add)
    dve.tensor_tensor(out=Ss[64:128, 0, :], in0=P2a[64:128, rng(0, 0, S, -2)], in1=P2a[64:128, rng(0, 0, S)], op=ALU.add)
    pool.tensor_tensor(out=P2b[:, rng(0, lo, S + 12)], in0=U[:, 1, rng(0, lo, S + 12)], in1=U[:, 1, rng(0, lo, S + 12, 1)], op=ALU.add)
    pool.tensor_tensor(out=P4b[:, rng(0, lo, S + 8)], in0=P2b[:, rng(0, lo, S + 8)], in1=P2b[:, rng(0, lo, S + 8, 2)], op=ALU.add)
    dve.tensor_tensor(out=Ss[0:64, 1, :], in0=P4b[0:64, rng(0, 0, S, -4)], in1=P4b[0:64, rng(0, 0, S)], op=ALU.add)
    pool.tensor_tensor(out=P8b[64:128, rng(0, lo, S + 4)], in0=P4b[64:128, rng(0, lo, S + 4)], in1=P4b[64:128, rng(0, lo, S + 4, 4)], op=ALU.add)
    dve.tensor_tensor(out=Ss[64:128, 1, :], in0=P8b[64:128, rng(0, 0, S, -8)], in1=P8b[64:128, rng(0, 0, S)], op=ALU.add)
    for c in range(2):
        dve.tensor_tensor(out=Ss[:, c, :], in0=Ss[:, c, :], in1=invc[:, c, :], op=ALU.mult)
        dve.tensor_tensor(out=dT[:, c, :], in0=Ss[:, c, :], in1=U[:, c, PADL:PADL + S], op=ALU.subtract)
    yst = [kb.sb("B_y%d" % i, [128, 512], BF16) for i in range(2)]
    k = 0
    for blk in range(8):
        for c in range(2):
            pt = PS[k % 2]
            pe.matmul(pt[:], lhsT=BD[:, c, :], rhs=dT[:, c, blk * 512:(blk + 1) * 512], start=True, stop=True)
            Y = yst[k % 2]
            k += 1
            act.activation(out=Y[:], in_=pt[:], func=AF.Copy, scale=psc[:, c:c + 1])
            sp.dma_start(out=yT_d[c * 128:(c + 1) * 128, blk * 512:(blk + 1) * 512], in_=Y[:])
    kb.end_phase()


def phase_C(kb, nc, layer, W, PS, identb, featT, mv_d, mo_d, gates_d, qk_d, yT_d, C_tri):
    pe, dve, act, pool, sp = kb.pe, kb.dve, kb.act, kb.pool, kb.sp
    kb.begin_phase()
    cw = kb.sb("C_cw", [128, 4, 5], F32)
    sp.dma_start(out=cw[:], in_=W["mlstm_conv_wT"][layer].rearrange("(c p) j -> p c j", p=128))
    Xp = [kb.sb("C_Xp%d" % i, [128, S + 4], F32) for i in range(2)]
    acc = [kb.sb("C_acc%d" % i, [128, S], F32) for i in range(2)]
    sgm = [kb.sb("C_sgm%d" % i, [128, S], F32) for i in range(2)]
    qko = [kb.sb("C_qko%d" % i, [128, S], BF16) for i in range(2)]
    for i in range(2):
        dve.memset(Xp[i][:, 0:2], 0.0)
        dve.memset(Xp[i][:, S + 2:S + 4], 0.0)
    def loadC0(ci):
        sp.dma_start(out=Xp[ci % 2][:, 2:S + 2], in_=featT[256 + ci * 128:256 + (ci + 1) * 128, :])
    loadC0(0)
    for ci in range(4):
        X = Xp[ci % 2]
        A_ = acc[ci % 2]
        G_ = sgm[ci % 2]
        O_ = qko[ci % 2]
        if ci + 1 < 4:
            loadC0(ci + 1)
        dve.tensor_scalar(out=A_[:], in0=X[:, 0:S], scalar1=cw[:, ci, 0:1], scalar2=None, op0=ALU.mult)
        for j in range(1, 5):
            dve.scalar_tensor_tensor(out=A_[:], in0=X[:, j:j + S], scalar=cw[:, ci, j:j + 1], in1=A_[:], op0=ALU.mult, op1=ALU.add)
        act.activation(out=G_[:], in_=A_[:], func=AF.Sigmoid)
        dve.scalar_tensor_tensor(out=O_[:], in0=A_[:], scalar=(1.0 if ci < 2 else 0.125), in1=G_[:], op0=ALU.mult, op1=ALU.mult)
        sp.dma_start(out=qk_d[ci], in_=O_[:])
    kb.end_phase()
    import os
    CSTOP = int(os.environ.get("CSTOP", "9"))
    if CSTOP <= 0:
        return
    kb.begin_phase()
    QK = kb.sb("C_QK", [128, 4, S], BF16)
    sp.dma_start(out=QK[:], in_=qk_d.rearrange("c p t -> p c t"))
    TRI = kb.sb("C_TRI", [128, 2, 128], F32)
    sp.dma_start(out=TRI[:], in_=C_tri.rearrange("p (a j) -> p a j", a=2))
    ones = kb.sb("C_ones", [128, 128], F32)
    dve.memset(ones[:], 1.0)
    G = kb.sb("C_G", [128, NT, 16], F32)
    sp.dma_start(out=G[:], in_=gates_d.rearrange("(c p) n -> p c n", p=128))
    ybf = kb.sb("C_ybf", [128, NT, 256], BF16)
    sp.dma_start(out=ybf[:], in_=mv_d.rearrange("(c p) n -> p c n", p=128))
    Va = kb.sb("C_Va", [128, NT, 4, 65], BF16)
    dve.memset(Va[:, :, :, 64:65], 1.0)
    dve.tensor_copy(out=Va[:, :, :, 0:64], in_=ybf[:].rearrange("p c (h d) -> p c h d", h=4))
    Kt = kb.sb("C_Kt", [128, NT, 256], BF16)
    for cg in range(8):
        pv = bfview(PS[cg % 2])
        for cl in range(4):
            c = cg * 4 + cl
            for hp in range(2):
                pe.transpose(pv[:, (cl * 2 + hp) * 128:(cl * 2 + hp + 1) * 128], QK[:, 2 + hp, c * 128:(c + 1) * 128], identb[:])
        act.activation(out=Kt[:, cg * 4:(cg + 1) * 4, :], in_=pv.rearrange("p (c n) -> p c n", c=4), func=AF.Copy)
    A = [kb.sb("C_A%d" % i, [128, NT, 4], F32) for i in range(2)]
    A2 = [kb.sb("C_A2%d" % i, [128, NT, 4], F32) for i in range(2)]
    Bc = [kb.sb("C_B%d" % i, [128, NT, 4], F32) for i in range(2)]
    Fc = [kb.sb("C_F%d" % i, [128, NT, 4], F32) for i in range(2)]
    at = kb.sb("C_at", [128, NT, 4], F32)
    lfc = kb.sb("C_lfc", [128, 2, NT * 4], F32)
    for d_ in range(2):
        dve.tensor_copy(out=lfc[:, d_, :].rearrange("p (c h) -> p c h", h=4), in_=G[:, :, 8 + 4 * d_:12 + 4 * d_])
    for d_ in range(2):
        lfv = lfc[:, d_, :]
        liv = G[:, :, 4 * d_:4 * d_ + 4]
        pc, ptot = PS[6], PS[7]
        pe.matmul(pc[:, 0:128], lhsT=TRI[:, d_, :], rhs=lfv, start=True, stop=True)
        pe.matmul(ptot[:, 0:128], lhsT=ones[:], rhs=lfv, start=True, stop=True)
        pc3 = pc[:, 0:128].rearrange("p (c h) -> p c h", h=4)
        pt3 = ptot[:, 0:128].rearrange("p (c h) -> p c h", h=4)
        dve.tensor_tensor(out=at[:], in0=liv, in1=pc3, op=ALU.subtract)
        act.activation(out=A[d_][:], in_=at[:], func=AF.Exp)
        dve.tensor_tensor(out=at[:], in0=at[:], in1=pt3, op=ALU.add)
        act.activation(out=A2[d_][:], in_=at[:], func=AF.Exp)
        act.activation(out=Bc[d_][:], in_=pc3, func=AF.Exp)
        act.activation(out=Fc[d_][:], in_=pt3, func=AF.Exp)
    if CSTOP <= 1:
        kb.end_phase()
        return
    Hd = [kb.sb("C_H%d" % i, [128, NT, 256], F32) for i in range(2)]
    Zf = [kb.sb("C_Zf%d" % i, [128, 4, 65], F32) for i in range(2)]
    Zb = [[kb.sb("C_Zb%d_%d" % (i, j), [128, 4, 65], BF16) for j in range(2)] for i in range(2)]
    for d_ in range(2):
        dve.memset(Zf[d_][:], 0.0)
        dve.memset(Zb[d_][0][:], 0.0)
    VWr = [kb.sb("C_VW%d" % i, [128, 2, 4, 65], BF16) for i in range(4)]
    SMr = [kb.sb("C_SM%d" % i, [128, 2, 2, 128], BF16) for i in range(4)]
    t4r = [kb.sb("C_t4%d" % i, [128, 4], F32) for i in range(4)]
    t4n = [kb.sb("C_t4n%d" % i, [128, 4], F32) for i in range(2)]
    for it in range(NT):
        cc = [it, NT - 1 - it]
        VWs = [VWr[(2 * it + d_) % 4] for d_ in range(2)]
        SMs = [SMr[(2 * it + d_) % 4] for d_ in range(2)]
        t4s = [t4r[(2 * it + d_) % 4] for d_ in range(2)]
        pss = [[PS[0], PS[1]], [PS[2], PS[3]]]
        psn = [PS[4], PS[5]]
        psu = [PS[6], PS[7]]
        for d_ in range(2):
            c = cc[d_]
            VW = VWs[d_]
            pool.tensor_tensor(out=VW[:, 0], in0=Va[:, c], in1=A[d_][:, c, :].unsqueeze(2).to_broadcast([128, 4, 65]), op=ALU.mult)
            pool.tensor_tensor(out=VW[:, 1], in0=Va[:, c], in1=A2[d_][:, c, :].unsqueeze(2).to_broadcast([128, 4, 65]), op=ALU.mult)
        for d_ in range(2):
            c = cc[d_]
            cs = slice(c * 128, (c + 1) * 128)
            for h in range(4):
                pb = (h % 2) * 64
                pe.matmul(pss[d_][h % 2][:, (h // 2) * 128:(h // 2 + 1) * 128], lhsT=QK[pb:pb + 64, 2 + h // 2, cs], rhs=QK[pb:pb + 64, h // 2, cs], start=True, stop=True)
        for d_ in range(2):
            for hh in range(2):
                dve.tensor_tensor(out=SMs[d_][:, hh], in0=pss[d_][hh][:, 0:256].rearrange("p (h j) -> p h j", h=2),
                                  in1=TRI[:, d_, :].unsqueeze(1).to_broadcast([128, 2, 128]), op=ALU.mult)
        for d_ in range(2):
            c = cc[d_]
            cs = slice(c * 128, (c + 1) * 128)
            Zc = Zb[d_][it % 2]
            for h in range(4):
                pe.matmul(psn[d_][:, h * 65:(h + 1) * 65], lhsT=SMs[d_][:, h % 2, h // 2, :], rhs=VWs[d_][:, 0, h, :], start=True, stop=False)
                pe.matmul(psn[d_][:, h * 65:(h + 1) * 65], lhsT=QK[:, h // 2, cs], rhs=Zc[:, h, :], start=False, stop=True)
            for hp in range(2):
                pe.matmul(psu[d_][:, hp * 130:(hp + 1) * 130], lhsT=Kt[:, c, hp * 128:(hp + 1) * 128],
                          rhs=VWs[d_][:, 1, 2 * hp:2 * hp + 2, :].rearrange("p a b -> p (a b)"), start=True, stop=True)
        for d_ in range(2):
            c = cc[d_]
            for hp in range(2):
                for hh in range(2):
                    rows = slice(hh * 64, (hh + 1) * 64)
                    h = 2 * hp + hh
                    dve.scalar_tensor_tensor(out=Zf[d_][rows, h, :], in0=Zf[d_][rows, h, :], scalar=Fc[d_][rows, c, h:h + 1],
                                             in1=psu[d_][rows, hp * 130 + hh * 65:hp * 130 + (hh + 1) * 65], op0=ALU.mult, op1=ALU.add)
            dve.tensor_copy(out=Zb[d_][(it + 1) % 2][:], in_=Zf[d_][:])
        n3s = [psn[d_][:, 0:260].rearrange("p (h e) -> p h e", h=4) for d_ in range(2)]
        for d_ in range(2):
            dve.tensor_tensor(out=t4s[d_][:], in0=n3s[d_][:, :, 64], in1=Bc[d_][:, cc[d_], :], op=ALU.mult)
        for d_ in range(2):
            dve.tensor_scalar(out=t4n[d_][:], in0=t4s[d_][:], scalar1=-1.0, scalar2=1.0, op0=ALU.mult, op1=ALU.max)
            dve.tensor_scalar(out=t4s[d_][:], in0=t4s[d_][:], scalar1=1.0, scalar2=None, op0=ALU.max)
            dve.tensor_tensor(out=t4s[d_][:], in0=t4s[d_][:], in1=t4n[d_][:], op=ALU.max)
            dve.reciprocal(out=t4s[d_][:], in_=t4s[d_][:])
            dve.tensor_tensor(out=t4s[d_][:], in0=t4s[d_][:], in1=Bc[d_][:, cc[d_], :], op=ALU.mult)
            dve.tensor_tensor(out=Hd[d_][:, cc[d_], :].rearrange("p (h e) -> p h e", h=4), in0=n3s[d_][:, :, 0:64],
                              in1=t4s[d_][:].unsqueeze(2).to_broadcast([128, 4, 64]), op=ALU.mult)
    if CSTOP <= 2:
        kb.end_phase()
        return
    H0, H1 = Hd
    ng = kb.sb("C_ng", [128, 256], F32)
    sp.dma_start(out=ng[:], in_=W["mlstm_norm_g"][layer].partition_broadcast(128))
    mo = kb.sb("C_mo", [128, NT, 256], BF16)
    sp.dma_start(out=mo[:], in_=mo_d.rearrange("(c p) n -> p c n", p=128))
    ss = kb.sb("C_ss", [128, NT, 4], F32)
    for half in range(2):
        cs = slice(half * 16, (half + 1) * 16)
        dve.tensor_tensor(out=H0[:, cs, :], in0=H0[:, cs, :], in1=H1[:, cs, :], op=ALU.add)
        pool.tensor_tensor(out=H1[:, cs, :], in0=H0[:, cs, :], in1=H0[:, cs, :], op=ALU.mult)
        dve.tensor_reduce(out=ss[:, cs, :], in_=H1[:, cs, :].rearrange("p c (h e) -> p c h e", h=4), axis=AX.X, op=ALU.add)
    act.activation(out=ss[:], in_=ss[:], func=AF.Sqrt, scale=1.0 / 64, bias=EPS)
    dve.reciprocal(out=ss[:], in_=ss[:])
    for half in range(2):
        cs = slice(half * 16, (half + 1) * 16)
        dve.tensor_tensor(out=H0[:, cs, :].rearrange("p c (h e) -> p c h e", h=4), in0=H0[:, cs, :].rearrange("p c (h e) -> p c h e", h=4),
                          in1=ss[:, cs, :].unsqueeze(3).to_broadcast([128, 16, 4, 64]), op=ALU.mult)
        pool.tensor_tensor(out=H0[:, cs, :], in0=H0[:, cs, :], in1=ng[:].unsqueeze(1).to_broadcast([128, 16, 256]), op=ALU.mult)
        dve.tensor_tensor(out=ybf[:, cs, :], in0=H0[:, cs, :], in1=mo[:, cs, :], op=ALU.mult)
    yst = [kb.sb("C_yst%d" % i, [128, 1024], BF16) for i in range(2)]
    k = 0
    for hp in range(2):
        for cg in range(4):
            pv = bfview(PS[k % 2])
            Y = yst[k % 2]
            k += 1
            for cl in range(8):
                c = cg * 8 + cl
                pe.transpose(pv[:, cl * 128:(cl + 1) * 128], ybf[:, c, hp * 128:(hp + 1) * 128], identb[:])
            act.activation(out=Y[:], in_=pv, func=AF.Copy)
            sp.dma_start(out=yT_d[256 + hp * 128:256 + (hp + 1) * 128, cg * 1024:(cg + 1) * 1024], in_=Y[:])
    kb.end_phase()


def phase_D(kb, nc, layer, PS, qT_d, kT_d, av_d, yT_d):
    pe, dve, act, pool, sp = kb.pe, kb.dve, kb.act, kb.pool, kb.sp
    kb.begin_phase()
    kT2 = kb.sb("D_kT", [128, 2, S], BF16)
    sp.dma_start(out=kT2[:], in_=kT_d.rearrange("c p t -> p c t"))
    Va = kb.sb("D_Va", [128, NT, 2, 128], BF16)
    dve.memset(Va[:, :, :, 64:128], 1.0)
    Vst = kb.sb("D_Vst", [128, NT, 128], BF16)
    sp.dma_start(out=Vst[:], in_=av_d.rearrange("(c p) n -> p c n", p=128))
    dve.tensor_copy(out=Va[:, :, :, 0:64], in_=Vst[:].rearrange("p c (h d) -> p c h d", h=2))
    Qe = [kb.sb("D_Qe%d" % i, [128, S], BF16) for i in range(2)]
    Qo = [kb.sb("D_Qo%d" % i, [128, S], BF16) for i in range(2)]
    for i in range(2):
        pool.memset(Qe[i][64:128, :], 0.0)
        pool.memset(Qo[i][0:64, :], 0.0)

    def load_q(pr):
        sp.dma_start(out=Qe[pr % 2][0:64, :], in_=qT_d[pr, 0:64, :])
        sp.dma_start(out=Qo[pr % 2][64:128, :], in_=qT_d[pr, 64:128, :])
    P = [kb.sb("D_P%d" % i, [128, 512], BF16) for i in range(4)]
    rd = [kb.sb("D_rd%d" % i, [64, 512], F32) for i in range(2)]
    yo = [kb.sb("D_yo%d" % i, [64, 512], BF16) for i in range(2)]
    SB = PS[0:4]
    OB = PS[4:6]
    steps = []
    for pr in range(4):
        for hh in range(2):
            for qb in range(8):
                for kc in range(NT):
                    steps.append((pr, hh, qb, kc))
    load_q(0)

    def issue_S(i):
        pr, hh, qb, kc = steps[i]
        kv = pr // 2
        if hh == 0 and qb == 0 and kc == 0 and pr + 1 < 4:
            load_q(pr + 1)
        Qt = (Qe if hh == 0 else Qo)[pr % 2]
        pe.matmul(SB[i % 4][:], lhsT=kT2[:, kv, kc * 128:(kc + 1) * 128],
                  rhs=Qt[:, qb * 512:(qb + 1) * 512], start=True, stop=True)
    LOOK = 2
    for i in range(min(LOOK, len(steps))):
        issue_S(i)
    ob_i = 0
    for i, (pr, hh, qb, kc) in enumerate(steps):
        if i + LOOK < len(steps):
            issue_S(i + LOOK)
        kv = pr // 2
        Pt = P[i % 4]
        act.activation(out=Pt[:], in_=SB[i % 4][:], func=AF.Exp)
        O = OB[ob_i % 2]
        pe.matmul(O[:], lhsT=Va[:, kc, kv, :], rhs=Pt[:], start=(kc == 0), stop=(kc == NT - 1))
        if kc == NT - 1:
            h = 2 * pr + hh
            R = rd[ob_i % 2]
            Y = yo[ob_i % 2]
            dve.reciprocal(out=R[:], in_=O[64:128, :])
            dve.tensor_tensor(out=Y[:], in0=O[0:64, :], in1=R[:], op=ALU.mult)
            sp.dma_start(out=yT_d[512 + h * 64:512 + (h + 1) * 64, qb * 512:(qb + 1) * 512], in_=Y[:])
            ob_i += 1
    kb.end_phase()


def phase_EF(kb, nc, layer, W, PS, identb, xsrc, xs, yT_d, mem_in, efw, g1args=None):
    pe, dve, act, pool, sp = kb.pe, kb.dve, kb.act, kb.pool, kb.sp
    kb.begin_phase()
    wo, wq, wkv, cwo = efw["wo"], efw["wq"], efw["wkv"], efw["cwo"]
    g1_block = g1_finish = None
    if g1args is not None:
        g1_block, g1_finish = make_g1(kb, nc, layer, W, PS, g1args["identf"], g1args["hn_d"], g1args["aff_d"])
    g_bc = kb.sb("E_g", [128, D], F32)
    sp.dma_start(out=g_bc[:], in_=W["norm_mem_g"][layer].partition_broadcast(128))
    ones = kb.sb("E_ones", [128, 128], BF16)
    dve.memset(ones[:], 1.0)
    memb = kb.sb("E_memb", [128, 2, D], BF16)
    for mc in range(2):
        pool.dma_start(out=memb[:, mc, :], in_=mem_in[mc * 128:(mc + 1) * 128, :])
    memT = kb.sb("E_memT", [128, 8, 256], BF16)
    for mc in range(2):
        pv = bfview(PS[mc])
        for c in range(8):
            pe.transpose(pv[:, c * 128:(c + 1) * 128], memb[:, mc, c * 128:(c + 1) * 128], identb[:])
        act.activation(out=memT[:, :, mc * 128:(mc + 1) * 128], in_=pv.rearrange("p (c t) -> p c t", c=8), func=AF.Copy)
    kcT = kb.sb("E_kcT", [128, 4, 256], BF16)
    for h in range(4):
        pt = PS[2 + h % 2]
        for c in range(8):
            pe.matmul(pt[:, 0:256], lhsT=wkv[:, c, h * 128:(h + 1) * 128], rhs=memT[:, c, :], start=(c == 0), stop=(c == 7))
        act.activation(out=kcT[:, h, :], in_=pt[:, 0:256], func=AF.Copy)
    Vc = kb.sb("E_Vc", [128, 2, 512], BF16)
    for mc in range(2):
        pt = PS[4 + mc]
        for c in range(8):
            pe.matmul(pt[:], lhsT=memT[:, c, mc * 128:(mc + 1) * 128], rhs=wkv[:, c, 512:1024], start=(c == 0), stop=(c == 7))
        act.activation(out=Vc[:, mc, :], in_=pt[:], func=AF.Copy)

    xb = [kb.sb("E_x%d" % i, [128, 4, D], F32) for i in range(2)]
    Yb = [kb.sb("E_Y%d" % i, [128, 8, 512], BF16) for i in range(2)]
    hn = [kb.sb("E_hn%d" % i, [128, D], BF16) for i in range(2)]
    junk = kb.sb("E_junk", [128, D], BF16)
    st = [kb.sb("E_st%d" % i, [128, 4], F32) for i in range(2)]
    hnT = [kb.sb("E_hnT%d" % i, [128, 8, 512], BF16) for i in range(2)]
    qcT = [kb.sb("E_qcT%d" % i, [128, 4, 512], BF16) for i in range(2)]
    PT = [kb.sb("E_PT%d" % i, [128, 2, 512], BF16) for i in range(2)]
    rden = [kb.sb("E_rden%d" % i, [128, 512], F32) for i in range(2)]
    oT = [kb.sb("E_oT%d" % i, [128, 4, 512], BF16) for i in range(2)]
    SC = float(128 ** -0.5)
    def loadE(blk):
        rows_ = slice(blk * 512, (blk + 1) * 512)
        sp.dma_start(out=xb[blk % 2][:], in_=xsrc[rows_, :].rearrange("(j p) d -> p j d", p=128))
        sp.dma_start(out=Yb[blk % 2][:], in_=yT_d[:, rows_].rearrange("(c p) t -> p c t", p=128))

    def stage1(blk):
        X = xb[blk % 2]
        Y = Yb[blk % 2]
        HT = hnT[blk % 2]
        stt = st[blk % 2]
        for j in range(4):
            for half in range(2):
                pt = PS[half]
                for c in range(8):
                    pe.matmul(pt[:], lhsT=Y[:, c, j * 128:(j + 1) * 128], rhs=wo[:, c, half * 512:(half + 1) * 512], start=(c == 0), stop=(c == 7))
                dve.tensor_tensor(out=X[:, j, half * 512:(half + 1) * 512], in0=X[:, j, half * 512:(half + 1) * 512], in1=pt[:], op=ALU.add)
            act.activation(out=junk[:], in_=X[:, j, :], func=AF.Square, accum_out=stt[:, j:j + 1])
        act.activation(out=stt[:], in_=stt[:], func=AF.Sqrt, scale=1.0 / D, bias=EPS)
        dve.reciprocal(out=stt[:], in_=stt[:])
        for j in range(4):
            H = hn[j % 2]
            dve.scalar_tensor_tensor(out=H[:], in0=X[:, j, :], scalar=stt[:, j:j + 1], in1=g_bc[:], op0=ALU.mult, op1=ALU.mult)
            pt = PS[2 + j % 2]
            pv = bfview(pt)
            for c in range(8):
                pe.transpose(pv[:, c * 128:(c + 1) * 128], H[:, c * 128:(c + 1) * 128], identb[:])
            act.activation(out=HT[:, :, j * 128:(j + 1) * 128], in_=pv.rearrange("p (c t) -> p c t", c=8), func=AF.Copy)

    def stage2(blk):
        rows = slice(blk * 512, (blk + 1) * 512)
        X = xb[blk % 2]
        HT = hnT[blk % 2]
        QC = qcT[blk % 2]
        for h in range(4):
            pt = PS[4 + h % 2]
            for c in range(8):
                pe.matmul(pt[:], lhsT=wq[:, c, h * 128:(h + 1) * 128], rhs=HT[:, c, :], start=(c == 0), stop=(c == 7))
            act.activation(out=QC[:, h, :], in_=pt[:], func=AF.Copy)
        OT = oT[blk % 2]
        for h in range(4):
            Pt = PT[h % 2]
            for mc in range(2):
                pt = PS[6 + mc]
                pe.matmul(pt[:], lhsT=kcT[:, h, mc * 128:(mc + 1) * 128], rhs=QC[:, h, :], start=True, stop=True)
                act.activation(out=Pt[:, mc, :], in_=pt[:], func=AF.Exp, scale=SC)
            po = PS[4]
            pd = PS[5]
            for mc in range(2):
                pe.matmul(po[:], lhsT=Vc[:, mc, h * 128:(h + 1) * 128], rhs=Pt[:, mc, :], start=(mc == 0), stop=(mc == 1))
            for mc in range(2):
                pe.matmul(pd[:], lhsT=ones[:], rhs=Pt[:, mc, :], start=(mc == 0), stop=(mc == 1))
            R = rden[h % 2]
            dve.reciprocal(out=R[:], in_=pd[:])
            dve.tensor_tensor(out=OT[:, h, :], in0=po[:], in1=R[:], op=ALU.mult)
        for j in range(4):
            for half in range(2):
                pt = PS[6 + half]
                for c in range(4):
                    pe.matmul(pt[:], lhsT=OT[:, c, j * 128:(j + 1) * 128], rhs=cwo[:, c, half * 512:(half + 1) * 512], start=(c == 0), stop=(c == 3))
                dve.tensor_tensor(out=X[:, j, half * 512:(half + 1) * 512], in0=X[:, j, half * 512:(half + 1) * 512], in1=pt[:], op=ALU.add)
        sp.dma_start(out=xs[rows, :].rearrange("(j p) d -> p j d", p=128), in_=X[:])
        if g1_block is not None:
            g1_block(X, blk)
    loadE(0)
    loadE(1)
    stage1(0)
    for blk in range(8):
        if blk + 1 < 8:
            stage1(blk + 1)
        stage2(blk)
        if blk + 2 < 8:
            loadE(blk + 2)
    if g1_finish is not None:
        g1_finish()
    kb.end_phase()


def make_g1(kb, nc, layer, W, PS, identf, hn_d, aff_d):
    pe, dve, act, pool, sp = kb.pe, kb.dve, kb.act, kb.pool, kb.sp
    g_bc = kb.sb("G_g", [128, D], F32)
    sp.dma_start(out=g_bc[:], in_=W["norm_ffn_g"][layer].partition_broadcast(128))
    rw = kb.sb("G_rw", [128, 8, NEXP], F32)
    sp.dma_start(out=rw[:], in_=W["router_w"][layer].rearrange("(c p) e -> p c e", p=128))
    Hf = [kb.sb("G_Hf%d" % i, [128, D], F32) for i in range(2)]
    Hb = [kb.sb("G_Hb%d" % i, [128, D], BF16) for i in range(2)]
    HT = [kb.sb("G_HT%d" % i, [128, 8, 128], F32) for i in range(2)]
    junk = kb.sb("G_junk", [128, D], BF16)
    st = [kb.sb("G_st%d" % i, [128, 4], F32) for i in range(2)]
    LG = kb.sb("G_LG", [128, NT, NEXP], F32)
    mx = kb.sb("G_mx", [128, NT], F32)
    wst = [kb.sb("G_wst%d" % i, [NEXP, 512], F32) for i in range(2)]

    def block_fn(X, blk):
        stt = st[blk % 2]
        for j in range(4):
            act.activation(out=junk[:], in_=X[:, j, :], func=AF.Square, accum_out=stt[:, j:j + 1])
        act.activation(out=stt[:], in_=stt[:], func=AF.Sqrt, scale=1.0 / D, bias=EPS)
        dve.reciprocal(out=stt[:], in_=stt[:])
        for j in range(4):
            jt = blk * 4 + j
            H = Hf[j % 2]
            dve.scalar_tensor_tensor(out=H[:], in0=X[:, j, :], scalar=stt[:, j:j + 1], in1=g_bc[:], op0=ALU.mult, op1=ALU.mult)
            B_ = Hb[j % 2]
            act.activation(out=B_[:], in_=H[:], func=AF.Copy)
            sp.dma_start(out=hn_d[jt * 128:(jt + 1) * 128, :], in_=B_[:])
            T = HT[j % 2]
            for half in range(2):
                pt = PS[half]
                for c in range(4):
                    cc = half * 4 + c
                    pe.transpose(pt[:, c * 128:(c + 1) * 128], H[:, cc * 128:(cc + 1) * 128], identf[:])
                act.activation(out=T[:, half * 4:(half + 1) * 4, :], in_=pt[:].rearrange("p (c t) -> p c t", c=4), func=AF.Copy)
            pl = PS[2 + j % 2]
            for c in range(8):
                pe.matmul(pl[:, 0:NEXP], lhsT=T[:, c, :], rhs=rw[:, c, :], start=(c == 0), stop=(c == 7))
            dve.tensor_copy(out=LG[:, jt, :], in_=pl[:, 0:NEXP])

    def finish_fn():
        dve.tensor_reduce(out=mx[:], in_=LG[:], axis=AX.X, op=ALU.max)
        dve.tensor_tensor(out=LG[:], in0=LG[:], in1=mx[:].unsqueeze(2).to_broadcast([128, NT, NEXP]), op=ALU.subtract)
        act.activation(out=LG[:], in_=LG[:], func=AF.Exp)
        dve.tensor_reduce(out=mx[:], in_=LG[:], axis=AX.X, op=ALU.add)
        dve.reciprocal(out=mx[:], in_=mx[:])
        dve.tensor_tensor(out=LG[:], in0=LG[:], in1=mx[:].unsqueeze(2).to_broadcast([128, NT, NEXP]), op=ALU.mult)
        for blk in range(8):
            pt = PS[4 + blk % 2]
            for j in range(4):
                jt = blk * 4 + j
                pe.transpose(pt[0:NEXP, j * 128:(j + 1) * 128], LG[:, jt, :], identf[:])
            wt = wst[blk % 2]
            act.activation(out=wt[:], in_=pt[0:NEXP, :], func=AF.Copy)
            sp.dma_start(out=aff_d[:, blk * 512:(blk + 1) * 512], in_=wt[:])
    return block_fn, finish_fn


def phase_G(kb, nc, layer, W, PS, identb, identf, xs, hn_d, idxT, gT, aff_d):
    pe, dve, act, pool, sp = kb.pe, kb.dve, kb.act, kb.pool, kb.sp
    kb.begin_phase()
    ws = [kb.sb("G_w%d" % i, [128, 8, 1024], BF16) for i in range(6)]
    wgu = W["expert_w_gu"][layer]
    wdn = W["expert_w_down"][layer]

    def load_gu(e, p):
        half, g = p // 2, p % 2
        pool.dma_start(out=ws[p][:], in_=wgu[e][:, g * DFF + half * 1024:g * DFF + (half + 1) * 1024].rearrange("(c p) f -> p c f", p=128))

    def load_wd(e, p):
        pool.dma_start(out=ws[4 + p][:], in_=wdn[e][p * 1024:(p + 1) * 1024, :].rearrange("(c p) n -> p c n", p=128))
    for p in range(4):
        load_gu(0, p)
    for p in range(2):
        load_wd(0, p)
    work = kb.sb("G_work", [NEXP, S], F32)
    sp.dma_start(out=work[:], in_=aff_d[:, :])
    top = kb.sb("G_top", [NEXP, CAP], F32)
    idx = kb.sb("G_idx", [NEXP, CAP], U32)
    for r in range(CAP // 8):
        sl = slice(r * 8, (r + 1) * 8)
        dve.max(out=top[:, sl], in_=work[:])
        dve.max_index(out=idx[:, sl], in_max=top[:, sl], in_values=work[:])
        dve.match_replace(out=work[:], in_to_replace=top[:, sl], in_values=work[:], imm_value=-1.0)
    idxf = kb.sb("G_idxf", [NEXP, CAP], F32)
    dve.tensor_copy(out=idxf[:], in_=idx[:])
    pt = PS[6]
    for ct in range(4):
        pe.transpose(pt[:, ct * NEXP:(ct + 1) * NEXP], idxf[:, ct * 128:(ct + 1) * 128], identf[0:NEXP, 0:NEXP])
    dve.tensor_copy(out=idxT[:], in_=pt[:, 0:4 * NEXP].rearrange("p (c e) -> p c e", c=4))
    pt = PS[7]
    for ct in range(4):
        pe.transpose(pt[:, ct * NEXP:(ct + 1) * NEXP], top[:, ct * 128:(ct + 1) * 128], identf[0:NEXP, 0:NEXP])
    dve.tensor_copy(out=gT[:], in_=pt[:, 0:4 * NEXP].rearrange("p (c e) -> p c e", c=4))
    if DBG.get("idx") is not None:
        sp.dma_start(out=DBG["idx"][:, :], in_=idxT[:].rearrange("p c e -> p (c e)"))
        sp.dma_start(out=DBG["g"][:, :], in_=gT[:].rearrange("p c e -> p (c e)"))
    Xe = [kb.sb("G_Xe%d" % i, [128, 4, D], BF16) for i in range(2)]
    XeT = [kb.sb("G_XeT%d" % i, [128, 8, CAP], BF16) for i in range(2)]
    hT = kb.sb("G_hT", [128, 16, CAP], BF16)
    sg = [kb.sb("G_sg%d" % i, [128, CAP], F32) for i in range(2)]
    yg = [kb.sb("G_yg%d" % i, [128, D], F32) for i in range(2)]
    xs_tok = kb.token("xs_tok")
    def gather(e):
        X = Xe[e % 2]
        for ct in range(4):
            pool.indirect_dma_start(out=X[:, ct, :], out_offset=None, in_=hn_d[:, :],
                                    in_offset=bass.IndirectOffsetOnAxis(ap=idxT[:, ct, e:e + 1].ap, axis=0),
                                    _reads=[idxT])
        return X
    yi = 0
    Xn = gather(0)
    for e in range(NEXP):
        X = Xn
        if e + 1 < NEXP:
            Xn = gather(e + 1)
        XT = XeT[e % 2]
        for ct in range(4):
            pv = bfview(PS[ct % 2])
            for c in range(8):
                pe.transpose(pv[:, c * 128:(c + 1) * 128], X[:, ct, c * 128:(c + 1) * 128], identb[:])
            act.activation(out=XT[:, :, ct * 128:(ct + 1) * 128], in_=pv.rearrange("p (c t) -> p c t", c=8), func=AF.Copy)
        for fj in range(16):
            half, fl = fj // 8, fj % 8
            wga, wup = ws[2 * half], ws[2 * half + 1]
            pa = PS[2 + (fj % 2) * 2]
            pb_ = PS[3 + (fj % 2) * 2]
            for c in range(8):
                pe.matmul(pa[:], lhsT=wga[:, c, fl * 128:(fl + 1) * 128], rhs=XT[:, c, :], start=(c == 0), stop=(c == 7))
            for c in range(8):
                pe.matmul(pb_[:], lhsT=wup[:, c, fl * 128:(fl + 1) * 128], rhs=XT[:, c, :], start=(c == 0), stop=(c == 7))
            sgt = sg[fj % 2]
            act.activation(out=sgt[:], in_=pa[:], func=AF.Sigmoid)
            dve.tensor_tensor(out=sgt[:], in0=sgt[:], in1=pa[:], op=ALU.mult)
            dve.tensor_tensor(out=hT[:, fj, :], in0=sgt[:], in1=pb_[:], op=ALU.mult)
            if fl == 7 and e + 1 < NEXP:
                load_gu(e + 1, 2 * half)
                load_gu(e + 1, 2 * half + 1)
        if e == 0 and DBG.get("X") is not None:
            sp.dma_start(out=DBG["X"][:, :], in_=X[:].rearrange("p c d -> p (c d)"))
            sp.dma_start(out=DBG["hT"][:, :], in_=hT[:].rearrange("p c d -> p (c d)"))
        for ct in range(4):
            Y = yg[yi % 2]
            yi += 1
            for half in range(2):
                pt = PS[6 + half]
                for fj in range(16):
                    pe.matmul(pt[:], lhsT=hT[:, fj, ct * 128:(ct + 1) * 128], rhs=ws[4 + fj // 8][:, fj % 8, half * 512:(half + 1) * 512],
                              start=(fj == 0), stop=(fj == 15))
                dve.tensor_scalar(out=Y[:, half * 512:(half + 1) * 512], in0=pt[:], scalar1=gT[:, ct, e:e + 1], scalar2=None, op0=ALU.mult)
            if e == 0 and ct == 0 and DBG.get("yg") is not None:
                sp.dma_start(out=DBG["yg"][:, :], in_=Y[:])
            pool.indirect_dma_start(out=xs[:, :], out_offset=bass.IndirectOffsetOnAxis(ap=idxT[:, ct, e:e + 1].ap, axis=0),
                                    in_=Y[:], in_offset=None, compute_op=ALU.add,
                                    _reads=[idxT], _writes=[xs_tok])
        if e + 1 < NEXP:
            load_wd(e + 1, 0)
            load_wd(e + 1, 1)
    kb.end_phase()


def rope_tables():
    t = np.arange(S)
    row = (t // 64).astype(np.float32)
    col = (t % 64).astype(np.float32)
    inv = (10000.0 ** (-(np.arange(16, dtype=np.float32)) / 16)).astype(np.float32)
    ang = np.concatenate([row[:, None] * inv, col[:, None] * inv], axis=-1).astype(np.float32)
    return np.cos(ang).astype(np.float32), np.sin(ang).astype(np.float32)


def pool_invcount():
    t = np.arange(S)
    tab = np.zeros((256, S), np.float32)
    for g, win in enumerate((2, 4, 8, 16)):
        lo = np.clip(t - win // 2, 0, S)
        hi = np.clip(t + win // 2, 0, S)
        tab[g * 64:(g + 1) * 64, :] = (1.0 / (hi - lo).astype(np.float32))[None, :]
    return tab


def consts():
    c, s = rope_tables()
    tri = np.zeros((128, 2, 128), np.float32)
    si = np.arange(128)[:, None]
    ji = np.arange(128)[None, :]
    tri[:, 0, :] = (si <= ji)
    tri[:, 1, :] = (si >= ji)
    return {"c_cos": c, "c_sin": s, "c_identf": np.eye(128, dtype=np.float32), "c_invc": pool_invcount(),
            "c_tri": tri.reshape(128, 256)}


_NC_CACHE = {}


def kernel(**inputs):
    if "nc" not in _NC_CACHE:
        _NC_CACHE["nc"] = build()
    nc = _NC_CACHE["nc"]
    cst = consts()
    shared = {k: np.ascontiguousarray(np.asarray(v, dtype=np.float32)) for k, v in inputs.items()
              if k not in ("x", "mem", "mlstm_conv_w")}
    shared["mlstm_conv_wT"] = np.ascontiguousarray(np.asarray(inputs["mlstm_conv_w"], dtype=np.float32).transpose(0, 2, 1))
    shared.update(cst)
    x = np.asarray(inputs["x"], dtype=np.float32)
    mem = np.asarray(inputs["mem"], dtype=np.float32)
    in_maps = []
    for b in range(8):
        m = dict(shared)
        m["x"] = np.ascontiguousarray(x[b])
        m["mem"] = np.ascontiguousarray(mem[b])
        in_maps.append(m)
    res = run_bass_kernel_spmd(nc, in_maps, core_ids=list(range(8)))
    return np.stack([r["out"] for r in res.results], axis=0).astype(np.float32)
```

```python
import numpy as np
from contextlib import ExitStack
import concourse.bass as bass
import concourse.mybir as mybir
from concourse.bass_utils import run_bass_kernel_spmd

F32 = mybir.dt.float32
BF16 = mybir.dt.bfloat16
U32 = mybir.dt.uint32
I32 = mybir.dt.int32
AF = mybir.ActivationFunctionType
ALU = mybir.AluOpType
AX = mybir.AxisListType

S = 4096
D = 1024
NT = S // 128
DEPTH = 4
INC = 2064
EPS = 1e-6
NEXP = 16
CAP = 512
DFF = 2048


class Tile:
    def __init__(self, kb, t, name):
        self.kb = kb
        self.t = t
        self.name = name
        self.last_write = None
        self.readers = {}
        self.dsem = None

    def __getitem__(self, key):
        return TAP(self.t[key], self)

    def ap(self):
        return TAP(self.t[:], self)


class TAP:
    def __init__(self, ap, tile):
        self.ap = ap
        self.tile = tile

    def __getitem__(self, key):
        return TAP(self.ap[key], self.tile)

    def __getattr__(self, name):
        attr = getattr(self.ap, name)
        if callable(attr):
            def f(*a, **kw):
                r = attr(*a, **kw)
                if isinstance(r, bass.AP):
                    return TAP(r, self.tile)
                return r
            return f
        return attr


class Eng:
    def __init__(self, kb, name, eng, sem):
        self.kb = kb
        self.name = name
        self.eng = eng
        self.sem = sem
        self.count = 0
        self.known = {}

    def __getattr__(self, fn):
        def f(*a, **kw):
            return self.kb.emit(self, fn, a, kw)
        return f


class KB:
    def __init__(self, nc, es, n_dma_sems=92):
        self.nc = nc
        self.es = es
        self.sems = {}
        self.engs = {}
        for name, eng in (("pe", nc.tensor), ("dve", nc.vector), ("act", nc.scalar),
                          ("pool", nc.gpsimd), ("sp", nc.sync)):
            sem = es.enter_context(nc.semaphore("S_" + name))
            self.sems[name] = sem
            self.engs[name] = Eng(self, name, eng, name)
        self.pe, self.dve, self.act, self.pool, self.sp = (self.engs[n] for n in ("pe", "dve", "act", "pool", "sp"))
        self.dma_free = []
        self.dma_count = {}
        for i in range(n_dma_sems):
            key = "D%d" % i
            self.sems[key] = es.enter_context(nc.semaphore(key))
            self.dma_count[key] = 0
            self.dma_free.append(key)
        self.phase_tiles = []
        self.phase_stack = None
        self.all_tiles = []
        self.n_inst = 0
        self.n_wait = 0

    def begin_phase(self):
        self.phase_stack = ExitStack()
        self.phase_tiles = []

    def end_phase(self):
        self.barrier()
        for t in self.phase_tiles:
            if t.dsem is not None:
                self.dma_free.append(t.dsem)
                t.dsem = None
        self.phase_stack.close()
        self.phase_stack = None
        self.phase_tiles = []

    def begin_hold(self):
        self.hold_stack = ExitStack()
        self.hold_tiles = []

    def end_hold(self):
        for t in self.hold_tiles:
            if t.dsem is not None:
                self.dma_free.append(t.dsem)
                t.dsem = None
        self.hold_stack.close()
        self.hold_stack = None
        self.hold_tiles = []

    def sb(self, name, shape, dtype, glob=False, hold=False):
        st = self.es if glob else (self.hold_stack if hold else self.phase_stack)
        self.n_alloc = getattr(self, "n_alloc", 0) + 1
        name = "%s_%d" % (name, self.n_alloc)
        t = st.enter_context(self.nc.sbuf_tensor(name, list(shape), dtype))
        tl = Tile(self, t, name)
        if hold:
            self.hold_tiles.append(tl)
        elif not glob:
            self.phase_tiles.append(tl)
        self.all_tiles.append(tl)
        return tl

    def ps(self, name, shape, dtype):
        t = self.es.enter_context(self.nc.psum_tensor(name, list(shape), dtype))
        tl = Tile(self, t, name)
        self.all_tiles.append(tl)
        return tl

    def token(self, name):
        tl = Tile(self, None, name)
        self.all_tiles.append(tl)
        return tl

    def tile_dsem(self, tile):
        if tile.dsem is None:
            tile.dsem = self.dma_free.pop()
        return tile.dsem

    def emit(self, E, fn, args, kw):
        reads, writes = [], []
        kw = dict(kw)
        xr = kw.pop("_reads", [])
        xw = kw.pop("_writes", [])

        def unwrap(v, is_out):
            if isinstance(v, TAP):
                (writes if is_out else reads).append(v.tile)
                return v.ap
            return v
        a2 = [unwrap(a, (i == 0 and fn in ("matmul", "transpose"))) for i, a in enumerate(args)]
        kw2 = {k_: unwrap(v, k_ in ("out", "accum_out", "ap", "out_ap")) for k_, v in kw.items()}
        reads.extend(xr)
        writes.extend(xw)
        is_dma = fn in ("dma_start", "indirect_dma_start")
        deps = {}

        def add(ev):
            if ev is None:
                return
            sk, val, src = ev
            if src is E and E.name == "pe":
                return
            if deps.get(sk, 0) < val:
                deps[sk] = val
        for t in reads:
            add(t.last_write)
        for t in writes:
            add(t.last_write)
            for sk, (val, src) in t.readers.items():
                add((sk, val, src))
        for sk, val in deps.items():
            if E.known.get(sk, 0) >= val:
                continue
            E.eng.wait_ge(self.sems[sk], val)
            E.known[sk] = val
            self.n_wait += 1
        inst = getattr(E.eng, fn)(*a2, **kw2)
        self.n_inst += 1
        if is_dma:
            sbt = None
            for t in writes + reads:
                if t.t is not None:
                    sbt = t
                    break
            sk = self.tile_dsem(sbt)
            self.dma_count[sk] += 16
            inst.then_inc(self.sems[sk], 16)
            ev = (sk, self.dma_count[sk], None)
        else:
            E.count += 1
            inst.then_inc(self.sems[E.sem], 1)
            ev = (E.sem, E.count, E)
        for t in writes:
            t.last_write = ev
            t.readers = {}
        for t in reads:
            if t in writes:
                continue
            sk, val, src = ev
            if t.readers.get(sk, (0, None))[0] < val:
                t.readers[sk] = (val, src)
        return inst

    def barrier(self):
        sp = self.sp
        for name in ("pe", "dve", "act", "pool"):
            e = self.engs[name]
            if sp.known.get(name, 0) < e.count:
                sp.eng.wait_ge(self.sems[name], e.count)
                sp.known[name] = e.count
        for sk, c in self.dma_count.items():
            if c > 0 and sp.known.get(sk, 0) < c:
                sp.eng.wait_ge(self.sems[sk], c)
                sp.known[sk] = c
        sp.count += 1
        sp.eng.nop().then_inc(self.sems["sp"], 1)
        for name in ("pe", "dve", "act", "pool"):
            e = self.engs[name]
            e.eng.wait_ge(self.sems["sp"], sp.count)
        for e in self.engs.values():
            for n2, e2 in self.engs.items():
                e.known[n2] = e2.count
            for sk, c in self.dma_count.items():
                e.known[sk] = c
        for t in self.all_tiles:
            t.last_write = None
            t.readers = {}


def bfview(pt):
    return pt[:].bitcast(BF16)


def load_cast(kb, dst, src, eng=None):
    n = src.shape[-1]
    step = 2048
    for c0 in range(0, n, step):
        c1 = min(n, c0 + step)
        kb.pool.dma_start(out=dst[..., c0:c1], in_=src[..., c0:c1])


DBG = {}


def build(depth=DEPTH, debug=(), phases=None, moe_depth=DEPTH, final_norm=True):
    nc = bass.Bass("TRN2", target_bir_lowering=False)
    dbg = set(debug)

    def din(name, shape, dt=F32):
        return nc.dram_tensor(name, list(shape), dt, kind="ExternalInput").ap()

    def dscr(name, shape, dt=F32):
        kind = "ExternalOutput" if name in dbg else "Internal"
        return nc.dram_tensor(name, list(shape), dt, kind=kind).ap()

    x_in = din("x", [S, D])
    mem_in = din("mem", [256, D])
    W = {}
    W["norm_mix_g"] = din("norm_mix_g", [DEPTH, D])
    W["w_in"] = din("w_in", [DEPTH, D, INC])
    W["pool_w"] = din("pool_w", [DEPTH, 4, 64, 64])
    W["pool_scale"] = din("pool_scale", [DEPTH, 256])
    W["mlstm_conv_wT"] = din("mlstm_conv_wT", [DEPTH, 512, 5])
    W["mlstm_gate_b"] = din("mlstm_gate_b", [DEPTH, 16])
    W["mlstm_norm_g"] = din("mlstm_norm_g", [DEPTH, 256])
    W["q_norm_g"] = din("q_norm_g", [DEPTH, 64])
    W["k_norm_g"] = din("k_norm_g", [DEPTH, 64])
    W["w_out"] = din("w_out", [DEPTH, D, D])
    W["norm_mem_g"] = din("norm_mem_g", [DEPTH, D])
    W["ca_wq"] = din("ca_wq", [DEPTH, D, 512])
    W["ca_wkv"] = din("ca_wkv", [DEPTH, D, D])
    W["ca_wo"] = din("ca_wo", [DEPTH, 512, D])
    W["norm_ffn_g"] = din("norm_ffn_g", [DEPTH, D])
    W["router_w"] = din("router_w", [DEPTH, D, NEXP])
    W["expert_w_gu"] = din("expert_w_gu", [moe_depth, NEXP, D, 2 * DFF])
    W["expert_w_down"] = din("expert_w_down", [moe_depth, NEXP, DFF, D])
    W["final_norm_g"] = din("final_norm_g", [D])
    C_cos = din("c_cos", [S, 32])
    C_sin = din("c_sin", [S, 32])
    C_identf = din("c_identf", [128, 128])
    C_invc = din("c_invc", [256, S])
    C_tri = din("c_tri", [128, 256])
    out = nc.dram_tensor("out", [S, D], F32, kind="ExternalOutput").ap()

    xs = dscr("xs", [S, D])
    featT = dscr("featT", [768, S])
    mv_d = dscr("mv_d", [S, 256], BF16)
    mo_d = dscr("mo_d", [S, 256], BF16)
    gates_d = dscr("gates_d", [S, 16])
    qT_d = dscr("qT_d", [4, 128, S], BF16)
    kT_d = dscr("kT_d", [2, 128, S], BF16)
    av_d = dscr("av_d", [S, 128], BF16)
    yT_d = dscr("yT_d", [D, S], BF16)
    hn_d = dscr("hn_d", [S, D], BF16)
    aff_d = dscr("aff_d", [NEXP, S])
    qk_d = dscr("qk_d", [4, 128, S], BF16)

    DBG.clear()
    if "dbg_X" in dbg:
        DBG["X"] = dscr("dbg_X", [128, 4 * D], BF16)
        DBG["hT"] = dscr("dbg_hT", [128, 16 * CAP], BF16)
        DBG["yg"] = dscr("dbg_yg", [128, D], F32)
    if "dbg_idx" in dbg:
        DBG["idx"] = dscr("dbg_idx", [128, 64], U32)
        DBG["g"] = dscr("dbg_g", [128, 64], F32)
    es = ExitStack()
    with es:
        kb = KB(nc, es)
        pe, dve, act, pool, sp = kb.pe, kb.dve, kb.act, kb.pool, kb.sp
        PS = [kb.ps("psum%d" % i, [128, 512], F32) for i in range(8)]
        identf = kb.sb("identf", [128, 128], F32, glob=True)
        identb = kb.sb("identb", [128, 128], BF16, glob=True)
        idxT = kb.sb("G_idxT", [128, 4, NEXP], U32, glob=True)
        gT = kb.sb("G_gT", [128, 4, NEXP], F32, glob=True)
        sp.dma_start(out=identf[:], in_=C_identf[:, :])
        pool.dma_start(out=identb[:], in_=C_identf[:, :])

        for layer in range(depth):
            if phases is None or "A" in phases:
                phase_A(kb, nc, layer, W, PS, identb, (x_in if layer == 0 else xs), featT, mv_d, mo_d, gates_d, qT_d, kT_d, av_d, C_cos, C_sin)
            if phases is None or "B" in phases:
                phase_B(kb, nc, layer, W, PS, featT, yT_d, C_invc)
            if phases is None or "C" in phases:
                phase_C(kb, nc, layer, W, PS, identb, featT, mv_d, mo_d, gates_d, qk_d, yT_d, C_tri)
            efw = None
            if phases is None or "E" in phases:
                kb.begin_hold()
                efw = {"wo": kb.sb("E_wo", [128, 8, D], BF16, hold=True), "wq": kb.sb("E_wq", [128, 8, 512], BF16, hold=True),
                       "wkv": kb.sb("E_wkv", [128, 8, D], BF16, hold=True), "cwo": kb.sb("E_cwo", [128, 4, D], BF16, hold=True)}
                for c in range(8):
                    kb.pool.dma_start(out=efw["wo"][:, c, :], in_=W["w_out"][layer][c * 128:(c + 1) * 128, :])
                    kb.pool.dma_start(out=efw["wq"][:, c, :], in_=W["ca_wq"][layer][c * 128:(c + 1) * 128, :])
                    kb.pool.dma_start(out=efw["wkv"][:, c, :], in_=W["ca_wkv"][layer][c * 128:(c + 1) * 128, :])
                for c in range(4):
                    kb.pool.dma_start(out=efw["cwo"][:, c, :], in_=W["ca_wo"][layer][c * 128:(c + 1) * 128, :])
            if phases is None or "D" in phases:
                phase_D(kb, nc, layer, PS, qT_d, kT_d, av_d, yT_d)
            if phases is None or "E" in phases:
                phase_EF(kb, nc, layer, W, PS, identb, (x_in if layer == 0 else xs), xs, yT_d, mem_in, efw,
                         g1args={"identf": identf, "hn_d": hn_d, "aff_d": aff_d})
                kb.end_hold()
            if phases is None or "G" in phases:
                phase_G(kb, nc, layer, W, PS, identb, identf, xs, hn_d, idxT, gT, aff_d)

        kb.begin_phase()
        cp = [kb.sb("cpo%d" % i, [128, 4, D], F32) for i in range(2)]
        co = [kb.sb("cpq%d" % i, [128, 4, D], F32) for i in range(2)]
        fst = [kb.sb("fst%d" % i, [128, 4], F32) for i in range(2)]
        fjunk = kb.sb("fjunk", [128, D], BF16)
        fg = kb.sb("fg", [128, D], F32)
        sp.dma_start(out=fg[:], in_=W["final_norm_g"].partition_broadcast(128))
        for blk in range(8):
            t = cp[blk % 2]
            o = co[blk % 2]
            stt = fst[blk % 2]
            rows = slice(blk * 512, (blk + 1) * 512)
            sp.dma_start(out=t[:], in_=xs[rows, :].rearrange("(j p) d -> p j d", p=128))
            if final_norm:
                for j in range(4):
                    act.activation(out=fjunk[:], in_=t[:, j, :], func=AF.Square, accum_out=stt[:, j:j + 1])
                act.activation(out=stt[:], in_=stt[:], func=AF.Sqrt, scale=1.0 / D, bias=EPS)
                dve.reciprocal(out=stt[:], in_=stt[:])
                for j in range(4):
                    dve.scalar_tensor_tensor(out=o[:, j, :], in0=t[:, j, :], scalar=stt[:, j:j + 1], in1=fg[:], op0=ALU.mult, op1=ALU.mult)
                sp.dma_start(out=out[rows, :].rearrange("(j p) d -> p j d", p=128), in_=o[:])
            else:
                sp.dma_start(out=out[rows, :].rearrange("(j p) d -> p j d", p=128), in_=t[:])
        kb.end_phase()
        print("instructions", kb.n_inst, "waits", kb.n_wait)
    return nc


def phase_A(kb, nc, layer, W, PS, identb, xs, featT, mv_d, mo_d, gates_d, qT_d, kT_d, av_d, C_cos, C_sin):
    pe, dve, act, pool, sp = kb.pe, kb.dve, kb.act, kb.pool, kb.sp
    kb.begin_phase()
    w = kb.sb("A_w", [128, 8, INC], BF16)
    wsrc = W["w_in"][layer].rearrange("(c p) n -> p c n", p=128)
    for c in range(8):
        pool.dma_start(out=w[:, c, 0:1280], in_=wsrc[:, c, 0:1280])
        pool.dma_start(out=w[:, c, 1280:1792], in_=wsrc[:, c, 1296:1808])
        pool.dma_start(out=w[:, c, 1792:1808], in_=wsrc[:, c, 1280:1296])
        pool.dma_start(out=w[:, c, 1808:2064], in_=wsrc[:, c, 1808:2064])
    g_bc = kb.sb("A_g", [128, D], F32)
    sp.dma_start(out=g_bc[:], in_=W["norm_mix_g"][layer].partition_broadcast(128))
    gq = kb.sb("A_gq", [128, 64], F32)
    gk = kb.sb("A_gk", [128, 64], F32)
    gb = kb.sb("A_gb", [128, 16], F32)
    sp.dma_start(out=gq[:], in_=W["q_norm_g"][layer].partition_broadcast(128))
    sp.dma_start(out=gk[:], in_=W["k_norm_g"][layer].partition_broadcast(128))
    sp.dma_start(out=gb[:], in_=W["mlstm_gate_b"][layer].partition_broadcast(128))
    dve.tensor_scalar(out=gq[:], in0=gq[:], scalar1=0.125, scalar2=None, op0=ALU.mult)
    cos_t = kb.sb("A_cos", [128, NT, 32], F32)
    sin_t = kb.sb("A_sin", [128, NT, 32], F32)
    sp.dma_start(out=cos_t[:], in_=C_cos.rearrange("(j p) f -> p j f", p=128))
    sp.dma_start(out=sin_t[:], in_=C_sin.rearrange("(j p) f -> p j f", p=128))

    xb = [kb.sb("A_x%d" % i, [128, 4, D], F32) for i in range(2)]
    hn = [kb.sb("A_hn%d" % i, [128, D], BF16) for i in range(2)]
    junk = kb.sb("A_junk", [128, D], BF16)
    st = [kb.sb("A_st%d" % i, [128, 4], F32) for i in range(2)]
    hnT = [kb.sb("A_hnT%d" % i, [128, 8, 512], BF16) for i in range(2)]
    fst = [kb.sb("A_fst%d" % i, [128, 512], F32) for i in range(2)]
    vo = [kb.sb("A_vo%d" % i, [128, 512], BF16) for i in range(2)]
    g2 = [kb.sb("A_g2%d" % i, [128, 272], F32) for i in range(2)]
    gt = [kb.sb("A_gt%d" % i, [128, 16], F32) for i in range(2)]
    avs = [kb.sb("A_av%d" % i, [128, 128], BF16) for i in range(2)]
    qs = [kb.sb("A_qs%d" % i, [128, 512], F32) for i in range(2)]
    sq = kb.sb("A_sq", [128, 512], F32)
    ss8 = kb.sb("A_ss8", [128, 8], F32)
    tmp = [kb.sb("A_tmp%d" % i, [128, 256], F32) for i in range(4)]
    qr = [kb.sb("A_qr%d" % i, [128, 512], BF16) for i in range(2)]
    kr = [kb.sb("A_kr%d" % i, [128, 2, 2, 64], BF16) for i in range(2)]
    qTb = [kb.sb("A_qTb%d" % i, [128, 4, 512], BF16) for i in range(2)]
    kTb = [kb.sb("A_kTb%d" % i, [128, 2, 512], BF16) for i in range(2)]

    sqk = kb.sb("A_sqk", [128, 128], F32)
    ss8k = kb.sb("A_ss8k", [128, 2], F32)
    tmpk = [kb.sb("A_tmpk%d" % i, [128, 64], F32) for i in range(4)]

    def hnr_a(src, nh, sqb, ssb):
        n = nh * 64
        dve.tensor_tensor(out=sqb[:, 0:n], in0=src, in1=src, op=ALU.mult)
        dve.tensor_reduce(out=ssb[:, 0:nh], in_=sqb[:, 0:n].rearrange("p (h d) -> p h d", d=64), axis=AX.X, op=ALU.add)

    def hnr_s(nh, ssb):
        act.activation(out=ssb[:, 0:nh], in_=ssb[:, 0:nh], func=AF.Sqrt, scale=1.0 / 64, bias=EPS)

    def hnr_b(src, nh, g, j_tile, dst_views, sqb, ssb, tmps):
        n = nh * 64
        dve.reciprocal(out=ssb[:, 0:nh], in_=ssb[:, 0:nh])
        s3 = src.rearrange("p (h d) -> p h d", d=64)
        q3 = sqb[:, 0:n].rearrange("p (h d) -> p h d", d=64)
        dve.tensor_tensor(out=q3, in0=s3, in1=ssb[:, 0:nh].unsqueeze(2).to_broadcast([128, nh, 64]), op=ALU.mult)
        dve.tensor_tensor(out=q3, in0=q3, in1=g[:].unsqueeze(1).to_broadcast([128, nh, 64]), op=ALU.mult)
        q4 = sqb[:, 0:n].rearrange("p (h i two) -> p h i two", h=nh, two=2)
        x0 = q4[:, :, :, 0]
        x1 = q4[:, :, :, 1]
        cb = cos_t[:, j_tile, :].unsqueeze(1).to_broadcast([128, nh, 32])
        sb_ = sin_t[:, j_tile, :].unsqueeze(1).to_broadcast([128, nh, 32])
        m = nh * 32
        tv = [t[:, 0:m].rearrange("p (h i) -> p h i", h=nh) for t in tmps]
        dve.tensor_tensor(out=tv[0], in0=x0, in1=cb, op=ALU.mult)
        dve.tensor_tensor(out=tv[1], in0=x1, in1=sb_, op=ALU.mult)
        dve.tensor_tensor(out=tv[2], in0=x0, in1=sb_, op=ALU.mult)
        dve.tensor_tensor(out=tv[3], in0=x1, in1=cb, op=ALU.mult)
        for dv in dst_views:
            dve.tensor_tensor(out=dv[:, :, :, 0], in0=tv[0], in1=tv[1], op=ALU.subtract)
            dve.tensor_tensor(out=dv[:, :, :, 1], in0=tv[2], in1=tv[3], op=ALU.add)

    def prologue(blk):
        rows = slice(blk * 512, (blk + 1) * 512)
        X = xb[blk % 2]
        HT = hnT[blk % 2]
        sp.dma_start(out=X[:], in_=xs[rows, :].rearrange("(j p) d -> p j d", p=128))
        stt = st[blk % 2]
        for j in range(4):
            act.activation(out=junk[:], in_=X[:, j, :], func=AF.Square, accum_out=stt[:, j:j + 1])
        act.activation(out=stt[:], in_=stt[:], func=AF.Sqrt, scale=1.0 / D, bias=EPS)
        dve.reciprocal(out=stt[:], in_=stt[:])
        for j in range(4):
            H = hn[j % 2]
            dve.scalar_tensor_tensor(out=H[:], in0=X[:, j, :], scalar=stt[:, j:j + 1], in1=g_bc[:], op0=ALU.mult, op1=ALU.mult)
            pt = PS[j % 2]
            pv = bfview(pt)
            for c in range(8):
                pe.transpose(pv[:, c * 128:(c + 1) * 128], H[:, c * 128:(c + 1) * 128], identb[:])
            act.activation(out=HT[:, :, j * 128:(j + 1) * 128], in_=pv.rearrange("p (c t) -> p c t", c=8), func=AF.Copy)

    def mainA(blk):
        rows = slice(blk * 512, (blk + 1) * 512)
        HT = hnT[blk % 2]
        for ch in range(6):
            pt = PS[2 + ch % 2]
            for c in range(8):
                pe.matmul(pt[:], lhsT=w[:, c, ch * 128:(ch + 1) * 128], rhs=HT[:, c, :], start=(c == 0), stop=(c == 7))
            f = fst[ch % 2]
            act.activation(out=f[:], in_=pt[:], func=AF.Copy)
            sp.dma_start(out=featT[ch * 128:(ch + 1) * 128, rows], in_=f[:])
        QT = qTb[blk % 2]
        KT = kTb[blk % 2]

        def part1(j):
            jt = blk * 4 + j
            trow = slice(jt * 128, (jt + 1) * 128)
            p1, p2, p3 = PS[4], PS[5], PS[7]
            for c in range(8):
                pe.matmul(p1[:], lhsT=HT[:, c, j * 128:(j + 1) * 128], rhs=w[:, c, 768:1280], start=(c == 0), stop=(c == 7))
            for c in range(8):
                pe.matmul(p2[:, 0:272], lhsT=HT[:, c, j * 128:(j + 1) * 128], rhs=w[:, c, 1792:2064], start=(c == 0), stop=(c == 7))
            for c in range(8):
                pe.matmul(p3[:], lhsT=HT[:, c, j * 128:(j + 1) * 128], rhs=w[:, c, 1280:1792], start=(c == 0), stop=(c == 7))
            V = vo[j % 2]
            G2 = g2[j % 2]
            Q = qs[j % 2]
            GT = gt[j % 2]
            AV = avs[j % 2]
            KR = kr[j % 2]
            QR = qr[j % 2]
            act.activation(out=G2[:], in_=p2[:, 0:272], func=AF.Copy)
            act.activation(out=Q[:], in_=p3[:], func=AF.Copy)
            act.activation(out=V[:, 0:256], in_=p1[:, 0:256], func=AF.Copy)
            act.activation(out=V[:, 256:512], in_=p1[:, 256:512], func=AF.Copy)
            sp.dma_start(out=mv_d[trow, :], in_=V[:, 0:256])
            sp.dma_start(out=mo_d[trow, :], in_=V[:, 256:512])
            dve.tensor_tensor(out=GT[:], in0=G2[:, 0:16], in1=gb[:], op=ALU.add)
            dve.tensor_copy(out=AV[:], in_=G2[:, 144:272])
            sp.dma_start(out=av_d[trow, :], in_=AV[:])
            hnr_a(G2[:, 16:144], 2, sqk, ss8k)
            hnr_a(Q[:], 8, sq, ss8)
            hnr_s(2, ss8k)
            hnr_s(8, ss8)
            sp.dma_start(out=gates_d[trow, :], in_=GT[:])
            hnr_b(G2[:, 16:144], 2, gk, jt,
                  [KR[:, :, 0, :].rearrange("p h (i two) -> p h i two", two=2),
                   KR[:, :, 1, :].rearrange("p h (i two) -> p h i two", two=2)], sqk, ss8k, tmpk)
            hnr_b(Q[:], 8, gq, jt, [QR[:].rearrange("p (h i two) -> p h i two", h=8, two=2)], sq, ss8, tmp)

        def part2(j):
            KR = kr[j % 2]
            QR = qr[j % 2]
            pv = bfview(PS[6])
            for kv in range(2):
                pe.transpose(pv[:, kv * 128:(kv + 1) * 128], KR[:, kv, :, :].rearrange("p a d -> p (a d)"), identb[:])
            for c in range(4):
                pe.transpose(pv[:, 256 + c * 128:256 + (c + 1) * 128], QR[:, c * 128:(c + 1) * 128], identb[:])
            act.activation(out=KT[:, :, j * 128:(j + 1) * 128], in_=pv[:, 0:256].rearrange("p (c t) -> p c t", c=2), func=AF.Copy)
            act.activation(out=QT[:, :, j * 128:(j + 1) * 128], in_=pv[:, 256:768].rearrange("p (c t) -> p c t", c=4), func=AF.Copy)
        for j in range(4):
            part1(j)
            if j >= 1:
                part2(j - 1)
        part2(3)
        sp.dma_start(out=qT_d[:, :, rows].rearrange("c p t -> p c t"), in_=QT[:])
        sp.dma_start(out=kT_d[:, :, rows].rearrange("c p t -> p c t"), in_=KT[:])

    prologue(0)
    for blk in range(8):
        if blk + 1 < 8:
            prologue(blk + 1)
        mainA(blk)
    kb.end_phase()


def phase_B(kb, nc, layer, W, PS, featT, yT_d, C_invc):
    pe, dve, act, pool, sp = kb.pe, kb.dve, kb.act, kb.pool, kb.sp
    kb.begin_phase()
    PADL = 16
    WID = S + 32
    U = kb.sb("B_U", [128, 2, WID], F32)
    dve.memset(U[:, :, 0:PADL], 0.0)
    dve.memset(U[:, :, PADL + S:WID], 0.0)
    for c in range(2):
        sp.dma_start(out=U[:, c, PADL:PADL + S], in_=featT[c * 128:(c + 1) * 128, :])
    invc = kb.sb("B_invc", [128, 2, S], F32)
    sp.dma_start(out=invc[:], in_=C_invc.rearrange("(c p) t -> p c t", p=128))
    BD = kb.sb("B_BD", [128, 2, 128], BF16)
    dve.memset(BD[:], 0.0)
    for c in range(2):
        pool.dma_start(out=BD[0:64, c, 0:64], in_=W["pool_w"][layer][2 * c])
        pool.dma_start(out=BD[64:128, c, 64:128], in_=W["pool_w"][layer][2 * c + 1])
    psc = kb.sb("B_psc", [128, 2], F32)
    for c in range(2):
        sp.dma_start(out=psc[:, c:c + 1], in_=W["pool_scale"][layer][c * 128:(c + 1) * 128].rearrange("(p o) -> p o", o=1))
    P2a = kb.sb("B_P2a", [128, WID], F32)
    P2b = kb.sb("B_P2b", [128, WID], F32)
    P4b = kb.sb("B_P4b", [128, WID], F32)
    P8b = kb.sb("B_P8b", [128, WID], F32)
    Ss = kb.sb("B_S", [128, 2, S], F32)
    dT = kb.sb("B_dT", [128, 2, S], BF16)

    def rng(t, lo, hi, sh=0):
        return slice(PADL + lo + sh, PADL + hi + sh)
    lo = -8
    u0, u1 = U[:, 0, :], U[:, 1, :]
    pool.tensor_tensor(out=Ss[0:64, 0, :], in0=U[0:64, 0, rng(0, 0, S, -1)], in1=U[0:64, 0, rng(0, 0, S)], op=ALU.add)
    dve.tensor_tensor(out=P2a[64:128, rng(0, lo, S + 12)], in0=U[64:128, 0, rng(0, lo, S + 12)], in1=U[64:128, 0, rng(0, lo, S + 12, 1)], op=ALU.add)
    dve.tensor_tensor(out=Ss[64:128, 0, :], in0=P2a[64:128, rng(0, 0, S, -2)], in1=P2a[64:128, rng(0, 0, S)], op=ALU.add)
    pool.tensor_tensor(out=P2b[:, rng(0, lo, S + 12)], in0=U[:, 1, rng(0, lo, S + 12)], in1=U[:, 1, rng(0, lo, S + 12, 1)], op=ALU.add)
    pool.tensor_tensor(out=P4b[:, rng(0, lo, S + 8)], in0=P2b[:, rng(0, lo, S + 8)], in1=P2b[:, rng(0, lo, S + 8, 2)], op=ALU.add)
    dve.tensor_tensor(out=Ss[0:64, 1, :], in0=P4b[0:64, rng(0, 0, S, -4)], in1=P4b[0:64, rng(0, 0, S)], op=ALU.add)
    pool.tensor_tensor(out=P8b[64:128, rng(0, lo, S + 4)], in0=P4b[64:128, rng(0, lo, S + 4)], in1=P4b[64:128, rng(0, lo, S + 4, 4)], op=ALU.add)
    dve.tensor_tensor(out=Ss[64:128, 1, :], in0=P8b[64:128, rng(0, 0, S, -8)], in1=P8b[64:128, rng(0, 0, S)], op=ALU.add)
    for c in range(2):
        dve.tensor_tensor(out=Ss[:, c, :], in0=Ss[:, c, :], in1=invc[:, c, :], op=ALU.mult)
        dve.tensor_tensor(out=dT[:, c, :], in0=Ss[:, c, :], in1=U[:, c, PADL:PADL + S], op=ALU.subtract)
    yst = [kb.sb("B_y%d" % i, [128, 512], BF16) for i in range(2)]
    k = 0
    for blk in range(8):
        for c in range(2):
            pt = PS[k % 2]
            pe.matmul(pt[:], lhsT=BD[:, c, :], rhs=dT[:, c, blk * 512:(blk + 1) * 512], start=True, stop=True)
            Y = yst[k % 2]
            k += 1
            act.activation(out=Y[:], in_=pt[:], func=AF.Copy, scale=psc[:, c:c + 1])
            sp.dma_start(out=yT_d[c * 128:(c + 1) * 128, blk * 512:(blk + 1) * 512], in_=Y[:])
    kb.end_phase()


def phase_C(kb, nc, layer, W, PS, identb, featT, mv_d, mo_d, gates_d, qk_d, yT_d, C_tri):
    pe, dve, act, pool, sp = kb.pe, kb.dve, kb.act, kb.pool, kb.sp
    kb.begin_phase()
    cw = kb.sb("C_cw", [128, 4, 5], F32)
    sp.dma_start(out=cw[:], in_=W["mlstm_conv_wT"][layer].rearrange("(c p) j -> p c j", p=128))
    Xp = [kb.sb("C_Xp%d" % i, [128, S + 4], F32) for i in range(2)]
    acc = [kb.sb("C_acc%d" % i, [128, S], F32) for i in range(2)]
    sgm = [kb.sb("C_sgm%d" % i, [128, S], F32) for i in range(2)]
    qko = [kb.sb("C_qko%d" % i, [128, S], BF16) for i in range(2)]
    for i in range(2):
        dve.memset(Xp[i][:, 0:2], 0.0)
        dve.memset(Xp[i][:, S + 2:S + 4], 0.0)
    def loadC0(ci):
        sp.dma_start(out=Xp[ci % 2][:, 2:S + 2], in_=featT[256 + ci * 128:256 + (ci + 1) * 128, :])
    loadC0(0)
    for ci in range(4):
        X = Xp[ci % 2]
        A_ = acc[ci % 2]
        G_ = sgm[ci % 2]
        O_ = qko[ci % 2]
        if ci + 1 < 4:
            loadC0(ci + 1)
        dve.tensor_scalar(out=A_[:], in0=X[:, 0:S], scalar1=cw[:, ci, 0:1], scalar2=None, op0=ALU.mult)
        for j in range(1, 5):
            dve.scalar_tensor_tensor(out=A_[:], in0=X[:, j:j + S], scalar=cw[:, ci, j:j + 1], in1=A_[:], op0=ALU.mult, op1=ALU.add)
        act.activation(out=G_[:], in_=A_[:], func=AF.Sigmoid)
        dve.scalar_tensor_tensor(out=O_[:], in0=A_[:], scalar=(1.0 if ci < 2 else 0.125), in1=G_[:], op0=ALU.mult, op1=ALU.mult)
        sp.dma_start(out=qk_d[ci], in_=O_[:])
    kb.end_phase()
    import os
    CSTOP = int(os.environ.get("CSTOP", "9"))
    if CSTOP <= 0:
        return
    kb.begin_phase()
    QK = kb.sb("C_QK", [128, 4, S], BF16)
    sp.dma_start(out=QK[:], in_=qk_d.rearrange("c p t -> p c t"))
    TRI = kb.sb("C_TRI", [128, 2, 128], F32)
    sp.dma_start(out=TRI[:], in_=C_tri.rearrange("p (a j) -> p a j", a=2))
    ones = kb.sb("C_ones", [128, 128], F32)
    dve.memset(ones[:], 1.0)
    G = kb.sb("C_G", [128, NT, 16], F32)
    sp.dma_start(out=G[:], in_=gates_d.rearrange("(c p) n -> p c n", p=128))
    act.activation(out=G[:, :, 8:16], in_=G[:, :, 8:16], func=AF.Exp, scale=-1.0)
    act.activation(out=G[:, :, 8:16], in_=G[:, :, 8:16], func=AF.Ln, bias=1.0)
    dve.tensor_scalar(out=G[:, :, 8:16], in0=G[:, :, 8:16], scalar1=-1.0, scalar2=None, op0=ALU.mult)
    ybf = kb.sb("C_ybf", [128, NT, 256], BF16)
    sp.dma_start(out=ybf[:], in_=mv_d.rearrange("(c p) n -> p c n", p=128))
    Va = kb.sb("C_Va", [128, NT, 4, 65], BF16)
    dve.memset(Va[:, :, :, 64:65], 1.0)
    dve.tensor_copy(out=Va[:, :, :, 0:64], in_=ybf[:].rearrange("p c (h d) -> p c h d", h=4))
    Kt = kb.sb("C_Kt", [128, NT, 256], BF16)
    for cg in range(8):
        pv = bfview(PS[cg % 2])
        for cl in range(4):
            c = cg * 4 + cl
            for hp in range(2):
                pe.transpose(pv[:, (cl * 2 + hp) * 128:(cl * 2 + hp + 1) * 128], QK[:, 2 + hp, c * 128:(c + 1) * 128], identb[:])
        act.activation(out=Kt[:, cg * 4:(cg + 1) * 4, :], in_=pv.rearrange("p (c n) -> p c n", c=4), func=AF.Copy)
    A = [kb.sb("C_A%d" % i, [128, NT, 4], F32) for i in range(2)]
    A2 = [kb.sb("C_A2%d" % i, [128, NT, 4], F32) for i in range(2)]
    Bc = [kb.sb("C_B%d" % i, [128, NT, 4], F32) for i in range(2)]
    Fc = [kb.sb("C_F%d" % i, [128, NT, 4], F32) for i in range(2)]
    at = kb.sb("C_at", [128, NT, 4], F32)
    lfc = kb.sb("C_lfc", [128, 2, NT * 4], F32)
    for d_ in range(2):
        dve.tensor_copy(out=lfc[:, d_, :].rearrange("p (c h) -> p c h", h=4), in_=G[:, :, 8 + 4 * d_:12 + 4 * d_])
    for d_ in range(2):
        lfv = lfc[:, d_, :]
        liv = G[:, :, 4 * d_:4 * d_ + 4]
        pc, ptot = PS[6], PS[7]
        pe.matmul(pc[:, 0:128], lhsT=TRI[:, d_, :], rhs=lfv, start=True, stop=True)
        pe.matmul(ptot[:, 0:128], lhsT=ones[:], rhs=lfv, start=True, stop=True)
        pc3 = pc[:, 0:128].rearrange("p (c h) -> p c h", h=4)
        pt3 = ptot[:, 0:128].rearrange("p (c h) -> p c h", h=4)
        dve.tensor_tensor(out=at[:], in0=liv, in1=pc3, op=ALU.subtract)
        act.activation(out=A[d_][:], in_=at[:], func=AF.Exp)
        dve.tensor_tensor(out=at[:], in0=at[:], in1=pt3, op=ALU.add)
        act.activation(out=A2[d_][:], in_=at[:], func=AF.Exp)
        act.activation(out=Bc[d_][:], in_=pc3, func=AF.Exp)
        act.activation(out=Fc[d_][:], in_=pt3, func=AF.Exp)
    if CSTOP <= 1:
        kb.end_phase()
        return
    Hd = [kb.sb("C_H%d" % i, [128, NT, 256], F32) for i in range(2)]
    Zf = [kb.sb("C_Zf%d" % i, [128, 4, 65], F32) for i in range(2)]
    Zb = [[kb.sb("C_Zb%d_%d" % (i, j), [128, 4, 65], BF16) for j in range(2)] for i in range(2)]
    for d_ in range(2):
        dve.memset(Zf[d_][:], 0.0)
        dve.memset(Zb[d_][0][:], 0.0)
    VWr = [kb.sb("C_VW%d" % i, [128, 2, 4, 65], BF16) for i in range(4)]
    SMr = [kb.sb("C_SM%d" % i, [128, 2, 2, 128], BF16) for i in range(4)]
    t4r = [kb.sb("C_t4%d" % i, [128, 4], F32) for i in range(4)]
    t4n = [kb.sb("C_t4n%d" % i, [128, 4], F32) for i in range(2)]
    for it in range(NT):
        cc = [it, NT - 1 - it]
        VWs = [VWr[(2 * it + d_) % 4] for d_ in range(2)]
        SMs = [SMr[(2 * it + d_) % 4] for d_ in range(2)]
        t4s = [t4r[(2 * it + d_) % 4] for d_ in range(2)]
        pss = [[PS[0], PS[1]], [PS[2], PS[3]]]
        psn = [PS[4], PS[5]]
        psu = [PS[6], PS[7]]
        for d_ in range(2):
            c = cc[d_]
            VW = VWs[d_]
            pool.tensor_tensor(out=VW[:, 0], in0=Va[:, c], in1=A[d_][:, c, :].unsqueeze(2).to_broadcast([128, 4, 65]), op=ALU.mult)
            pool.tensor_tensor(out=VW[:, 1], in0=Va[:, c], in1=A2[d_][:, c, :].unsqueeze(2).to_broadcast([128, 4, 65]), op=ALU.mult)
        for d_ in range(2):
            c = cc[d_]
            cs = slice(c * 128, (c + 1) * 128)
            for h in range(4):
                pb = (h % 2) * 64
                pe.matmul(pss[d_][h % 2][:, (h // 2) * 128:(h // 2 + 1) * 128], lhsT=QK[pb:pb + 64, 2 + h // 2, cs], rhs=QK[pb:pb + 64, h // 2, cs], start=True, stop=True)
        for d_ in range(2):
            for hh in range(2):
                dve.tensor_tensor(out=SMs[d_][:, hh], in0=pss[d_][hh][:, 0:256].rearrange("p (h j) -> p h j", h=2),
                                  in1=TRI[:, d_, :].unsqueeze(1).to_broadcast([128, 2, 128]), op=ALU.mult)
        for d_ in range(2):
            c = cc[d_]
            cs = slice(c * 128, (c + 1) * 128)
            Zc = Zb[d_][it % 2]
            for h in range(4):
                pe.matmul(psn[d_][:, h * 65:(h + 1) * 65], lhsT=SMs[d_][:, h % 2, h // 2, :], rhs=VWs[d_][:, 0, h, :], start=True, stop=False)
                pe.matmul(psn[d_][:, h * 65:(h + 1) * 65], lhsT=QK[:, h // 2, cs], rhs=Zc[:, h, :], start=False, stop=True)
            for hp in range(2):
                pe.matmul(psu[d_][:, hp * 130:(hp + 1) * 130], lhsT=Kt[:, c, hp * 128:(hp + 1) * 128],
                          rhs=VWs[d_][:, 1, 2 * hp:2 * hp + 2, :].rearrange("p a b -> p (a b)"), start=True, stop=True)
        for d_ in range(2):
            c = cc[d_]
            for hp in range(2):
                for hh in range(2):
                    rows = slice(hh * 64, (hh + 1) * 64)
                    h = 2 * hp + hh
                    dve.scalar_tensor_tensor(out=Zf[d_][rows, h, :], in0=Zf[d_][rows, h, :], scalar=Fc[d_][rows, c, h:h + 1],
                                             in1=psu[d_][rows, hp * 130 + hh * 65:hp * 130 + (hh + 1) * 65], op0=ALU.mult, op1=ALU.add)
            dve.tensor_copy(out=Zb[d_][(it + 1) % 2][:], in_=Zf[d_][:])
        n3s = [psn[d_][:, 0:260].rearrange("p (h e) -> p h e", h=4) for d_ in range(2)]
        for d_ in range(2):
            dve.tensor_tensor(out=t4s[d_][:], in0=n3s[d_][:, :, 64], in1=Bc[d_][:, cc[d_], :], op=ALU.mult)
        for d_ in range(2):
            dve.tensor_scalar(out=t4n[d_][:], in0=t4s[d_][:], scalar1=-1.0, scalar2=1.0, op0=ALU.mult, op1=ALU.max)
            dve.tensor_scalar(out=t4s[d_][:], in0=t4s[d_][:], scalar1=1.0, scalar2=None, op0=ALU.max)
            dve.tensor_tensor(out=t4s[d_][:], in0=t4s[d_][:], in1=t4n[d_][:], op=ALU.max)
            dve.reciprocal(out=t4s[d_][:], in_=t4s[d_][:])
            dve.tensor_tensor(out=t4s[d_][:], in0=t4s[d_][:], in1=Bc[d_][:, cc[d_], :], op=ALU.mult)
            dve.tensor_tensor(out=Hd[d_][:, cc[d_], :].rearrange("p (h e) -> p h e", h=4), in0=n3s[d_][:, :, 0:64],
                              in1=t4s[d_][:].unsqueeze(2).to_broadcast([128, 4, 64]), op=ALU.mult)
    if CSTOP <= 2:
        kb.end_phase()
        return
    H0, H1 = Hd
    ng = kb.sb("C_ng", [128, 256], F32)
    sp.dma_start(out=ng[:], in_=W["mlstm_norm_g"][layer].partition_broadcast(128))
    mo = kb.sb("C_mo", [128, NT, 256], BF16)
    sp.dma_start(out=mo[:], in_=mo_d.rearrange("(c p) n -> p c n", p=128))
    act.activation(out=mo[:], in_=mo[:], func=AF.Sigmoid)
    ss = kb.sb("C_ss", [128, NT, 4], F32)
    for half in range(2):
        cs = slice(half * 16, (half + 1) * 16)
        dve.tensor_tensor(out=H0[:, cs, :], in0=H0[:, cs, :], in1=H1[:, cs, :], op=ALU.add)
        pool.tensor_tensor(out=H1[:, cs, :], in0=H0[:, cs, :], in1=H0[:, cs, :], op=ALU.mult)
        dve.tensor_reduce(out=ss[:, cs, :], in_=H1[:, cs, :].rearrange("p c (h e) -> p c h e", h=4), axis=AX.X, op=ALU.add)
    act.activation(out=ss[:], in_=ss[:], func=AF.Sqrt, scale=1.0 / 64, bias=EPS)
    dve.reciprocal(out=ss[:], in_=ss[:])
    for half in range(2):
        cs = slice(half * 16, (half + 1) * 16)
        dve.tensor_tensor(out=H0[:, cs, :].rearrange("p c (h e) -> p c h e", h=4), in0=H0[:, cs, :].rearrange("p c (h e) -> p c h e", h=4),
                          in1=ss[:, cs, :].unsqueeze(3).to_broadcast([128, 16, 4, 64]), op=ALU.mult)
        pool.tensor_tensor(out=H0[:, cs, :], in0=H0[:, cs, :], in1=ng[:].unsqueeze(1).to_broadcast([128, 16, 256]), op=ALU.mult)
        dve.tensor_tensor(out=ybf[:, cs, :], in0=H0[:, cs, :], in1=mo[:, cs, :], op=ALU.mult)
    yst = [kb.sb("C_yst%d" % i, [128, 1024], BF16) for i in range(2)]
    k = 0
    for hp in range(2):
        for cg in range(4):
            pv = bfview(PS[k % 2])
            Y = yst[k % 2]
            k += 1
            for cl in range(8):
                c = cg * 8 + cl
                pe.transpose(pv[:, cl * 128:(cl + 1) * 128], ybf[:, c, hp * 128:(hp + 1) * 128], identb[:])
            act.activation(out=Y[:], in_=pv, func=AF.Copy)
            sp.dma_start(out=yT_d[256 + hp * 128:256 + (hp + 1) * 128, cg * 1024:(cg + 1) * 1024], in_=Y[:])
    kb.end_phase()


def phase_D(kb, nc, layer, PS, qT_d, kT_d, av_d, yT_d):
    pe, dve, act, pool, sp = kb.pe, kb.dve, kb.act, kb.pool, kb.sp
    kb.begin_phase()
    kT2 = kb.sb("D_kT", [128, 2, S], BF16)
    sp.dma_start(out=kT2[:], in_=kT_d.rearrange("c p t -> p c t"))
    Va = kb.sb("D_Va", [128, NT, 2, 128], BF16)
    dve.memset(Va[:, :, :, 64:128], 1.0)
    Vst = kb.sb("D_Vst", [128, NT, 128], BF16)
    sp.dma_start(out=Vst[:], in_=av_d.rearrange("(c p) n -> p c n", p=128))
    dve.tensor_copy(out=Va[:, :, :, 0:64], in_=Vst[:].rearrange("p c (h d) -> p c h d", h=2))
    Qe = [kb.sb("D_Qe%d" % i, [128, S], BF16) for i in range(2)]
    Qo = [kb.sb("D_Qo%d" % i, [128, S], BF16) for i in range(2)]
    for i in range(2):
        pool.memset(Qe[i][64:128, :], 0.0)
        pool.memset(Qo[i][0:64, :], 0.0)

    def load_q(pr):
        sp.dma_start(out=Qe[pr % 2][0:64, :], in_=qT_d[pr, 0:64, :])
        sp.dma_start(out=Qo[pr % 2][64:128, :], in_=qT_d[pr, 64:128, :])
    P = [kb.sb("D_P%d" % i, [128, 512], BF16) for i in range(4)]
    rd = [kb.sb("D_rd%d" % i, [64, 512], F32) for i in range(2)]
    yo = [kb.sb("D_yo%d" % i, [64, 512], BF16) for i in range(2)]
    SB = PS[0:4]
    OB = PS[4:6]
    steps = []
    for pr in range(4):
        for hh in range(2):
            for qb in range(8):
                for kc in range(NT):
                    steps.append((pr, hh, qb, kc))
    load_q(0)

    def issue_S(i):
        pr, hh, qb, kc = steps[i]
        kv = pr // 2
        if hh == 0 and qb == 0 and kc == 0 and pr + 1 < 4:
            load_q(pr + 1)
        Qt = (Qe if hh == 0 else Qo)[pr % 2]
        pe.matmul(SB[i % 4][:], lhsT=kT2[:, kv, kc * 128:(kc + 1) * 128],
                  rhs=Qt[:, qb * 512:(qb + 1) * 512], start=True, stop=True)
    LOOK = 2
    for i in range(min(LOOK, len(steps))):
        issue_S(i)
    ob_i = 0
    for i, (pr, hh, qb, kc) in enumerate(steps):
        if i + LOOK < len(steps):
            issue_S(i + LOOK)
        kv = pr // 2
        Pt = P[i % 4]
        act.activation(out=Pt[:], in_=SB[i % 4][:], func=AF.Exp)
        O = OB[ob_i % 2]
        pe.matmul(O[:], lhsT=Va[:, kc, kv, :], rhs=Pt[:], start=(kc == 0), stop=(kc == NT - 1))
        if kc == NT - 1:
            h = 2 * pr + hh
            R = rd[ob_i % 2]
            Y = yo[ob_i % 2]
            dve.reciprocal(out=R[:], in_=O[64:128, :])
            dve.tensor_tensor(out=Y[:], in0=O[0:64, :], in1=R[:], op=ALU.mult)
            sp.dma_start(out=yT_d[512 + h * 64:512 + (h + 1) * 64, qb * 512:(qb + 1) * 512], in_=Y[:])
            ob_i += 1
    kb.end_phase()


def phase_EF(kb, nc, layer, W, PS, identb, xsrc, xs, yT_d, mem_in, efw, g1args=None):
    pe, dve, act, pool, sp = kb.pe, kb.dve, kb.act, kb.pool, kb.sp
    kb.begin_phase()
    wo, wq, wkv, cwo = efw["wo"], efw["wq"], efw["wkv"], efw["cwo"]
    g1_block = g1_finish = None
    if g1args is not None:
        g1_block, g1_finish = make_g1(kb, nc, layer, W, PS, g1args["identf"], g1args["hn_d"], g1args["aff_d"])
    g_bc = kb.sb("E_g", [128, D], F32)
    sp.dma_start(out=g_bc[:], in_=W["norm_mem_g"][layer].partition_broadcast(128))
    ones = kb.sb("E_ones", [128, 128], BF16)
    dve.memset(ones[:], 1.0)
    memb = kb.sb("E_memb", [128, 2, D], BF16)
    for mc in range(2):
        pool.dma_start(out=memb[:, mc, :], in_=mem_in[mc * 128:(mc + 1) * 128, :])
    memT = kb.sb("E_memT", [128, 8, 256], BF16)
    for mc in range(2):
        pv = bfview(PS[mc])
        for c in range(8):
            pe.transpose(pv[:, c * 128:(c + 1) * 128], memb[:, mc, c * 128:(c + 1) * 128], identb[:])
        act.activation(out=memT[:, :, mc * 128:(mc + 1) * 128], in_=pv.rearrange("p (c t) -> p c t", c=8), func=AF.Copy)
    kcT = kb.sb("E_kcT", [128, 4, 256], BF16)
    for h in range(4):
        pt = PS[2 + h % 2]
        for c in range(8):
            pe.matmul(pt[:, 0:256], lhsT=wkv[:, c, h * 128:(h + 1) * 128], rhs=memT[:, c, :], start=(c == 0), stop=(c == 7))
        act.activation(out=kcT[:, h, :], in_=pt[:, 0:256], func=AF.Copy)
    Vc = kb.sb("E_Vc", [128, 2, 512], BF16)
    for mc in range(2):
        pt = PS[4 + mc]
        for c in range(8):
            pe.matmul(pt[:], lhsT=memT[:, c, mc * 128:(mc + 1) * 128], rhs=wkv[:, c, 512:1024], start=(c == 0), stop=(c == 7))
        act.activation(out=Vc[:, mc, :], in_=pt[:], func=AF.Copy)

    xb = [kb.sb("E_x%d" % i, [128, 4, D], F32) for i in range(2)]
    Yb = [kb.sb("E_Y%d" % i, [128, 8, 512], BF16) for i in range(2)]
    hn = [kb.sb("E_hn%d" % i, [128, D], BF16) for i in range(2)]
    junk = kb.sb("E_junk", [128, D], BF16)
    st = [kb.sb("E_st%d" % i, [128, 4], F32) for i in range(2)]
    hnT = [kb.sb("E_hnT%d" % i, [128, 8, 512], BF16) for i in range(2)]
    qcT = [kb.sb("E_qcT%d" % i, [128, 4, 512], BF16) for i in range(2)]
    PT = [kb.sb("E_PT%d" % i, [128, 2, 512], BF16) for i in range(2)]
    rden = [kb.sb("E_rden%d" % i, [128, 512], F32) for i in range(2)]
    oT = [kb.sb("E_oT%d" % i, [128, 4, 512], BF16) for i in range(2)]
    SC = float(128 ** -0.5)
    def loadE(blk):
        rows_ = slice(blk * 512, (blk + 1) * 512)
        sp.dma_start(out=xb[blk % 2][:], in_=xsrc[rows_, :].rearrange("(j p) d -> p j d", p=128))
        sp.dma_start(out=Yb[blk % 2][:], in_=yT_d[:, rows_].rearrange("(c p) t -> p c t", p=128))

    def stage1(blk):
        X = xb[blk % 2]
        Y = Yb[blk % 2]
        HT = hnT[blk % 2]
        stt = st[blk % 2]
        for j in range(4):
            for half in range(2):
                pt = PS[half]
                for c in range(8):
                    pe.matmul(pt[:], lhsT=Y[:, c, j * 128:(j + 1) * 128], rhs=wo[:, c, half * 512:(half + 1) * 512], start=(c == 0), stop=(c == 7))
                dve.tensor_tensor(out=X[:, j, half * 512:(half + 1) * 512], in0=X[:, j, half * 512:(half + 1) * 512], in1=pt[:], op=ALU.add)
            act.activation(out=junk[:], in_=X[:, j, :], func=AF.Square, accum_out=stt[:, j:j + 1])
        act.activation(out=stt[:], in_=stt[:], func=AF.Sqrt, scale=1.0 / D, bias=EPS)
        dve.reciprocal(out=stt[:], in_=stt[:])
        for j in range(4):
            H = hn[j % 2]
            dve.scalar_tensor_tensor(out=H[:], in0=X[:, j, :], scalar=stt[:, j:j + 1], in1=g_bc[:], op0=ALU.mult, op1=ALU.mult)
            pt = PS[2 + j % 2]
            pv = bfview(pt)
            for c in range(8):
                pe.transpose(pv[:, c * 128:(c + 1) * 128], H[:, c * 128:(c + 1) * 128], identb[:])
            act.activation(out=HT[:, :, j * 128:(j + 1) * 128], in_=pv.rearrange("p (c t) -> p c t", c=8), func=AF.Copy)

    def stage2(blk):
        rows = slice(blk * 512, (blk + 1) * 512)
        X = xb[blk % 2]
        HT = hnT[blk % 2]
        QC = qcT[blk % 2]
        for h in range(4):
            pt = PS[4 + h % 2]
            for c in range(8):
                pe.matmul(pt[:], lhsT=wq[:, c, h * 128:(h + 1) * 128], rhs=HT[:, c, :], start=(c == 0), stop=(c == 7))
            act.activation(out=QC[:, h, :], in_=pt[:], func=AF.Copy)
        OT = oT[blk % 2]
        for h in range(4):
            Pt = PT[h % 2]
            for mc in range(2):
                pt = PS[6 + mc]
                pe.matmul(pt[:], lhsT=kcT[:, h, mc * 128:(mc + 1) * 128], rhs=QC[:, h, :], start=True, stop=True)
                act.activation(out=Pt[:, mc, :], in_=pt[:], func=AF.Exp, scale=SC)
            po = PS[4]
            pd = PS[5]
            for mc in range(2):
                pe.matmul(po[:], lhsT=Vc[:, mc, h * 128:(h + 1) * 128], rhs=Pt[:, mc, :], start=(mc == 0), stop=(mc == 1))
            for mc in range(2):
                pe.matmul(pd[:], lhsT=ones[:], rhs=Pt[:, mc, :], start=(mc == 0), stop=(mc == 1))
            R = rden[h % 2]
            dve.reciprocal(out=R[:], in_=pd[:])
            dve.tensor_tensor(out=OT[:, h, :], in0=po[:], in1=R[:], op=ALU.mult)
        for j in range(4):
            for half in range(2):
                pt = PS[6 + half]
                for c in range(4):
                    pe.matmul(pt[:], lhsT=OT[:, c, j * 128:(j + 1) * 128], rhs=cwo[:, c, half * 512:(half + 1) * 512], start=(c == 0), stop=(c == 3))
                dve.tensor_tensor(out=X[:, j, half * 512:(half + 1) * 512], in0=X[:, j, half * 512:(half + 1) * 512], in1=pt[:], op=ALU.add)
        sp.dma_start(out=xs[rows, :].rearrange("(j p) d -> p j d", p=128), in_=X[:])
        if g1_block is not None:
            g1_block(X, blk)
    loadE(0)
    loadE(1)
    stage1(0)
    for blk in range(8):
        if blk + 1 < 8:
            stage1(blk + 1)
        stage2(blk)
        if blk + 2 < 8:
            loadE(blk + 2)
    if g1_finish is not None:
        g1_finish()
    kb.end_phase()


def make_g1(kb, nc, layer, W, PS, identf, hn_d, aff_d):
    pe, dve, act, pool, sp = kb.pe, kb.dve, kb.act, kb.pool, kb.sp
    g_bc = kb.sb("G_g", [128, D], F32)
    sp.dma_start(out=g_bc[:], in_=W["norm_ffn_g"][layer].partition_broadcast(128))
    rw = kb.sb("G_rw", [128, 8, NEXP], F32)
    sp.dma_start(out=rw[:], in_=W["router_w"][layer].rearrange("(c p) e -> p c e", p=128))
    Hf = [kb.sb("G_Hf%d" % i, [128, D], F32) for i in range(2)]
    Hb = [kb.sb("G_Hb%d" % i, [128, D], BF16) for i in range(2)]
    HT = [kb.sb("G_HT%d" % i, [128, 8, 128], F32) for i in range(2)]
    junk = kb.sb("G_junk", [128, D], BF16)
    st = [kb.sb("G_st%d" % i, [128, 4], F32) for i in range(2)]
    LG = kb.sb("G_LG", [128, NT, NEXP], F32)
    mx = kb.sb("G_mx", [128, NT], F32)
    wst = [kb.sb("G_wst%d" % i, [NEXP, 512], F32) for i in range(2)]

    def block_fn(X, blk):
        stt = st[blk % 2]
        for j in range(4):
            act.activation(out=junk[:], in_=X[:, j, :], func=AF.Square, accum_out=stt[:, j:j + 1])
        act.activation(out=stt[:], in_=stt[:], func=AF.Sqrt, scale=1.0 / D, bias=EPS)
        dve.reciprocal(out=stt[:], in_=stt[:])
        for j in range(4):
            jt = blk * 4 + j
            H = Hf[j % 2]
            dve.scalar_tensor_tensor(out=H[:], in0=X[:, j, :], scalar=stt[:, j:j + 1], in1=g_bc[:], op0=ALU.mult, op1=ALU.mult)
            B_ = Hb[j % 2]
            act.activation(out=B_[:], in_=H[:], func=AF.Copy)
            sp.dma_start(out=hn_d[jt * 128:(jt + 1) * 128, :], in_=B_[:])
            T = HT[j % 2]
            for half in range(2):
                pt = PS[half]
                for c in range(4):
                    cc = half * 4 + c
                    pe.transpose(pt[:, c * 128:(c + 1) * 128], H[:, cc * 128:(cc + 1) * 128], identf[:])
                act.activation(out=T[:, half * 4:(half + 1) * 4, :], in_=pt[:].rearrange("p (c t) -> p c t", c=4), func=AF.Copy)
            pl = PS[2 + j % 2]
            for c in range(8):
                pe.matmul(pl[:, 0:NEXP], lhsT=T[:, c, :], rhs=rw[:, c, :], start=(c == 0), stop=(c == 7))
            dve.tensor_copy(out=LG[:, jt, :], in_=pl[:, 0:NEXP])

    def finish_fn():
        dve.tensor_reduce(out=mx[:], in_=LG[:], axis=AX.X, op=ALU.max)
        dve.tensor_tensor(out=LG[:], in0=LG[:], in1=mx[:].unsqueeze(2).to_broadcast([128, NT, NEXP]), op=ALU.subtract)
        act.activation(out=LG[:], in_=LG[:], func=AF.Exp)
        dve.tensor_reduce(out=mx[:], in_=LG[:], axis=AX.X, op=ALU.add)
        dve.reciprocal(out=mx[:], in_=mx[:])
        dve.tensor_tensor(out=LG[:], in0=LG[:], in1=mx[:].unsqueeze(2).to_broadcast([128, NT, NEXP]), op=ALU.mult)
        for blk in range(8):
            pt = PS[4 + blk % 2]
            for j in range(4):
                jt = blk * 4 + j
                pe.transpose(pt[0:NEXP, j * 128:(j + 1) * 128], LG[:, jt, :], identf[:])
            wt = wst[blk % 2]
            act.activation(out=wt[:], in_=pt[0:NEXP, :], func=AF.Copy)
            sp.dma_start(out=aff_d[:, blk * 512:(blk + 1) * 512], in_=wt[:])
    return block_fn, finish_fn


def phase_G(kb, nc, layer, W, PS, identb, identf, xs, hn_d, idxT, gT, aff_d):
    pe, dve, act, pool, sp = kb.pe, kb.dve, kb.act, kb.pool, kb.sp
    kb.begin_phase()
    ws = [kb.sb("G_w%d" % i, [128, 8, 1024], BF16) for i in range(6)]
    wgu = W["expert_w_gu"][layer]
    wdn = W["expert_w_down"][layer]

    def load_gu(e, p):
        half, g = p // 2, p % 2
        pool.dma_start(out=ws[p][:], in_=wgu[e][:, g * DFF + half * 1024:g * DFF + (half + 1) * 1024].rearrange("(c p) f -> p c f", p=128))

    def load_wd(e, p):
        pool.dma_start(out=ws[4 + p][:], in_=wdn[e][p * 1024:(p + 1) * 1024, :].rearrange("(c p) n -> p c n", p=128))
    for p in range(4):
        load_gu(0, p)
    for p in range(2):
        load_wd(0, p)
    work = kb.sb("G_work", [NEXP, S], F32)
    sp.dma_start(out=work[:], in_=aff_d[:, :])
    top = kb.sb("G_top", [NEXP, CAP], F32)
    idx = kb.sb("G_idx", [NEXP, CAP], U32)
    for r in range(CAP // 8):
        sl = slice(r * 8, (r + 1) * 8)
        dve.max(out=top[:, sl], in_=work[:])
        dve.max_index(out=idx[:, sl], in_max=top[:, sl], in_values=work[:])
        dve.match_replace(out=work[:], in_to_replace=top[:, sl], in_values=work[:], imm_value=-1.0)
    idxf = kb.sb("G_idxf", [NEXP, CAP], F32)
    dve.tensor_copy(out=idxf[:], in_=idx[:])
    pt = PS[6]
    for ct in range(4):
        pe.transpose(pt[:, ct * NEXP:(ct + 1) * NEXP], idxf[:, ct * 128:(ct + 1) * 128], identf[0:NEXP, 0:NEXP])
    dve.tensor_copy(out=idxT[:], in_=pt[:, 0:4 * NEXP].rearrange("p (c e) -> p c e", c=4))
    pt = PS[7]
    for ct in range(4):
        pe.transpose(pt[:, ct * NEXP:(ct + 1) * NEXP], top[:, ct * 128:(ct + 1) * 128], identf[0:NEXP, 0:NEXP])
    dve.tensor_copy(out=gT[:], in_=pt[:, 0:4 * NEXP].rearrange("p (c e) -> p c e", c=4))
    if DBG.get("idx") is not None:
        sp.dma_start(out=DBG["idx"][:, :], in_=idxT[:].rearrange("p c e -> p (c e)"))
        sp.dma_start(out=DBG["g"][:, :], in_=gT[:].rearrange("p c e -> p (c e)"))
    Xe = [kb.sb("G_Xe%d" % i, [128, 4, D], BF16) for i in range(2)]
    XeT = [kb.sb("G_XeT%d" % i, [128, 8, CAP], BF16) for i in range(2)]
    hT = kb.sb("G_hT", [128, 16, CAP], BF16)
    sg = [kb.sb("G_sg%d" % i, [128, CAP], F32) for i in range(2)]
    yg = [kb.sb("G_yg%d" % i, [128, D], F32) for i in range(2)]
    xs_tok = kb.token("xs_tok")
    def gather(e):
        X = Xe[e % 2]
        for ct in range(4):
            pool.indirect_dma_start(out=X[:, ct, :], out_offset=None, in_=hn_d[:, :],
                                    in_offset=bass.IndirectOffsetOnAxis(ap=idxT[:, ct, e:e + 1].ap, axis=0),
                                    _reads=[idxT])
        return X
    yi = 0
    Xn = gather(0)
    for e in range(NEXP):
        X = Xn
        if e + 1 < NEXP:
            Xn = gather(e + 1)
        XT = XeT[e % 2]
        for ct in range(4):
            pv = bfview(PS[ct % 2])
            for c in range(8):
                pe.transpose(pv[:, c * 128:(c + 1) * 128], X[:, ct, c * 128:(c + 1) * 128], identb[:])
            act.activation(out=XT[:, :, ct * 128:(ct + 1) * 128], in_=pv.rearrange("p (c t) -> p c t", c=8), func=AF.Copy)
        for fj in range(16):
            half, fl = fj // 8, fj % 8
            wga, wup = ws[2 * half], ws[2 * half + 1]
            pa = PS[2 + (fj % 2) * 2]
            pb_ = PS[3 + (fj % 2) * 2]
            for c in range(8):
                pe.matmul(pa[:], lhsT=wga[:, c, fl * 128:(fl + 1) * 128], rhs=XT[:, c, :], start=(c == 0), stop=(c == 7))
            for c in range(8):
                pe.matmul(pb_[:], lhsT=wup[:, c, fl * 128:(fl + 1) * 128], rhs=XT[:, c, :], start=(c == 0), stop=(c == 7))
            sgt = sg[fj % 2]
            act.activation(out=sgt[:], in_=pa[:], func=AF.Sigmoid)
            dve.tensor_tensor(out=sgt[:], in0=sgt[:], in1=pa[:], op=ALU.mult)
            dve.tensor_tensor(out=hT[:, fj, :], in0=sgt[:], in1=pb_[:], op=ALU.mult)
            if fl == 7 and e + 1 < NEXP:
                load_gu(e + 1, 2 * half)
                load_gu(e + 1, 2 * half + 1)
        if e == 0 and DBG.get("X") is not None:
            sp.dma_start(out=DBG["X"][:, :], in_=X[:].rearrange("p c d -> p (c d)"))
            sp.dma_start(out=DBG["hT"][:, :], in_=hT[:].rearrange("p c d -> p (c d)"))
        for ct in range(4):
            Y = yg[yi % 2]
            yi += 1
            for half in range(2):
                pt = PS[6 + half]
                for fj in range(16):
                    pe.matmul(pt[:], lhsT=hT[:, fj, ct * 128:(ct + 1) * 128], rhs=ws[4 + fj // 8][:, fj % 8, half * 512:(half + 1) * 512],
                              start=(fj == 0), stop=(fj == 15))
                dve.tensor_scalar(out=Y[:, half * 512:(half + 1) * 512], in0=pt[:], scalar1=gT[:, ct, e:e + 1], scalar2=None, op0=ALU.mult)
            if e == 0 and ct == 0 and DBG.get("yg") is not None:
                sp.dma_start(out=DBG["yg"][:, :], in_=Y[:])
            pool.indirect_dma_start(out=xs[:, :], out_offset=bass.IndirectOffsetOnAxis(ap=idxT[:, ct, e:e + 1].ap, axis=0),
                                    in_=Y[:], in_offset=None, compute_op=ALU.add,
                                    _reads=[idxT], _writes=[xs_tok])
        if e + 1 < NEXP:
            load_wd(e + 1, 0)
            load_wd(e + 1, 1)
    kb.end_phase()


def rope_tables():
    t = np.arange(S)
    row = (t // 64).astype(np.float32)
    col = (t % 64).astype(np.float32)
    inv = (10000.0 ** (-(np.arange(16, dtype=np.float32)) / 16)).astype(np.float32)
    ang = np.concatenate([row[:, None] * inv, col[:, None] * inv], axis=-1).astype(np.float32)
    return np.cos(ang).astype(np.float32), np.sin(ang).astype(np.float32)


def pool_invcount():
    t = np.arange(S)
    tab = np.zeros((256, S), np.float32)
    for g, win in enumerate((2, 4, 8, 16)):
        lo = np.clip(t - win // 2, 0, S)
        hi = np.clip(t + win // 2, 0, S)
        tab[g * 64:(g + 1) * 64, :] = (1.0 / (hi - lo).astype(np.float32))[None, :]
    return tab


def consts():
    c, s = rope_tables()
    tri = np.zeros((128, 2, 128), np.float32)
    si = np.arange(128)[:, None]
    ji = np.arange(128)[None, :]
    tri[:, 0, :] = (si <= ji)
    tri[:, 1, :] = (si >= ji)
    return {"c_cos": c, "c_sin": s, "c_identf": np.eye(128, dtype=np.float32), "c_invc": pool_invcount(),
            "c_tri": tri.reshape(128, 256)}


_NC_CACHE = {}


def kernel(**inputs):
    if "nc" not in _NC_CACHE:
        _NC_CACHE["nc"] = build()
    nc = _NC_CACHE["nc"]
    cst = consts()
    shared = {k: np.ascontiguousarray(np.asarray(v, dtype=np.float32)) for k, v in inputs.items()
              if k not in ("x", "mem", "mlstm_conv_w")}
    shared["mlstm_conv_wT"] = np.ascontiguousarray(np.asarray(inputs["mlstm_conv_w"], dtype=np.float32).transpose(0, 2, 1))
    shared.update(cst)
    x = np.asarray(inputs["x"], dtype=np.float32)
    mem = np.asarray(inputs["mem"], dtype=np.float32)
    in_maps = []
    for b in range(8):
        m = dict(shared)
        m["x"] = np.ascontiguousarray(x[b])
        m["mem"] = np.ascontiguousarray(mem[b])
        in_maps.append(m)
    res = run_bass_kernel_spmd(nc, in_maps, core_ids=list(range(8)))
    return np.stack([r["out"] for r in res.results], axis=0).astype(np.float32)
```
